# Optimizing a Trainium2 kernel written in Bass

```python
import math
import numpy as np
import jax
import jax.numpy as jnp
from jax import lax

D_MODEL = 1024
BATCH = 32
SEQ = 2048
DEPTH = 2

GRID_W = 64
CTX_LEN = 256
EPS = 1e-6
D_MIX = D_MODEL
W_POOL = D_MIX // 4
W_CONV = D_MIX // 4
W_NA = D_MIX // 4
W_SSD = D_MIX - W_POOL - W_CONV - W_NA
POOL_WINDOWS = (2, 4, 8, 16)
POOL_GROUP = W_POOL // len(POOL_WINDOWS)
CONV_K = 3
NA_HEADS = 4
NA_HEAD_DIM = W_NA // NA_HEADS
WIN_R = 8
WIN_C = 16
COL_BLOCK = 16
COL_BAND = 32
SSD_HEAD_DIM = 64
SSD_HEADS = W_SSD // SSD_HEAD_DIM
SSD_GROUPS = 2
SSD_STATE = 128
SSD_CONV_K = 3
SSD_CHUNK = 128
SSD_XBC = W_SSD + 2 * SSD_GROUPS * SSD_STATE
D_IN_PROJ = W_POOL + 3 * W_CONV + 3 * W_NA + W_SSD + SSD_XBC + 2 * SSD_HEADS
PROJ_SPLITS = (W_POOL, W_POOL + 3 * W_CONV, W_POOL + 3 * W_CONV + 3 * W_NA)
D_FF = 2816
N_EXPERTS = 8
TOP_K = 2
D_FF_EXPERT = 1408

kernel_name = 'hybrid_pool_conv_na_ssd_moe_dit'


def rmsnorm(x, g):
    xf = x.astype(jnp.float32)
    y = xf * lax.rsqrt(jnp.mean(xf * xf, axis=-1, keepdims=True) + EPS)
    return (y * g.astype(jnp.float32)).astype(x.dtype)


def dwconv_centered(u, w, b=None):
    k = w.shape[0]
    L = u.shape[1]
    p = k // 2
    up = jnp.pad(u, ((0, 0), (p, p), (0, 0)))
    y = up[:, 0:L] * w[0]
    for i in range(1, k):
        y = y + up[:, i:i + L] * w[i]
    return y if b is None else y + b


def pool_mix(u, w_grp, scale):
    bn, L, _ = u.shape
    s = jnp.pad(jnp.cumsum(u.astype(jnp.float32), axis=1), ((0, 0), (1, 0), (0, 0)))
    t = np.arange(L)
    outs = []
    for g, win in enumerate(POOL_WINDOWS):
        lo = np.clip(t - win // 2, 0, L)
        hi = np.clip(t - win // 2 + win, 0, L)
        sg = s[..., g * POOL_GROUP:(g + 1) * POOL_GROUP]
        cnt = jnp.asarray((hi - lo)[:, None], jnp.float32)
        mean = (sg[:, hi] - sg[:, lo]) / cnt
        outs.append(mean.astype(u.dtype) - u[..., g * POOL_GROUP:(g + 1) * POOL_GROUP])
    p = jnp.stack(outs, axis=2)
    y = jnp.einsum('blgc,gcd->blgd', p, w_grp).reshape(bn, L, W_POOL)
    return y * scale


def short_conv_mix(pr, w_conv):
    h, bg, cg = jnp.split(pr, 3, axis=-1)
    return bg * dwconv_centered(cg * h, w_conv)


def na_geometry():
    n_cb = GRID_W // COL_BLOCK
    c = np.arange(GRID_W).reshape(n_cb, COL_BLOCK)
    sc = np.clip(c - WIN_C // 2, 0, GRID_W - WIN_C)
    band0 = np.clip(np.arange(n_cb) * COL_BLOCK - WIN_C // 2, 0, GRID_W - COL_BAND)
    band_cols = band0[:, None] + np.arange(COL_BAND)
    kc = band_cols[:, None, :]
    col_mask = (kc >= sc[..., None]) & (kc < sc[..., None] + WIN_C)
    dc_idx = np.clip(kc - c[..., None] + WIN_C - 1, 0, 2 * WIN_C - 2)
    return band_cols, col_mask, dc_idx


def na_attention(q, k, v, k_ctx, v_ctx, rpb):
    bn, S, H, hd = q.shape
    rows = S // GRID_W
    kr = min(WIN_R, rows)
    band_cols, col_mask, dc_idx = na_geometry()
    n_cb = band_cols.shape[0]
    qg = q.reshape(bn, rows, n_cb, COL_BLOCK, H, hd)
    kg = k.reshape(bn, rows, GRID_W, H, hd)
    vg = v.reshape(bn, rows, GRID_W, H, hd)
    sm = 1.0 / math.sqrt(hd)
    mask = col_mask[:, :, None, :]

    def row_fn(r):
        sr = jnp.clip(r - kr // 2, 0, rows - kr)
        q_r = lax.dynamic_index_in_dim(qg, r, axis=1, keepdims=False)
        k_r = lax.dynamic_slice_in_dim(kg, sr, kr, axis=1)[:, :, band_cols]
        v_r = lax.dynamic_slice_in_dim(vg, sr, kr, axis=1)[:, :, band_cols]
        s_win = jnp.einsum('bjqhd,bijkhd->bhjqik', q_r, k_r).astype(jnp.float32) * sm
        ri = sr + jnp.arange(kr) - r + WIN_R - 1
        bias = jnp.take(rpb[:, ri], dc_idx, axis=2).transpose(0, 2, 3, 1, 4)
        s_win = jnp.where(mask, s_win + bias.astype(jnp.float32), -jnp.inf)
        s_win = s_win.reshape(bn, H, n_cb, COL_BLOCK, kr * COL_BAND)
        s_ctx = jnp.einsum('bjqhd,bkhd->bhjqk', q_r, k_ctx).astype(jnp.float32) * sm
        p = jax.nn.softmax(jnp.concatenate([s_win, s_ctx], axis=-1), axis=-1).astype(q.dtype)
        p_win = p[..., :kr * COL_BAND].reshape(bn, H, n_cb, COL_BLOCK, kr, COL_BAND)
        p_ctx = p[..., kr * COL_BAND:]
        return (jnp.einsum('bhjqik,bijkhd->bjqhd', p_win, v_r)
                + jnp.einsum('bhjqk,bkhd->bjqhd', p_ctx, v_ctx))

    o = lax.map(row_fn, jnp.arange(rows))
    return jnp.moveaxis(o, 0, 1).reshape(bn, S, H * hd)


def ctx_attention(q, k, v):
    bn, L, H, hd = q.shape
    s = jnp.einsum('bqhd,bkhd->bhqk', q, k).astype(jnp.float32) * (1.0 / math.sqrt(hd))
    p = jax.nn.softmax(s, axis=-1).astype(q.dtype)
    return jnp.einsum('bhqk,bkhd->bqhd', p, v).reshape(bn, L, H * hd)


def ssd_inputs(pr, conv_w, conv_b):
    bn, L, _ = pr.shape
    z, xbc, dt_raw = jnp.split(pr, [W_SSD, W_SSD + SSD_XBC], axis=-1)
    xbc = jax.nn.silu(dwconv_centered(xbc, conv_w, conv_b))
    xs, bs, cs = jnp.split(xbc, [W_SSD, W_SSD + SSD_GROUPS * SSD_STATE], axis=-1)
    xs = xs.reshape(bn, L, SSD_HEADS, SSD_HEAD_DIM)
    bs = bs.reshape(bn, L, SSD_GROUPS, SSD_STATE)
    cs = cs.reshape(bn, L, SSD_GROUPS, SSD_STATE)
    return z, xs, bs, cs, dt_raw


def ssd_chunked(x, dt, a, bmat, cmat, h0, need_y):
    bn, L, H, P = x.shape
    T = SSD_CHUNK
    nc = L // T
    f32 = jnp.float32
    rep = H // bmat.shape[2]
    bh = jnp.repeat(bmat.astype(f32), rep, axis=2).reshape(bn, nc, T, H, -1)
    ch = jnp.repeat(cmat.astype(f32), rep, axis=2).reshape(bn, nc, T, H, -1)
    xdt = (x.astype(f32) * dt[..., None]).reshape(bn, nc, T, H, P)
    la = (dt * a).reshape(bn, nc, T, H).transpose(0, 3, 1, 2)
    acum = jnp.cumsum(la, axis=-1)
    states = jnp.einsum('bclhn,bhcl,bclhp->bchpn', bh, jnp.exp(acum[..., -1:] - acum), xdt)
    states = jnp.concatenate([h0[:, None].astype(f32), states], axis=1)
    tot = jnp.pad(jnp.cumsum(acum[..., -1], axis=-1), ((0, 0), (0, 0), (1, 0)))
    tril_c = np.tril(np.ones((nc + 1, nc + 1), dtype=bool))
    decay_chunk = jnp.exp(jnp.where(tril_c, tot[..., :, None] - tot[..., None, :], -jnp.inf))
    states = jnp.einsum('bhzc,bchpn->bzhpn', decay_chunk, states)
    h_final = states[:, -1]
    if not need_y:
        return None, h_final
    tril_t = np.tril(np.ones((T, T), dtype=bool))
    lmat = jnp.exp(jnp.where(tril_t, acum[..., :, None] - acum[..., None, :], -jnp.inf))
    scores = jnp.einsum('bclhn,bcshn->bhcls', ch, bh) * lmat
    y = (jnp.einsum('bhcls,bcshp->bclhp', scores, xdt)
         + jnp.einsum('bclhn,bchpn,bhcl->bclhp', ch, states[:, :-1], jnp.exp(acum)))
    return y.reshape(bn, L, H, P), h_final


def rev(t, d):
    return t if d == 0 else jnp.flip(t, axis=1)


def ssd_mix(pr, pr_c, conv_w, conv_b, dt_bias, a_log, d_skip, norm_g, ctx_out):
    z, xs, bs, cs, dtr = ssd_inputs(pr, conv_w, conv_b)
    zc, xsc, bsc, csc, dtrc = ssd_inputs(pr_c, conv_w, conv_b)
    h0 = jnp.zeros((pr.shape[0], SSD_HEADS, SSD_HEAD_DIM, SSD_STATE), jnp.float32)
    y = xs.astype(jnp.float32) * d_skip.astype(jnp.float32)[:, None]
    yc = xsc.astype(jnp.float32) * d_skip.astype(jnp.float32)[:, None] if ctx_out else None
    for d in range(2):
        a = -jnp.exp(a_log[d].astype(jnp.float32))
        hs = slice(d * SSD_HEADS, (d + 1) * SSD_HEADS)
        dt_l = jax.nn.softplus(dtr[..., hs].astype(jnp.float32) + dt_bias[d].astype(jnp.float32))
        dt_c = jax.nn.softplus(dtrc[..., hs].astype(jnp.float32) + dt_bias[d].astype(jnp.float32))
        y_cd, h_c = ssd_chunked(rev(xsc, d), rev(dt_c, d), a, rev(bsc, d), rev(csc, d), h0, ctx_out)
        y_ld, _ = ssd_chunked(rev(xs, d), rev(dt_l, d), a, rev(bs, d), rev(cs, d), h_c, True)
        y = y + rev(y_ld, d)
        if ctx_out:
            yc = yc + rev(y_cd, d)
    o = rmsnorm(y.reshape(z.shape).astype(z.dtype) * jax.nn.silu(z), norm_g)
    if not ctx_out:
        return o, None
    oc = rmsnorm(yc.reshape(zc.shape).astype(zc.dtype) * jax.nn.silu(zc), norm_g)
    return o, oc


def mixer(pr, pr_c, pool_w, pool_scale, conv_w, rpb, s_conv_w, s_conv_b, dt_bias, a_log, d_skip,
          s_norm_g, ctx_out):
    bn, S, _ = pr.shape
    Lc = pr_c.shape[1]
    p_pool, p_conv, p_na, p_ssd = jnp.split(pr, PROJ_SPLITS, axis=-1)
    c_pool, c_conv, c_na, c_ssd = jnp.split(pr_c, PROJ_SPLITS, axis=-1)
    q, k, v = [t.reshape(bn, S, NA_HEADS, NA_HEAD_DIM) for t in jnp.split(p_na, 3, axis=-1)]
    qc, kc, vc = [t.reshape(bn, Lc, NA_HEADS, NA_HEAD_DIM) for t in jnp.split(c_na, 3, axis=-1)]
    o_ssd, oc_ssd = ssd_mix(p_ssd, c_ssd, s_conv_w, s_conv_b, dt_bias, a_log, d_skip, s_norm_g, ctx_out)
    o = jnp.concatenate([pool_mix(p_pool, pool_w, pool_scale),
                         short_conv_mix(p_conv, conv_w),
                         na_attention(q, k, v, kc, vc, rpb),
                         o_ssd], axis=-1)
    if not ctx_out:
        return o, None
    oc = jnp.concatenate([pool_mix(c_pool, pool_w, pool_scale),
                          short_conv_mix(c_conv, conv_w),
                          ctx_attention(qc, kc, vc),
                          oc_ssd], axis=-1)
    return o, oc


def swiglu(h, w_gu, w_down):
    g, u = jnp.split(h @ w_gu, 2, axis=-1)
    return (jax.nn.silu(g) * u) @ w_down


def moe_swiglu(h, w_router, w_gu, w_down):
    logits = (h @ w_router).astype(jnp.float32)
    top_v, top_i = lax.top_k(logits, TOP_K)
    gates = jax.nn.softmax(top_v, axis=-1)
    out = jnp.zeros_like(h)
    for e in range(N_EXPERTS):
        w_e = jnp.sum(jnp.where(top_i == e, gates, 0.0), axis=-1).astype(h.dtype)
        out = out + w_e[..., None] * swiglu(h, w_gu[e], w_down[e])
    return out


def setup_inputs(seed: int = 0) -> dict:
    key = jax.random.key(seed)
    keys = jax.random.split(key, 32)
    n_dense = (DEPTH + 1) // 2
    n_moe = DEPTH // 2

    def nrm(i, shape, s):
        return jax.random.normal(keys[i], shape, jnp.float32) * s

    dt0 = jnp.exp(jax.random.uniform(keys[17], (DEPTH, 2, SSD_HEADS), jnp.float32,
                                     minval=math.log(1e-3), maxval=math.log(1e-1)))
    return {
        'x': nrm(0, (BATCH, SEQ, D_MODEL), 1.0),
        'c': nrm(1, (BATCH, D_MODEL), 1.0),
        'ctx': nrm(2, (BATCH, CTX_LEN, D_MODEL), 1.0),
        'c_ctx': nrm(3, (D_MODEL,), 1.0),
        'w_ada': nrm(4, (DEPTH, D_MODEL, 6 * D_MODEL), D_MODEL ** -0.5),
        'b_ada': nrm(5, (DEPTH, 6 * D_MODEL), 0.02),
        'g_mix': 1.0 + nrm(6, (DEPTH, D_MODEL), 0.05),
        'g_ffn': 1.0 + nrm(7, (DEPTH, D_MODEL), 0.05),
        'w_in': nrm(8, (DEPTH, D_MODEL, D_IN_PROJ), D_MODEL ** -0.5),
        'w_out': nrm(9, (DEPTH, D_MIX, D_MODEL), D_MIX ** -0.5),
        'pool_w': nrm(10, (DEPTH, len(POOL_WINDOWS), POOL_GROUP, POOL_GROUP), POOL_GROUP ** -0.5),
        'pool_scale': 1.0 + nrm(11, (DEPTH, W_POOL), 0.1),
        'conv_w': nrm(12, (DEPTH, CONV_K, W_CONV), CONV_K ** -0.5),
        'na_rpb': nrm(13, (DEPTH, NA_HEADS, 2 * WIN_R - 1, 2 * WIN_C - 1), 0.1),
        'ssd_conv_w': nrm(14, (DEPTH, SSD_CONV_K, SSD_XBC), SSD_CONV_K ** -0.5),
        'ssd_conv_b': nrm(15, (DEPTH, SSD_XBC), 0.01),
        'ssd_dt_bias': dt0 + jnp.log(-jnp.expm1(-dt0)),
        'ssd_a_log': jnp.log(jax.random.uniform(keys[16], (DEPTH, 2, SSD_HEADS), jnp.float32,
                                                minval=1.0, maxval=16.0)),
        'ssd_d': 1.0 + nrm(18, (DEPTH, SSD_HEADS), 0.1),
        'ssd_norm_g': 1.0 + nrm(19, (DEPTH, W_SSD), 0.05),
        'ffn_w_gu': nrm(20, (n_dense, D_MODEL, 2 * D_FF), D_MODEL ** -0.5),
        'ffn_w_down': nrm(21, (n_dense, D_FF, D_MODEL), D_FF ** -0.5),
        'moe_router': nrm(22, (n_moe, D_MODEL, N_EXPERTS), D_MODEL ** -0.5),
        'moe_w_gu': nrm(23, (n_moe, N_EXPERTS, D_MODEL, 2 * D_FF_EXPERT), D_MODEL ** -0.5),
        'moe_w_down': nrm(24, (n_moe, N_EXPERTS, D_FF_EXPERT, D_MODEL), D_FF_EXPERT ** -0.5),
        'g_final': 1.0 + nrm(25, (D_MODEL,), 0.05),
    }


def reference(x, c, ctx, c_ctx, w_ada, b_ada, g_mix, g_ffn, w_in, w_out, pool_w, pool_scale, conv_w,
              na_rpb, ssd_conv_w, ssd_conv_b, ssd_dt_bias, ssd_a_log, ssd_d, ssd_norm_g, ffn_w_gu,
              ffn_w_down, moe_router, moe_w_gu, moe_w_down, g_final):
    s_c = jax.nn.silu(c)
    s_cc = jax.nn.silu(c_ctx)
    xc = ctx
    for l in range(DEPTH):
        ctx_out = l < DEPTH - 1
        sh1, sc1, gt1, sh2, sc2, gt2 = [m[:, None, :] for m in
                                        jnp.split(s_c @ w_ada[l] + b_ada[l], 6, axis=-1)]
        sh1c, sc1c, gt1c, sh2c, sc2c, gt2c = jnp.split(s_cc @ w_ada[l] + b_ada[l], 6, axis=-1)
        h = rmsnorm(x, g_mix[l]) * (1.0 + sc1) + sh1
        hc = rmsnorm(xc, g_mix[l]) * (1.0 + sc1c) + sh1c
        o, oc = mixer(h @ w_in[l], hc @ w_in[l], pool_w[l], pool_scale[l], conv_w[l], na_rpb[l],
                      ssd_conv_w[l], ssd_conv_b[l], ssd_dt_bias[l], ssd_a_log[l], ssd_d[l],
                      ssd_norm_g[l], ctx_out)
        x = x + gt1 * (o @ w_out[l])
        j = l // 2
        h = rmsnorm(x, g_ffn[l]) * (1.0 + sc2) + sh2
        if l % 2 == 0:
            x = x + gt2 * swiglu(h, ffn_w_gu[j], ffn_w_down[j])
        else:
            x = x + gt2 * moe_swiglu(h, moe_router[j], moe_w_gu[j], moe_w_down[j])
        if ctx_out:
            xc = xc + gt1c * (oc @ w_out[l])
            hc = rmsnorm(xc, g_ffn[l]) * (1.0 + sc2c) + sh2c
            if l % 2 == 0:
                xc = xc + gt2c * swiglu(hc, ffn_w_gu[j], ffn_w_down[j])
            else:
                xc = xc + gt2c * moe_swiglu(hc, moe_router[j], moe_w_gu[j], moe_w_down[j])
    return rmsnorm(x, g_final)
```

```python
import numpy as np
import concourse.bass as bass
import concourse.mybir as mybir

F32 = mybir.dt.float32
BF16 = mybir.dt.bfloat16
ALU = mybir.AluOpType
AF = mybir.ActivationFunctionType
AX = mybir.AxisListType


class T:
    def __init__(self, name, handle, space):
        self.name = name
        self.h = handle
        self.space = space
        self.state = {}

    def ap(self):
        return self.h.ap() if self.space == "dram" else self.h[:]

    def __getitem__(self, idx):
        return self.ap()[idx]


class Prog:
    N_DMA_SEMS = 48

    def __init__(self):
        self.nc = bass.Bass("TRN2", target_bir_lowering=False)
        nc = self.nc
        self.eng = {"pe": nc.tensor, "act": nc.scalar, "dve": nc.vector, "pool": nc.gpsimd, "sp": nc.sync}
        self.sem = {}
        self.cnt = {}
        self._cms = []
        for e in self.eng:
            cm = nc.semaphore("s_" + e)
            self.sem[e] = cm.__enter__()
            self._cms.append(cm)
            self.cnt[e] = 0
        self.dma_sems = []
        for i in range(self.N_DMA_SEMS):
            cm = nc.semaphore("d%d" % i)
            self.dma_sems.append(cm.__enter__())
            self._cms.append(cm)
        self.dma_cnt = [0] * self.N_DMA_SEMS
        self.dma_last = [None] * self.N_DMA_SEMS
        self.dma_i = 0
        self.seen = {e: {} for e in self.eng}
        self.n_ops = 0
        self.n_waits = 0
        self.out_deps = []

    def sbuf(self, name, shape, dtype):
        return T(name, self.nc.alloc_sbuf_tensor(name, list(shape), dtype), "sbuf")

    def psum(self, name, shape, dtype=F32):
        return T(name, self.nc.alloc_psum_tensor(name, list(shape), dtype), "psum")

    def dram(self, name, shape, dtype, kind="Internal"):
        return T(name, self.nc.dram_tensor(name, list(shape), dtype, kind=kind), "dram")

    @staticmethod
    def _norm(acc):
        out = []
        for a in acc:
            if isinstance(a, T):
                out.append((a, None))
            else:
                out.append((a[0], a[1]))
        return out

    def _collect(self, reads, writes):
        deps = []
        for t, k in self._norm(reads):
            keys = list(t.state.keys()) if k is None else [kk for kk in (k, None) if kk in t.state]
            for kk in keys:
                w = t.state[kk][0]
                if w is not None:
                    deps.append(w)
        for t, k in self._norm(writes):
            keys = list(t.state.keys()) if k is None else [kk for kk in (k, None) if kk in t.state]
            for kk in keys:
                w, rs = t.state[kk]
                if w is not None:
                    deps.append(w)
                deps.extend(rs.items())
        return deps

    def _update(self, reads, writes, me):
        for t, k in self._norm(reads):
            if k is None:
                if None not in t.state:
                    t.state[None] = [None, {}]
                for kk in t.state:
                    self._addr(t.state[kk][1], me)
            else:
                if k not in t.state:
                    w = t.state[None][0] if None in t.state else None
                    t.state[k] = [w, {}]
                self._addr(t.state[k][1], me)
        for t, k in self._norm(writes):
            if k is None:
                t.state = {None: [me, {}]}
            else:
                t.state[k] = [me, {}]
                if None in t.state:
                    self._addr(t.state[None][1], me)

    @staticmethod
    def _addr(d, me):
        if d.get(me[0], 0) < me[1]:
            d[me[0]] = me[1]

    def _emit_waits(self, e, deps):
        eng = self.eng[e]
        need = {}
        for d in deps:
            if d is None:
                continue
            key, val = d
            if need.get(key, 0) < val:
                need[key] = val
        for key, val in need.items():
            if self.seen[e].get(key, 0) >= val:
                continue
            if key == e and e == "pe":
                continue
            sem = self.sem[key] if isinstance(key, str) else self.dma_sems[key]
            eng.wait_ge(sem, val)
            self.n_waits += 1
            self.seen[e][key] = val

    @staticmethod
    def _excl(reads, writes):
        r2, w2 = [], []
        for a in reads:
            t = a if isinstance(a, T) else a[0]
            if t.space == "psum":
                w2.append(t)
            else:
                r2.append(a)
        for a in writes:
            t = a if isinstance(a, T) else a[0]
            w2.append(t if t.space == "psum" else a)
        return r2, w2

    def op(self, e, fn, reads=(), writes=()):
        reads, writes = self._excl(reads, writes)
        deps = self._collect(reads, writes)
        self._emit_waits(e, deps)
        ins = fn(self.eng[e])
        self.cnt[e] += 1
        ins.then_inc(self.sem[e], 1)
        me = (e, self.cnt[e])
        self._update(reads, writes, me)
        self.n_ops += 1
        return me

    def dma(self, out_ap, in_ap, reads=(), writes=(), q="sp", **kw):
        deps = self._collect(reads, writes)
        i = self.dma_i % self.N_DMA_SEMS
        self.dma_i += 1
        if self.dma_last[i] is not None:
            deps.append(self.dma_last[i])
        self._emit_waits(q, deps)
        ins = self.eng[q].dma_start(out=out_ap, in_=in_ap, **kw)
        self.dma_cnt[i] += 1
        ins.then_inc(self.dma_sems[i], 16)
        me = (i, 16 * self.dma_cnt[i])
        self.dma_last[i] = me
        self._update(reads, writes, me)
        self.n_ops += 1
        return me

    def finish(self, final_deps):
        self._emit_waits("sp", list(final_deps))
        return self.nc


def _mm(self, out, lhsT, rhs, start, stop, R, W):
    return self.op("pe", lambda e: e.matmul(out, lhsT=lhsT, rhs=rhs, start=start, stop=stop), R, W)


def _tr(self, out, in_, ident, R, W):
    return self.op("pe", lambda e: e.transpose(out=out, in_=in_, identity=ident), R, W)


def _act(self, out, in_, func, R, W, **kw):
    return self.op("act", lambda e: e.activation(out=out, in_=in_, func=func, **kw), R, W)


def _tt(self, eng, out, in0, in1, op, R, W):
    return self.op(eng, lambda e: e.tensor_tensor(out=out, in0=in0, in1=in1, op=op), R, W)


def _ts(self, eng, out, in0, s1, s2, op0, op1, R, W):
    if op1 is None:
        return self.op(eng, lambda e: e.tensor_scalar(out=out, in0=in0, scalar1=s1, scalar2=None, op0=op0), R, W)
    return self.op(eng, lambda e: e.tensor_scalar(out=out, in0=in0, scalar1=s1, scalar2=s2, op0=op0, op1=op1), R, W)


def _stt(self, eng, out, in0, scalar, in1, op0, op1, R, W):
    return self.op(eng, lambda e: e.scalar_tensor_tensor(out=out, in0=in0, scalar=scalar, in1=in1, op0=op0, op1=op1), R, W)


def _copy(self, eng, out, in_, R, W):
    if eng == "act":
        return self.op("act", lambda e: e.copy(out=out, in_=in_), R, W)
    return self.op(eng, lambda e: e.tensor_copy(out=out, in_=in_), R, W)


def _memset(self, eng, t_ap, val, W):
    return self.op(eng, lambda e: e.memset(t_ap, val), (), W)


def _phase_begin(self):
    import contextlib
    if not hasattr(self, "_stacks"):
        self._stacks = []
    self._stacks.append(contextlib.ExitStack())
    self._stack = self._stacks[-1]


def _psb(self, name, shape, dtype):
    self._uid = getattr(self, "_uid", 0) + 1
    name = "%s_%d" % (name, self._uid)
    h = self._stack.enter_context(self.nc.sbuf_tensor(name, list(shape), dtype))
    t = T(name, h, "sbuf")
    return t


def _barrier(self):
    deps = [(e, c) for e, c in self.cnt.items() if c > 0]
    deps += [d for d in self.dma_last if d is not None]
    for e in self.eng:
        self._emit_waits(e, [d for d in deps if d[0] != e])


def _phase_end(self):
    self.barrier()
    self._stacks.pop().close()
    self._stack = self._stacks[-1] if self._stacks else None


Prog.mm = _mm
Prog.tr = _tr
Prog.act = _act
Prog.tt = _tt
Prog.ts = _ts
Prog.stt = _stt
Prog.copy = _copy
Prog.memset = _memset
Prog.phase_begin = _phase_begin
Prog.psb = _psb
Prog.barrier = _barrier
Prog.phase_end = _phase_end

from concourse.bass_utils import run_bass_kernel_spmd

D = 1024
SEQ = 2048
CTXL = 256
TOK = 2304
NB = 4
NL = 2
DIN = 2824
WP = 2368
NCH = 18
TILES = [(0, 256)] + [(256 + i * 512, 512) for i in range(4)]
EPS = 1e-6
O_BADA, O_PSC, O_CW, O_SCW, O_SCB, O_DTB, O_ALOG, O_DSK, O_SNG, O_PW, NSM = 0, 48, 50, 56, 74, 80, 88, 96, 352, 608, 864
C_ID, C_TRI, C_TRU, C_SG, C_SL, C_ONE, C_SEL, NCST = 0, 128, 256, 384, 512, 640, 768, 1792


def pcol(t):
    return 16 + t if t < 256 else t + 48


def ccol(c):
    return pcol(c * 128)


def fm_view(dr, r0, nch, t0, n):
    return dr.ap()[r0:r0 + nch * 128, t0:t0 + n].rearrange("(m p) t -> p m t", p=128)


class Model:
    def __init__(self, dbg=False, nl=NL, stop_after=None):
        self.P = Prog()
        self.dbg = dbg
        self.nl = nl
        self.stop_after = stop_after
        P = self.P
        k_in = "ExternalInput"
        self.x_in = P.dram("x", [NB, SEQ, D], F32, k_in)
        self.ctx_in = P.dram("ctx", [NB, CTXL, D], F32, k_in)
        self.cc = P.dram("cc", [D, 5], F32, k_in)
        self.w_ada = P.dram("w_ada", [NL, D, 6 * D], F32, k_in)
        self.w_in = P.dram("w_in", [NL, D, DIN], F32, k_in)
        self.w_out = P.dram("w_out", [NL, D, D], F32, k_in)
        self.small = P.dram("small", [NL, 128, NSM], F32, k_in)
        self.ttab = P.dram("ttab", [NL, 128, 4 * 960], F32, k_in)
        self.gsm = P.dram("gsm", [128, 104], F32, k_in)
        self.cst = P.dram("cst", [128, NCST], F32, k_in)
        self.invc = P.dram("invc", [128, 2 * WP], F32, k_in)
        self.ffn_gu = P.dram("ffn_gu", [D, 5632], F32, k_in)
        self.ffn_down = P.dram("ffn_down", [2816, D], F32, k_in)
        self.moe_gu = P.dram("moe_gu", [8, D, 2816], F32, k_in)
        self.moe_down = P.dram("moe_down", [8, 1408, D], F32, k_in)
        self.out = P.dram("out", [NB, SEQ, D], F32, "ExternalOutput")
        dk = "ExternalOutput" if dbg else "Internal"
        self.w_in_b = [P.dram("w_in_b%d" % l, [D, DIN], BF16) for l in range(NL)]
        self.w_out_b = [P.dram("w_out_b%d" % l, [D, D], BF16) for l in range(NL)]
        self.ffn_gu_b = P.dram("ffn_gu_b", [D, 5632], BF16)
        self.ffn_down_b = P.dram("ffn_down_b", [2816, D], BF16)
        self.moe_gu_b = [P.dram("moe_gu_b%d" % e, [D, 2816], BF16) for e in range(8)]
        self.moe_down_b = [P.dram("moe_down_b%d" % e, [1408, D], BF16) for e in range(8)]
        self.xs = [P.dram("xs%d" % b, [D, TOK], F32, dk if b == 0 else "Internal") for b in range(NB)]
        self.pr = [P.dram("pr%d" % b, [2816, TOK], BF16, dk if b == 0 else "Internal") for b in range(NB)]
        self.vtm = [P.dram("vtm%d" % b, [TOK, 256], BF16, dk if b == 0 else "Internal") for b in range(NB)]
        self.dtm = [P.dram("dtm%d" % b, [8, TOK], F32, dk if b == 0 else "Internal") for b in range(NB)]
        self.o = [P.dram("o%d" % b, [D, TOK], BF16, dk if b == 0 else "Internal") for b in range(NB)]
        self.h2 = [P.dram("h2_%d" % b, [D, TOK], BF16, dk if b == 0 else "Internal") for b in range(NB)]
        self.gT = [P.dram("gT%d" % b, [8, SEQ], F32, dk if b == 0 else "Internal") for b in range(NB)]
        self.ps = [P.psum("ps%d" % i, [128, 512], F32) for i in range(6)]
        self.pb = [P.psum("pb%d" % i, [128, 1024], BF16) for i in range(2)]
        self.psi = 0
        self.cst_s = P.sbuf("cst_s", [128, NCST], F32)
        self.id_b = P.sbuf("id_b", [128, 128], BF16)
        self.one_b = P.sbuf("one_b", [128, 128], BF16)
        self.cst_b = P.sbuf("cst_b", [128, 640], BF16)
        self.sm_s = [P.sbuf("sm_s%d" % l, [128, NSM], F32) for l in range(NL)]
        self.gsm_s = P.sbuf("gsm_s", [128, 104], F32)
        self.s_t = P.sbuf("s_t", [128, 8, 5], F32)
        self.mod = P.sbuf("mod", [128, 48, 5], F32)
        self.G1 = P.sbuf("G1", [128, 8, 5], F32)
        self.G2 = P.sbuf("G2", [128, 8, 5], F32)
        self.pwb = P.sbuf("pwb", [128, 2, 128], BF16)

    def nps(self):
        p = self.ps[self.psi % 6]
        self.psi += 1
        return p

    def setup(self):
        P = self.P
        c = self.cst_s
        P.dma(c[:], self.cst[:], writes=[c])
        for l in range(NL):
            P.dma(self.sm_s[l][:], self.small[l], writes=[self.sm_s[l]])
        P.dma(self.gsm_s[:], self.gsm[:], writes=[self.gsm_s])
        P.copy("dve", self.id_b[:], c[:, C_ID:C_ID + 128], [c], [self.id_b])
        P.copy("dve", self.one_b[:], c[:, C_ONE:C_ONE + 128], [c], [self.one_b])
        P.copy("dve", self.cst_b[:], c[:, 0:640], [c], [self.cst_b])
        st = self.s_t
        P.dma(st[:], self.cc.ap().rearrange("(k p) j -> p k j", p=128), writes=[st])
        P.act(st[:], st[:], AF.Silu, [st], [st])

    def convert(self, layers):
        P = self.P
        P.phase_begin()
        f32b = [P.psb("cv32", [128, 4096], F32) for _ in range(3)]
        b16b = [P.psb("cv16", [128, 4096], BF16) for _ in range(3)]
        jobs = []

        def add(dst, src_ap):
            sv = src_ap.rearrange("(a p) n -> p a n", p=128)
            dv = dst.ap().rearrange("(a p) n -> p a n", p=128)
            A, N = sv.shape[1], sv.shape[2]
            if N <= 2048:
                ga = 4096 // N
                for a0 in range(0, A, ga):
                    g = min(ga, A - a0)
                    jobs.append((dst, sv[:, a0:a0 + g, :], dv[:, a0:a0 + g, :], g, N))
            else:
                for a0 in range(A):
                    for c0 in range(0, N, 4096):
                        n = min(4096, N - c0)
                        jobs.append((dst, sv[:, a0:a0 + 1, c0:c0 + n], dv[:, a0:a0 + 1, c0:c0 + n], 1, n))
        for l in layers:
            add(self.w_in_b[l], self.w_in[l])
            add(self.w_out_b[l], self.w_out[l])
            if l == 0:
                add(self.ffn_gu_b, self.ffn_gu.ap())
                add(self.ffn_down_b, self.ffn_down.ap())
        for i, (dst, sv, dv, g, n) in enumerate(jobs):
            fb, bb = f32b[i % 3], b16b[i % 3]
            fv = fb[:, 0:g * n].rearrange("p (a n) -> p a n", a=g)
            bv = bb[:, 0:g * n].rearrange("p (a n) -> p a n", a=g)
            P.dma(fv, sv, writes=[fb])
            P.copy("pool" if i % 2 == 0 else "act", bv, fv, [fb], [bb])
            P.dma(dv, bv, reads=[bb], writes=[dst])
        P.phase_end()

    def bg_jobs_init(self):
        jobs = []

        def add(dst, src_ap):
            sv = src_ap.rearrange("(a p) n -> p a n", p=128)
            dv = dst.ap().rearrange("(a p) n -> p a n", p=128)
            A, N = sv.shape[1], sv.shape[2]
            for a0 in range(A):
                for c0 in range(0, N, 1024):
                    n = min(1024, N - c0)
                    jobs.append((dst, sv[:, a0, c0:c0 + n], dv[:, a0, c0:c0 + n], n))
        add(self.w_in_b[1], self.w_in[1])
        add(self.w_out_b[1], self.w_out[1])
        for e in range(8):
            add(self.moe_gu_b[e], self.moe_gu[e])
            add(self.moe_down_b[e], self.moe_down[e])
        self.bg_jobs = jobs
        self.bg_i = 0

    def bg_step(self, k, f32b, b16b):
        P = self.P
        for _ in range(k):
            if self.bg_i >= len(self.bg_jobs):
                return
            i = self.bg_i
            self.bg_i += 1
            dst, sv, dv, n = self.bg_jobs[i]
            fb, bb = f32b[i % len(f32b)], b16b[i % len(b16b)]
            P.dma(fb[:, 0:n], sv, writes=[fb], q="pool")
            P.copy("pool", bb[:, 0:n], fb[:, 0:n], [fb], [bb])
            P.dma(dv, bb[:, 0:n], reads=[bb], writes=[dst], q="pool")

    def adaln(self, l):
        P = self.P
        P.phase_begin()
        wa = [P.psb("wa", [128, 8, 1024], F32) for _ in range(2)]
        sm = self.sm_s[l]
        mod = self.mod

        def ld(part):
            P.dma(wa[part % 2][:], self.w_ada[l][:, part * 1024:(part + 1) * 1024].rearrange("(k p) n -> p k n", p=128),
                  writes=[wa[part % 2]])
        ld(0)
        for part in range(6):
            if part + 1 < 6:
                ld(part + 1)
            w = wa[part % 2]
            for fc in range(8):
                ci = part * 8 + fc
                ps = self.nps()
                for k in range(8):
                    P.mm(ps[:, 0:5], w[:, k, fc * 128:(fc + 1) * 128], self.s_t[:, k, :], k == 0, k == 7, [w, self.s_t], [ps])
                P.ts("dve", mod[:, ci, :], ps[:, 0:5], sm[:, O_BADA + ci:O_BADA + ci + 1], None, ALU.add, None,
                     [ps, sm], [(mod, ci)])
        g = self.gsm_s
        for k in range(8):
            P.ts("dve", self.G1[:, k, :], mod[:, 8 + k, :], 1.0, g[:, l * 8 + k:l * 8 + k + 1], ALU.add, ALU.mult,
                 [mod, g], [(self.G1, k)])
            P.ts("dve", self.G2[:, k, :], mod[:, 32 + k, :], 1.0, g[:, 16 + l * 8 + k:16 + l * 8 + k + 1], ALU.add, ALU.mult,
                 [mod, g], [(self.G2, k)])
        P.copy("dve", self.pwb[:], sm[:, O_PW:O_PW + 256].rearrange("p (c n) -> p c n", c=2), [sm], [self.pwb])
        P.phase_end()

    def norm_p1(self, xt, n, sq, rstd, tmp):
        P = self.P
        P.act(sq[:, :, :n], xt[:, :, :n], AF.Square, [xt], [sq])
        ps = self.nps()
        for k in range(8):
            P.mm(ps[:, :n], self.one_b[:], sq[:, k, :n], k == 0, k == 7, [self.one_b, sq], [ps])
        P.act(rstd[:, :n], ps[:, :n], AF.Ln, [ps], [rstd], scale=1.0 / D, bias=EPS)
        P.act(rstd[:, :n], rstd[:, :n], AF.Exp, [rstd], [rstd], scale=-0.5)
        P.tt("dve", tmp[:, :, :n], xt[:, :, :n], rstd[:, :n].unsqueeze(1).broadcast_to([128, 8, n]), ALU.mult, [xt, rstd], [tmp])

    def norm_p2(self, n, G, shoff, j, h_out, tmp, h32=None):
        P = self.P
        for k in range(8):
            if h32 is not None:
                P.act(h32[:, k, :n], tmp[:, k, :n], AF.Identity, [tmp, self.mod, G], [(h32, k)], scale=G[:, k, j:j + 1],
                      bias=self.mod[:, shoff + k, j:j + 1])
            else:
                P.act(h_out[:, k, :n], tmp[:, k, :n], AF.Identity, [tmp, self.mod, G], [(h_out, k)], scale=G[:, k, j:j + 1],
                      bias=self.mod[:, shoff + k, j:j + 1])
        if h32 is not None:
            P.copy("dve", h_out[:, :, :n], h32[:, :, :n], [h32], [h_out])

    def norm_mod(self, xt, n, G, shoff, j, h_out, sq, rstd, tmp, h32=None):
        self.norm_p1(xt, n, sq, rstd, tmp)
        self.norm_p2(n, G, shoff, j, h_out, tmp, h32)

    def phaseA(self, l):
        P = self.P
        P.phase_begin()
        cs = self.cst_s
        w = P.psb("w_in_s", [128, 8, DIN], BF16)
        P.dma(w[:], self.w_in_b[l].ap().rearrange("(k p) n -> p k n", p=128), reads=[self.w_in_b[l]], writes=[w])
        xt2 = [P.psb("xt", [128, 8, 512], F32) for _ in range(2)]
        xtm = [P.psb("xtm", [128, 1024], F32) for _ in range(2)] if l == 0 else None
        sq2 = [P.psb("sq", [128, 8, 512], BF16) for _ in range(2)]
        rstd2 = [P.psb("rstd", [128, 512], F32) for _ in range(2)]
        tmp = P.psb("tmp", [128, 8, 512], F32)
        h2 = [P.psb("h", [128, 8, 512], BF16) for _ in range(2)]
        stg = [P.psb("stg", [128, 22, 512], BF16) for _ in range(2)]
        vst = P.psb("vst", [128, 4, 256], BF16)
        dst = P.psb("dst", [8, 512], F32)
        tiles = [(b, t0, n) for b in range(NB) for (t0, n) in TILES]

        def load(i):
            b, t0, n = tiles[i]
            xt = xt2[i % 2]
            if l == 0:
                for s_ in range(n // 128):
                    xm = xtm[s_ % 2]
                    src = self.ctx_in[b, s_ * 128:(s_ + 1) * 128, :] if t0 == 0 else \
                        self.x_in[b, t0 - 256 + s_ * 128:t0 - 256 + (s_ + 1) * 128, :]
                    P.dma(xm[:], src, writes=[xm])
                    for hf in range(2):
                        ps = self.nps()
                        for kk in range(4):
                            k = hf * 4 + kk
                            P.tr(ps[:, kk * 128:(kk + 1) * 128], xm[:, k * 128:(k + 1) * 128], cs[:, C_ID:C_ID + 128],
                                 [xm, cs], [ps])
                        dst_ap = xt[:, hf * 4:(hf + 1) * 4, s_ * 128:(s_ + 1) * 128]
                        src_ap = ps[:, :].rearrange("p (k t) -> p k t", k=4)
                        P.copy("dve", dst_ap, src_ap, [ps], [xt])
                P.dma(fm_view(self.xs[b], 0, 8, t0, n), xt[:, :, :n], reads=[xt], writes=[self.xs[b]])
            else:
                P.dma(xt[:, :, :n], fm_view(self.xs[b], 0, 8, t0, n), reads=[self.xs[b]], writes=[xt])

        def norm(i):
            b, t0, n = tiles[i]
            j = 4 if t0 == 0 else b
            self.norm_mod(xt2[i % 2], n, self.G1, 0, j, h2[i % 2], sq2[i % 2], rstd2[i % 2], tmp)

        def proj(i):
            b, t0, n = tiles[i]
            h = h2[i % 2]
            st = stg[i % 2]
            ns = n // 128
            for m in range(22):
                ps = self.nps()
                for k in range(8):
                    P.mm(ps[:, :n], w[:, k, m * 128:(m + 1) * 128], h[:, k, :n], k == 0, k == 7, [w, h], [ps])
                if m % 3 == 0:
                    P.copy("act", st[:, m, :n], ps[:, :n], [ps], [(st, m)])
                else:
                    P.copy("dve", st[:, m, :n], ps[:, :n], [ps], [(st, m)])
            P.dma(fm_view(self.pr[b], 0, 22, t0, n), st[:, :, :n], reads=[st], writes=[self.pr[b]])
            for s_ in range(ns):
                ps = self.nps()
                for k in range(8):
                    P.mm(ps[:, 0:256], h[:, k, s_ * 128:(s_ + 1) * 128], w[:, k, 1536:1792], k == 0, k == 7, [w, h], [ps])
                P.copy("dve", vst[:, s_, :], ps[:, 0:256], [ps], [(vst, s_)])
            P.dma(self.vtm[b].ap()[t0:t0 + n, :].rearrange("(s p) c -> p s c", p=128), vst[:, :ns, :],
                  reads=[vst], writes=[self.vtm[b]])
            psd = self.nps()
            for k in range(8):
                P.mm(psd[0:8, :n], w[:, k, 2816:2824], h[:, k, :n], k == 0, k == 7, [w, h], [psd])
            P.copy("dve", dst[:, :n], psd[0:8, :n], [psd], [dst])
            P.dma(self.dtm[b].ap()[:, t0:t0 + n], dst[:, :n], reads=[dst], writes=[self.dtm[b]])

        NT = len(tiles)
        load(0)
        norm(0)
        load(1)
        for i in range(NT):
            if i + 1 < NT:
                norm(i + 1)
            if i + 2 < NT:
                load(i + 2)
            proj(i)
        P.phase_end()

    def zero_pads(self, t):
        P = self.P
        for (c0, c1) in ((0, 16), (272, 304), (2352, WP)):
            P.memset("dve", t[:, :, c0:c1], 0.0, [t])

    def load_padded(self, t, ch, dr, row0, R=None):
        P = self.P
        P.dma(t[:, ch, 16:272], dr.ap()[row0:row0 + 128, 0:256], reads=[dr], writes=[t])
        P.dma(t[:, ch, 304:2352], dr.ap()[row0:row0 + 128, 256:2304], reads=[dr], writes=[t])

    def store_padded(self, dr, row0, src_t, ch):
        P = self.P
        P.dma(dr.ap()[row0:row0 + 128, 0:256], src_t[:, ch, 16:272], reads=[src_t], writes=[dr])
        P.dma(dr.ap()[row0:row0 + 128, 256:2304], src_t[:, ch, 304:2352], reads=[src_t], writes=[dr])

    def m1(self, l, b):
        P = self.P
        sm = self.sm_s[l]
        P.phase_begin()
        invc = P.psb("invc", [128, 2, WP], F32)
        P.dma(invc[:], self.invc.ap().rearrange("p (c w) -> p c w", c=2), writes=[invc])
        up = P.psb("up", [128, 2, WP], BF16)
        self.zero_pads(up)
        for ch in range(2):
            self.load_padded(up, ch, self.pr[b], ch * 128)
        z = P.psb("z", [128, 2, WP], F32)
        y = P.psb("y", [128, 2, WP], BF16)
        A = P.psb("A", [128, WP], F32)
        B = P.psb("B", [128, WP], F32)
        C = P.psb("C", [128, WP], F32)
        Dd = P.psb("Dd", [128, WP], F32)
        tp = P.psb("tp", [128, WP], F32)
        W = WP
        for ch in range(2):
            for c0 in range(0, W, 512):
                n = min(512, W - c0)
                ps = self.nps()
                P.mm(ps[:, :n], self.pwb[:, ch, :], up[:, ch, c0:c0 + n], True, True, [self.pwb, up], [ps])
                P.copy("act", z[:, ch, c0:c0 + n], ps[:, :n], [ps], [z])
            zc = z[:, ch, :]
            P.tt("dve", A[:, 4:W - 4], zc[:, 3:W - 5], zc[:, 4:W - 4], ALU.add, [z], [A])
            if ch == 0:
                P.tt("dve", B[64:128, 6:W - 6], A[64:128, 5:W - 7], A[64:128, 7:W - 5], ALU.add, [A], [B])
                S0, S1 = A, B
            else:
                P.tt("dve", B[:, 6:W - 6], A[:, 5:W - 7], A[:, 7:W - 5], ALU.add, [A], [B])
                P.tt("dve", C[:, 8:W - 8], B[:, 6:W - 10], B[:, 10:W - 6], ALU.add, [B], [C])
                P.tt("dve", Dd[64:128, 12:W - 12], C[64:128, 8:W - 16], C[64:128, 16:W - 8], ALU.add, [C], [Dd])
                S0, S1 = C, Dd
            for (p0, p1, S) in ((0, 64, S0), (64, 128, S1)):
                P.tt("dve", tp[p0:p1, 16:W - 16], S[p0:p1, 16:W - 16], invc[p0:p1, ch, 16:W - 16], ALU.mult, [S, invc], [tp])
                P.tt("dve", tp[p0:p1, 16:W - 16], tp[p0:p1, 16:W - 16], zc[p0:p1, 16:W - 16], ALU.subtract, [tp, z], [tp])
                P.act(y[p0:p1, ch, 16:W - 16], tp[p0:p1, 16:W - 16], AF.Identity, [tp, sm], [y],
                      scale=sm[p0:p1, O_PSC + ch:O_PSC + ch + 1])
            self.store_padded(self.o[b], ch * 128, y, ch)
        hp = P.psb("hp", [128, 2, WP], BF16)
        bp = P.psb("bp", [128, 2, WP], BF16)
        cp = P.psb("cp", [128, 2, WP], BF16)
        for t in (hp, bp, cp):
            self.zero_pads(t)
        y2 = P.psb("y2", [128, 2, WP], BF16)
        for ch in range(2):
            self.load_padded(hp, ch, self.pr[b], 256 + ch * 128)
            self.load_padded(bp, ch, self.pr[b], 512 + ch * 128)
            self.load_padded(cp, ch, self.pr[b], 768 + ch * 128)
        mC = P.psb("mC", [128, WP], F32)
        accC = P.psb("accC", [128, WP], F32)
        for ch in range(2):
            m, acc = mC, accC
            cw = lambda tap: sm[:, O_CW + ch * 3 + tap:O_CW + ch * 3 + tap + 1]
            P.tt("dve", m[:], cp[:, ch, :], hp[:, ch, :], ALU.mult, [cp, hp], [m])
            P.ts("dve", acc[:, 1:W - 1], m[:, 0:W - 2], cw(0), None, ALU.mult, None, [m, sm], [acc])
            P.stt("dve", acc[:, 1:W - 1], m[:, 1:W - 1], cw(1), acc[:, 1:W - 1], ALU.mult, ALU.add, [m, sm, acc], [acc])
            P.stt("dve", acc[:, 1:W - 1], m[:, 2:W], cw(2), acc[:, 1:W - 1], ALU.mult, ALU.add, [m, sm, acc], [acc])
            P.tt("dve", y2[:, ch, 1:W - 1], bp[:, ch, 1:W - 1], acc[:, 1:W - 1], ALU.mult, [bp, acc], [y2])
            self.store_padded(self.o[b], 256 + ch * 128, y2, ch)
        P.phase_end()

    def m2(self, l, b, ctx_out):
        P = self.P
        sm = self.sm_s[l]
        cs = self.cst_s
        W = WP
        P.phase_begin()
        xp = P.psb("xp", [128, 6, WP], BF16)
        self.zero_pads(xp)
        for ch in range(6):
            self.load_padded(xp, ch, self.pr[b], 2048 + ch * 128)
        xc = P.psb("xc", [128, 6, WP], BF16)
        acc = [P.psb("acc", [128, WP], F32) for _ in range(2)]
        for ch in range(6):
            a = acc[ch % 2]
            cw = lambda tap: sm[:, O_SCW + ch * 3 + tap:O_SCW + ch * 3 + tap + 1]
            P.ts("dve", a[:, 1:W - 1], xp[:, ch, 0:W - 2], cw(0), None, ALU.mult, None, [xp, sm], [a])
            P.stt("dve", a[:, 1:W - 1], xp[:, ch, 1:W - 1], cw(1), a[:, 1:W - 1], ALU.mult, ALU.add, [xp, sm, a], [a])
            P.stt("dve", a[:, 1:W - 1], xp[:, ch, 2:W], cw(2), a[:, 1:W - 1], ALU.mult, ALU.add, [xp, sm, a], [a])
            P.act(xc[:, ch, 1:W - 1], a[:, 1:W - 1], AF.Silu, [a, sm], [(xc, ch)], bias=sm[:, O_SCB + ch:O_SCB + ch + 1])
        zf = P.psb("zf", [128, 2, TOK], BF16)
        P.dma(zf[:], fm_view(self.pr[b], 1792, 2, 0, TOK), reads=[self.pr[b]], writes=[zf])
        P.act(zf[:], zf[:], AF.Silu, [zf], [zf])
        XBZ = P.psb("XBZ", [128, NCH, 768], BF16)
        for c in range(NCH):
            pbk = self.pb[c % 2]
            c0 = ccol(c)
            for i in range(4):
                P.tr(pbk[:, i * 128:(i + 1) * 128], xc[:, i, c0:c0 + 128], self.id_b[:], [(xc, i), self.id_b], [pbk])
            for i in range(2):
                P.tr(pbk[:, 512 + i * 128:512 + (i + 1) * 128], zf[:, i, c * 128:(c + 1) * 128], self.id_b[:], [zf, self.id_b], [pbk])
            P.copy("dve" if c % 2 else "act", XBZ[:, c, :], pbk[:, 0:768], [pbk], [(XBZ, c)])
        def t3(name):
            return P.psb(name, [128, NCH, 8], F32)
        dt, la, acum, tot, eac, wdec, dA = [t3(n) for n in ("dt", "la", "acum", "tot", "eac", "wdec", "dA")]
        ea = P.psb("ea", [128, 8], F32)
        dtf = P.psb("dtf", [8, TOK], F32)
        P.dma(dtf[:], self.dtm[b].ap(), reads=[self.dtm[b]], writes=[dtf])
        psq = self.nps()
        for c in range(NCH):
            P.tr(psq[:, c * 8:(c + 1) * 8], dtf[0:8, c * 128:(c + 1) * 128], cs[0:8, C_ID:C_ID + 8], [dtf, cs], [psq])
        P.copy("dve", dt[:].rearrange("p c j -> p (c j)"), psq[:, 0:144], [psq], [dt])
        bc = lambda off: sm[:, off:off + 8].unsqueeze(1).broadcast_to([128, NCH, 8])
        P.tt("dve", dt[:], dt[:], bc(O_DTB), ALU.add, [dt, sm], [dt])
        P.act(dt[:], dt[:], AF.Exp, [dt], [dt])
        P.act(dt[:], dt[:], AF.Ln, [dt], [dt], bias=1.0)
        P.act(ea[:], sm[:, O_ALOG:O_ALOG + 8], AF.Exp, [sm], [ea])
        P.stt("dve", la[:], dt[:], -1.0, ea[:, 0:8].unsqueeze(1).broadcast_to([128, NCH, 8]), ALU.mult, ALU.mult, [dt, ea], [la])
        laf = la[:].rearrange("p c j -> p (c j)")
        psA, psB, psT = self.nps(), self.nps(), self.nps()
        P.mm(psA[:, 0:144], cs[:, C_TRI:C_TRI + 128], laf, True, True, [cs, la], [psA])
        P.mm(psB[:, 0:144], cs[:, C_TRU:C_TRU + 128], laf, True, True, [cs, la], [psB])
        P.mm(psT[:, 0:144], cs[:, C_ONE:C_ONE + 128], laf, True, True, [cs, la], [psT])
        v3 = lambda ps: ps[:, 0:144].rearrange("p (c j) -> p c j", j=8)
        P.copy("dve", acum[:, :, 0:4], v3(psA)[:, :, 0:4], [psA], [acum])
        P.copy("dve", acum[:, :, 4:8], v3(psB)[:, :, 4:8], [psB], [acum])
        P.copy("dve", tot[:], v3(psT), [psT], [tot])
        P.act(eac[:], acum[:], AF.Exp, [acum], [eac])
        P.act(dA[:], tot[:], AF.Exp, [tot], [dA])
        P.tt("dve", wdec[:], tot[:], acum[:], ALU.subtract, [tot, acum], [wdec])
        P.act(wdec[:], wdec[:], AF.Exp, [wdec], [wdec])
        H32 = P.psb("H32", [128, 8, 64], F32)
        Hb = P.psb("Hb", [128, 8, 64], BF16)
        P.memset("dve", H32[:], 0.0, [H32])
        P.memset("dve", Hb[:], 0.0, [Hb])
        ytot = P.psb("ytot", [128, NCH, 256], F32)
        for c in range(NCH):
            P.tt("pool", ytot[:, c, :], XBZ[:, c, 0:256], sm[:, O_DSK:O_DSK + 256], ALU.mult, [(XBZ, c), sm], [(ytot, c)])
        Gm = [[P.psb("Gm", [128, 128], F32) for _ in range(2)] for _ in range(2)]
        LD = [P.psb("LD", [128, 128], F32) for _ in range(2)]
        E = [P.psb("E", [128, 128], F32) for _ in range(2)]
        M = [P.psb("M", [128, 128], BF16) for _ in range(2)]
        xdt = [P.psb("xdt", [128, 64], BF16) for _ in range(2)]
        xdw = [P.psb("xdw", [128, 64], BF16) for _ in range(2)]
        y1 = [P.psb("y1", [128, 64], F32) for _ in range(2)]
        y2 = [P.psb("y2", [128, 64], F32) for _ in range(2)]
        it = 0
        for d in range(2):
            order = list(range(NCH)) if d == 0 else [1, 0] + list(range(NCH - 1, 1, -1))
            cmask = C_TRI if d == 0 else C_TRU
            cstr = C_SG if d == 0 else C_SL
            for c in order:
                need_y = ctx_out or c >= 2
                c0 = ccol(c)
                for g in range(2):
                    gm = Gm[it % 2][g]
                    if need_y:
                        psG = self.nps()
                        P.mm(psG[:, 0:128], xc[:, 2 + g, c0:c0 + 128], xc[:, 4 + g, c0:c0 + 128], True, True, [xc], [psG])
                        P.tt("dve", gm[:], psG[:, 0:128], cs[:, cmask:cmask + 128], ALU.mult, [psG, cs], [gm])
                    for hh in range(2):
                        h = 2 * g + hh
                        j = d * 4 + h
                        i2 = it % 2
                        it += 1
                        if need_y:
                            P.ts("pool", LD[i2][:], cs[:, cstr:cstr + 128], la[:, c, j:j + 1], None, ALU.mult, None, [cs, la], [LD[i2]])
                            psD = self.nps()
                            P.mm(psD[:, 0:128], LD[i2][:], cs[:, cmask:cmask + 128], True, True, [LD[i2], cs], [psD])
                            P.act(E[i2][:], psD[:, 0:128], AF.Exp, [psD], [E[i2]])
                            P.tt("dve", M[i2][:], gm[:], E[i2][:], ALU.mult, [gm, E[i2]], [M[i2]])
                        P.ts("pool", xdt[i2][:], XBZ[:, c, h * 64:(h + 1) * 64], dt[:, c, j:j + 1], None, ALU.mult, None,
                             [(XBZ, c), dt], [xdt[i2]])
                        P.ts("pool", xdw[i2][:], xdt[i2][:], wdec[:, c, j:j + 1], None, ALU.mult, None, [xdt[i2], wdec], [xdw[i2]])
                        if need_y:
                            psY = self.nps()
                            P.mm(psY[:, 0:64], M[i2][:], xdt[i2][:], True, True, [M[i2], xdt[i2]], [psY])
                            psI = self.nps()
                            P.mm(psI[:, 0:64], xc[:, 4 + g, c0:c0 + 128], Hb[:, j, :], True, True, [xc, (Hb, j)], [psI])
                            P.copy("act", y1[i2][:], psY[:, 0:64], [psY], [y1[i2]])
                            P.stt("dve", y2[i2][:], psI[:, 0:64], eac[:, c, j:j + 1], y1[i2][:], ALU.mult, ALU.add,
                                  [psI, eac, y1[i2]], [y2[i2]])
                            P.tt("pool", ytot[:, c, h * 64:(h + 1) * 64], ytot[:, c, h * 64:(h + 1) * 64], y2[i2][:], ALU.add,
                                 [(ytot, c), y2[i2]], [(ytot, c)])
                        psS = self.nps()
                        P.mm(psS[:, 0:64], XBZ[:, c, 256 + g * 128:256 + (g + 1) * 128], xdw[i2][:], True, True,
                             [(XBZ, c), xdw[i2]], [psS])
                        P.stt("dve", H32[:, j, :], H32[:, j, :], dA[:, c, j:j + 1], psS[:, 0:64], ALU.mult, ALU.add,
                              [(H32, j), dA, psS], [(H32, j)])
                        P.copy("act", Hb[:, j, :], H32[:, j, :], [(H32, j)], [(Hb, j)])
        ofm = P.psb("ofm", [128, 2, TOK], BF16)
        gte = [P.psb("gte", [128, 256], F32) for _ in range(2)]
        junk = P.psb("junk", [128, 256], F32)
        ssq = [P.psb("ssq", [128, 1], F32) for _ in range(2)]
        ob = [P.psb("ob", [128, 256], BF16) for _ in range(2)]
        cstart = 0 if ctx_out else 2
        for c in range(cstart, NCH):
            i2 = c % 2
            P.tt("dve", gte[i2][:], ytot[:, c, :], XBZ[:, c, 512:768], ALU.mult, [(ytot, c), (XBZ, c)], [gte[i2]])
            P.act(junk[:], gte[i2][:], AF.Square, [gte[i2]], [junk, ssq[i2]], accum_out=ssq[i2][:])
            P.act(ssq[i2][:], ssq[i2][:], AF.Sqrt, [ssq[i2]], [ssq[i2]], scale=1.0 / 256, bias=EPS)
            P.op("dve", lambda e: e.reciprocal(out=ssq[i2][:], in_=ssq[i2][:]), [ssq[i2]], [ssq[i2]])
            P.stt("dve", ob[i2][:], gte[i2][:], ssq[i2][:, 0:1], sm[:, O_SNG:O_SNG + 256], ALU.mult, ALU.mult,
                  [gte[i2], ssq[i2], sm], [ob[i2]])
            pbk = self.pb[c % 2]
            for i in range(2):
                P.tr(pbk[:, i * 128:(i + 1) * 128], ob[i2][:, i * 128:(i + 1) * 128], self.id_b[:], [ob[i2], self.id_b], [pbk])
            P.copy("act", ofm[:, :, c * 128:(c + 1) * 128], pbk[:, 0:256].rearrange("p (i t) -> p i t", i=2), [pbk], [ofm])
        t0 = cstart * 128
        P.dma(fm_view(self.o[b], 768, 2, t0, TOK - t0), ofm[:, :, t0:TOK], reads=[ofm], writes=[self.o[b]])
        P.phase_end()

    def m3(self, l, b, ctx_out):
        P = self.P
        P.phase_begin()
        q = P.psb("q", [128, 2, TOK], BF16)
        k = P.psb("k", [128, 2, TOK], BF16)
        v = P.psb("v", [128, NCH, 256], BF16)
        vs = P.psb("vs", [128, NCH - 1, 256], BF16)
        P.dma(q[:], fm_view(self.pr[b], 1024, 2, 0, TOK), reads=[self.pr[b]], writes=[q])
        P.dma(k[:], fm_view(self.pr[b], 1280, 2, 0, TOK), reads=[self.pr[b]], writes=[k])
        P.dma(v[:], self.vtm[b].ap().rearrange("(c p) d -> p c d", p=128), reads=[self.vtm[b]], writes=[v])
        P.dma(vs[:], self.vtm[b].ap()[64:64 + (NCH - 1) * 128, :].rearrange("(c p) d -> p c d", p=128),
              reads=[self.vtm[b]], writes=[vs])
        t32 = P.psb("t32", [128, 3840], F32)
        T8 = P.psb("T8", [128, 4, 960], BF16)
        P.dma(t32[:], self.ttab[l], writes=[t32])
        P.act(T8[:].rearrange("p h n -> p (h n)"), t32[:], AF.Copy, [t32], [T8], scale=8.0)
        pT = [P.psb("pT", [128, 6, 64], BF16) for _ in range(2)]
        rec = [P.psb("rec", [64, 512], F32) for _ in range(2)]
        obf = [P.psb("obf", [64, 512], BF16) for _ in range(2)]
        it = 0
        for h in range(4):
            p0 = (h % 2) * 64
            ch = h // 2
            if ctx_out:
                psS = self.nps()
                for jc in range(2):
                    P.mm(psS[:, jc * 256:(jc + 1) * 256], k[p0:p0 + 64, ch, jc * 128:(jc + 1) * 128], q[p0:p0 + 64, ch, 0:256],
                         True, True, [k, q], [psS])
                pt2 = P.psb("pt2", [128, 512], BF16)
                P.act(pt2[:], psS[:, :], AF.Exp, [psS], [pt2], scale=0.125)
                pn, pd = self.nps(), self.nps()
                for jc in range(2):
                    P.mm(pn[0:64, 0:256], v[:, jc, h * 64:(h + 1) * 64], pt2[:, jc * 256:(jc + 1) * 256], jc == 0, jc == 1, [v, pt2], [pn])
                for jc in range(2):
                    P.mm(pd[0:64, 0:256], self.one_b[:, 0:64], pt2[:, jc * 256:(jc + 1) * 256], jc == 0, jc == 1, [self.one_b, pt2], [pd])
                i2 = it % 2
                it += 1
                P.op("dve", lambda e: e.reciprocal(out=rec[i2][:, 0:256], in_=pd[0:64, 0:256]), [pd], [rec[i2]])
                P.tt("dve", obf[i2][:, 0:256], pn[0:64, 0:256], rec[i2][:, 0:256], ALU.mult, [pn, rec[i2]], [obf[i2]])
                P.dma(self.o[b].ap()[512 + h * 64:512 + (h + 1) * 64, 0:256], obf[i2][:, 0:256], reads=[obf[i2]], writes=[self.o[b]])
            for r0 in range(0, 32, 8):
                gi = (r0 // 8) % 2
                pn, pd = self.ps[gi * 2], self.ps[gi * 2 + 1]
                for r in range(r0, r0 + 8):
                    sr = min(max(r - 4, 0), 24)
                    psS = self.ps[4 + r % 2]
                    qs = q[p0:p0 + 64, ch, 256 + r * 64:256 + (r + 1) * 64]
                    kts = []
                    for jc in range(6):
                        kt0 = 256 + (sr + 2 * jc) * 64 if jc < 4 else (jc - 4) * 128
                        kts.append(kt0)
                        P.mm(psS[:, jc * 64:(jc + 1) * 64], k[p0:p0 + 64, ch, kt0:kt0 + 128], qs, True, jc >= 4, [k, q], [psS])
                        if jc < 4:
                            off = (sr - r + 7 + 2 * jc) * 64
                            P.mm(psS[:, jc * 64:(jc + 1) * 64], T8[p0:p0 + 64, h, off:off + 128], self.id_b[p0:p0 + 64, p0:p0 + 64],
                                 False, True, [T8, self.id_b], [psS])
                    pt = pT[r % 2]
                    P.act(pt[:].rearrange("p a b -> p (a b)"), psS[:, 0:384], AF.Exp, [psS], [pt], scale=0.125)
                    col = (r - r0) * 64
                    for jc in range(6):
                        kt0 = kts[jc]
                        if kt0 % 128 == 0:
                            vv = v[:, kt0 // 128, h * 64:(h + 1) * 64]
                        else:
                            vv = vs[:, (kt0 - 64) // 128, h * 64:(h + 1) * 64]
                        P.mm(pn[0:64, col:col + 64], vv, pt[:, jc, :], jc == 0, jc == 5, [v, vs, pt], [pn])
                    for jc in range(6):
                        P.mm(pd[0:64, col:col + 64], self.one_b[:, 0:64], pt[:, jc, :], jc == 0, jc == 5, [self.one_b, pt], [pd])
                i2 = it % 2
                it += 1
                P.op("dve", lambda e: e.reciprocal(out=rec[i2][:], in_=pd[0:64, :]), [pd], [rec[i2]])
                P.tt("dve", obf[i2][:], pn[0:64, :], rec[i2][:], ALU.mult, [pn, rec[i2]], [obf[i2]])
                P.dma(self.o[b].ap()[512 + h * 64:512 + (h + 1) * 64, 256 + r0 * 64:256 + r0 * 64 + 512], obf[i2][:],
                      reads=[obf[i2]], writes=[self.o[b]])
        P.phase_end()

    def m23(self, l, b, ctx_out):
        P = self.P
        sm = self.sm_s[l]
        cs = self.cst_s
        W = WP
        P.phase_begin()
        xc = P.psb("xc", [128, 6, WP], BF16)
        XBZ = P.psb("XBZ", [128, NCH, 768], BF16)
        T8 = P.psb("T8", [128, 4, 960], BF16)
        P.phase_begin()
        xp = P.psb("xp", [128, 6, WP], BF16)
        self.zero_pads(xp)
        for ch in range(6):
            self.load_padded(xp, ch, self.pr[b], 2048 + ch * 128)
        zf = P.psb("zf", [128, 2, TOK], BF16)
        P.dma(zf[:], fm_view(self.pr[b], 1792, 2, 0, TOK), reads=[self.pr[b]], writes=[zf])
        t32 = P.psb("t32", [128, 3840], F32)
        P.dma(t32[:], self.ttab[l], writes=[t32])
        acc = [P.psb("acc", [128, WP], F32) for _ in range(2)]
        for ch in range(6):
            a = acc[ch % 2]
            cw = lambda tap: sm[:, O_SCW + ch * 3 + tap:O_SCW + ch * 3 + tap + 1]
            eng = "dve"
            P.ts(eng, a[:, 1:W - 1], xp[:, ch, 0:W - 2], cw(0), None, ALU.mult, None, [xp, sm], [a])
            P.stt(eng, a[:, 1:W - 1], xp[:, ch, 1:W - 1], cw(1), a[:, 1:W - 1], ALU.mult, ALU.add, [xp, sm, a], [a])
            P.stt(eng, a[:, 1:W - 1], xp[:, ch, 2:W], cw(2), a[:, 1:W - 1], ALU.mult, ALU.add, [xp, sm, a], [a])
            P.act(xc[:, ch, 1:W - 1], a[:, 1:W - 1], AF.Silu, [a, sm], [(xc, ch)], bias=sm[:, O_SCB + ch:O_SCB + ch + 1])
        P.act(zf[:], zf[:], AF.Silu, [zf], [zf])
        P.act(T8[:].rearrange("p h n -> p (h n)"), t32[:], AF.Copy, [t32], [T8], scale=8.0)
        for c in range(NCH):
            pbk = self.pb[c % 2]
            c0 = ccol(c)
            for i in range(4):
                P.tr(pbk[:, i * 128:(i + 1) * 128], xc[:, i, c0:c0 + 128], self.id_b[:], [(xc, i), self.id_b], [pbk])
            for i in range(2):
                P.tr(pbk[:, 512 + i * 128:512 + (i + 1) * 128], zf[:, i, c * 128:(c + 1) * 128], self.id_b[:], [zf, self.id_b], [pbk])
            P.copy("dve" if c % 2 else "act", XBZ[:, c, :], pbk[:, 0:768], [pbk], [(XBZ, c)])
        P.phase_end()
        q = P.psb("q", [128, 2, TOK], BF16)
        k = P.psb("k", [128, 2, TOK], BF16)
        v = P.psb("v", [128, NCH, 256], BF16)
        vs = P.psb("vs", [128, NCH - 1, 256], BF16)
        P.dma(q[:], fm_view(self.pr[b], 1024, 2, 0, TOK), reads=[self.pr[b]], writes=[q])
        P.dma(k[:], fm_view(self.pr[b], 1280, 2, 0, TOK), reads=[self.pr[b]], writes=[k])
        P.dma(v[:], self.vtm[b].ap().rearrange("(c p) d -> p c d", p=128), reads=[self.vtm[b]], writes=[v])
        P.dma(vs[:], self.vtm[b].ap()[64:64 + (NCH - 1) * 128, :].rearrange("(c p) d -> p c d", p=128),
              reads=[self.vtm[b]], writes=[vs])

        def t3(name):
            return P.psb(name, [128, NCH, 8], F32)
        dt, la, acum, tot, eac, wdec, dA, dtw = [t3(n) for n in ("dt", "la", "acum", "tot", "eac", "wdec", "dA", "dtw")]
        ea = P.psb("ea", [128, 8], F32)
        dtf = P.psb("dtf", [8, TOK], F32)
        P.dma(dtf[:], self.dtm[b].ap(), reads=[self.dtm[b]], writes=[dtf])
        psq = self.ps[4]
        for c in range(NCH):
            P.tr(psq[:, c * 8:(c + 1) * 8], dtf[0:8, c * 128:(c + 1) * 128], cs[0:8, C_ID:C_ID + 8], [dtf, cs], [psq])
        P.copy("dve", dt[:].rearrange("p c j -> p (c j)"), psq[:, 0:144], [psq], [dt])
        bc = lambda off: sm[:, off:off + 8].unsqueeze(1).broadcast_to([128, NCH, 8])
        P.tt("dve", dt[:], dt[:], bc(O_DTB), ALU.add, [dt, sm], [dt])
        P.act(dt[:], dt[:], AF.Exp, [dt], [dt])
        P.act(dt[:], dt[:], AF.Ln, [dt], [dt], bias=1.0)
        P.act(ea[:], sm[:, O_ALOG:O_ALOG + 8], AF.Exp, [sm], [ea])
        P.stt("dve", la[:], dt[:], -1.0, ea[:, 0:8].unsqueeze(1).broadcast_to([128, NCH, 8]), ALU.mult, ALU.mult, [dt, ea], [la])
        la_hb = P.psb("la_hb", [128, NCH, 8], BF16)
        la_hi = t3("la_hi")
        la_lo = t3("la_lo")
        P.copy("dve", la_hb[:], la[:], [la], [la_hb])
        P.copy("dve", la_hi[:], la_hb[:], [la_hb], [la_hi])
        P.tt("dve", la_lo[:], la[:], la_hi[:], ALU.subtract, [la, la_hi], [la_lo])
        laf = la[:].rearrange("p c j -> p (c j)")
        psA, psB, psT = self.ps[4], self.ps[5], self.ps[0]
        P.mm(psA[:, 0:144], cs[:, C_TRI:C_TRI + 128], laf, True, True, [cs, la], [psA])
        P.mm(psB[:, 0:144], cs[:, C_TRU:C_TRU + 128], laf, True, True, [cs, la], [psB])
        P.mm(psT[:, 0:144], cs[:, C_ONE:C_ONE + 128], laf, True, True, [cs, la], [psT])
        v3 = lambda ps: ps[:, 0:144].rearrange("p (c j) -> p c j", j=8)
        P.copy("dve", acum[:, :, 0:4], v3(psA)[:, :, 0:4], [psA], [acum])
        P.copy("dve", acum[:, :, 4:8], v3(psB)[:, :, 4:8], [psB], [acum])
        P.copy("dve", tot[:], v3(psT), [psT], [tot])
        P.act(eac[:], acum[:], AF.Exp, [acum], [eac])
        P.act(dA[:], tot[:], AF.Exp, [tot], [dA])
        P.tt("dve", wdec[:], tot[:], acum[:], ALU.subtract, [tot, acum], [wdec])
        P.act(wdec[:], wdec[:], AF.Exp, [wdec], [wdec])
        P.tt("dve", dtw[:], dt[:], wdec[:], ALU.mult, [dt, wdec], [dtw])
        H32 = P.psb("H32", [128, 8, 64], F32)
        Hb = P.psb("Hb", [128, 8, 64], BF16)
        P.memset("dve", H32[:], 0.0, [H32])
        P.memset("dve", Hb[:], 0.0, [Hb])
        ytot = P.psb("ytot", [128, NCH, 256], F32)
        P.tt("dve", ytot[:], XBZ[:, :, 0:256], sm[:, O_DSK:O_DSK + 256].unsqueeze(1).broadcast_to([128, NCH, 256]), ALU.mult,
             [XBZ, sm], [ytot])
        NBUF = 6
        Gm = [P.psb("Gm", [128, 128], F32) for _ in range(4)]
        LDf = [P.psb("LDf", [128, 128], F32) for _ in range(NBUF)]
        E = [P.psb("E", [128, 128], F32) for _ in range(NBUF)]
        M = [P.psb("M", [128, 128], BF16) for _ in range(NBUF)]
        xdw = [P.psb("xdw", [128, 64], BF16) for _ in range(NBUF)]
        subs = []
        gcount = 0
        for d in range(2):
            order = list(range(NCH)) if d == 0 else [1, 0] + list(range(NCH - 1, 1, -1))
            for c in order:
                for g in range(2):
                    for hh in range(2):
                        subs.append(dict(d=d, c=c, g=g, hh=hh, h=2 * g + hh, j=d * 4 + 2 * g + hh, gi=gcount,
                                         need_y=(ctx_out or c >= 2), c0=ccol(c),
                                         cmask=(C_TRI if d == 0 else C_TRU), cstr=(C_SG if d == 0 else C_SL)))
                    gcount += 1
        NS = len(subs)
        bank = {}
        _nps = self.nps

        def nps_safe():
            for _ in range(12):
                p = _nps()
                if all(p is not q_ for q_ in bank.values()):
                    return p
            raise RuntimeError("no free PSUM bank")

        def S0(i):
            u = subs[i]; c, g, h, j, c0 = u["c"], u["g"], u["h"], u["j"], u["c0"]; ib = i % NBUF
            if u["need_y"]:
                if u["hh"] == 0:
                    bG = nps_safe()
                    gm = Gm[u["gi"] % 4]
                    P.mm(bG[:, 0:128], xc[:, 2 + g, c0:c0 + 128], xc[:, 4 + g, c0:c0 + 128], True, True, [xc], [bG])
                    P.tt("dve", gm[:], bG[:, 0:128], cs[:, u["cmask"]:u["cmask"] + 128], ALU.mult, [bG, cs], [gm])
                P.act(LDf[ib][:], cs[:, u["cstr"]:u["cstr"] + 128], AF.Identity, [cs, la], [LDf[ib]], scale=la[:, c, j:j + 1])
            P.act(xdw[ib][:], XBZ[:, c, h * 64:(h + 1) * 64], AF.Identity, [(XBZ, c), dtw], [xdw[ib]], scale=dtw[:, c, j:j + 1])

        def S1(i):
            u = subs[i]; c, g, h, j, c0 = u["c"], u["g"], u["h"], u["j"], u["c0"]; ib = i % NBUF
            if u["need_y"]:
                bD = nps_safe()
                P.mm(bD[:, 0:128], LDf[ib][:], cs[:, u["cmask"]:u["cmask"] + 128], True, True, [LDf[ib], cs], [bD])
                bI = nps_safe()
                P.mm(bI[:, 0:64], xc[:, 4 + g, c0:c0 + 128], Hb[:, j, :], True, True, [xc, (Hb, j)], [bI])
                bank[(i, "D")] = bD
                bank[(i, "I")] = bI
            bS = nps_safe()
            P.mm(bS[:, 0:64], XBZ[:, c, 256 + g * 128:256 + (g + 1) * 128], xdw[ib][:], True, True, [(XBZ, c), xdw[ib]], [bS])
            bank[(i, "S")] = bS

        def S2(i):
            u = subs[i]; c, g, h, j = u["c"], u["g"], u["h"], u["j"]; ib = i % NBUF
            ysl = ytot[:, c, h * 64:(h + 1) * 64]
            if u["need_y"]:
                bD = bank.pop((i, "D"))
                bI = bank.pop((i, "I"))
                P.act(E[ib][:], bD[:, 0:128], AF.Exp, [bD], [E[ib]])
                P.stt("dve", ysl, bI[:, 0:64], eac[:, c, j:j + 1], ysl, ALU.mult, ALU.add, [bI, eac, (ytot, c)], [(ytot, c)])
            bS = bank.pop((i, "S"))
            P.stt("dve", H32[:, j, :], H32[:, j, :], dA[:, c, j:j + 1], bS[:, 0:64], ALU.mult, ALU.add,
                  [(H32, j), dA, bS], [(H32, j)])
            P.copy("pool", Hb[:, j, :], H32[:, j, :], [(H32, j)], [(Hb, j)])

        def S3(i):
            u = subs[i]; c, h, j = u["c"], u["h"], u["j"]; ib = i % NBUF
            if u["need_y"]:
                gm = Gm[u["gi"] % 4]
                P.stt("dve", M[ib][:], gm[:], dt[:, c, j:j + 1], E[ib][:], ALU.mult, ALU.mult, [gm, dt, E[ib]], [M[ib]])
                bY = nps_safe()
                P.mm(bY[:, 0:64], M[ib][:], XBZ[:, c, h * 64:(h + 1) * 64], True, True, [M[ib], (XBZ, c)], [bY])
                bank[(i, "Y")] = bY

        def S4(i):
            u = subs[i]; c, h = u["c"], u["h"]
            if u["need_y"]:
                ysl = ytot[:, c, h * 64:(h + 1) * 64]
                bY = bank.pop((i, "Y"))
                P.tt("dve", ysl, bY[:, 0:64], ysl, ALU.add, [bY, (ytot, c)], [(ytot, c)])

        def ssd_gen():
            stages = [S0, S1, S2, S3, S4]
            for t in range(NS + len(stages) - 1):
                for k in reversed(range(len(stages))):
                    i = t - k
                    if 0 <= i < NS:
                        stages[k](i)
                yield

        pT = [P.psb("pT", [128, 6, 64], BF16) for _ in range(3)]
        rec = [P.psb("rec", [64, 512], F32) for _ in range(2)]
        obf = [P.psb("obf", [64, 512], BF16) for _ in range(2)]
        pt2 = P.psb("pt2", [128, 512], BF16) if ctx_out else None

        def na_gen():
            it = 0
            for h in range(4):
                p0 = (h % 2) * 64
                ch = h // 2
                if ctx_out:
                    psS, pn, pd = self.ps[4], self.ps[0], self.ps[1]
                    for jc in range(2):
                        P.mm(psS[:, jc * 256:(jc + 1) * 256], k[p0:p0 + 64, ch, jc * 128:(jc + 1) * 128], q[p0:p0 + 64, ch, 0:256],
                             True, True, [k, q], [psS])
                    P.act(pt2[:], psS[:, :], AF.Exp, [psS], [pt2], scale=0.125)
                    for jc in range(2):
                        P.mm(pn[0:64, 0:256], v[:, jc, h * 64:(h + 1) * 64], pt2[:, jc * 256:(jc + 1) * 256], jc == 0, jc == 1, [v, pt2], [pn])
                    for jc in range(2):
                        P.mm(pd[0:64, 0:256], self.one_b[:, 0:64], pt2[:, jc * 256:(jc + 1) * 256], jc == 0, jc == 1, [self.one_b, pt2], [pd])
                    i2 = it % 2
                    it += 1
                    P.op("dve", lambda e: e.reciprocal(out=rec[i2][:, 0:256], in_=pd[0:64, 0:256]), [pd], [rec[i2]])
                    P.tt("dve", obf[i2][:, 0:256], pn[0:64, 0:256], rec[i2][:, 0:256], ALU.mult, [pn, rec[i2]], [obf[i2]])
                    P.dma(self.o[b].ap()[512 + h * 64:512 + (h + 1) * 64, 0:256], obf[i2][:, 0:256], reads=[obf[i2]], writes=[self.o[b]])
                    yield
                def scores(r):
                    sr = min(max(r - 4, 0), 24)
                    psS = self.ps[4 + r % 2]
                    qs = q[p0:p0 + 64, ch, 256 + r * 64:256 + (r + 1) * 64]
                    kts = []
                    for jc in range(6):
                        kt0 = 256 + (sr + 2 * jc) * 64 if jc < 4 else (jc - 4) * 128
                        kts.append(kt0)
                        P.mm(psS[:, jc * 64:(jc + 1) * 64], k[p0:p0 + 64, ch, kt0:kt0 + 128], qs, True, jc >= 4, [k, q], [psS])
                        if jc < 4:
                            off = (sr - r + 7 + 2 * jc) * 64
                            P.mm(psS[:, jc * 64:(jc + 1) * 64], T8[p0:p0 + 64, h, off:off + 128], self.id_b[p0:p0 + 64, p0:p0 + 64],
                                 False, True, [T8, self.id_b], [psS])
                    pt = pT[r % 3]
                    P.act(pt[:].rearrange("p a b -> p (a b)"), psS[:, 0:384], AF.Exp, [psS], [pt], scale=0.125)
                    return kts

                def pv(r, kts):
                    nonlocal it
                    r0 = (r // 8) * 8
                    gi_ = (r0 // 8) % 2
                    pn, pd = self.ps[gi_ * 2], self.ps[gi_ * 2 + 1]
                    pt = pT[r % 3]
                    col = (r - r0) * 64
                    for jc in range(6):
                        kt0 = kts[jc]
                        if kt0 % 128 == 0:
                            vv = v[:, kt0 // 128, h * 64:(h + 1) * 64]
                        else:
                            vv = vs[:, (kt0 - 64) // 128, h * 64:(h + 1) * 64]
                        P.mm(pn[0:64, col:col + 64], vv, pt[:, jc, :], jc == 0, jc == 5, [v, vs, pt], [pn])
                    for jc in range(6):
                        P.mm(pd[0:64, col:col + 64], self.one_b[:, 0:64], pt[:, jc, :], jc == 0, jc == 5, [self.one_b, pt], [pd])
                    if r == r0 + 7:
                        i2 = it % 2
                        it += 1
                        P.op("dve", lambda e: e.reciprocal(out=rec[i2][:], in_=pd[0:64, :]), [pd], [rec[i2]])
                        P.tt("dve", obf[i2][:], pn[0:64, :], rec[i2][:], ALU.mult, [pn, rec[i2]], [obf[i2]])
                        P.dma(self.o[b].ap()[512 + h * 64:512 + (h + 1) * 64, 256 + r0 * 64:256 + r0 * 64 + 512], obf[i2][:],
                              reads=[obf[i2]], writes=[self.o[b]])

                prev = None
                for r in range(33):
                    cur = scores(r) if r < 32 else None
                    if prev is not None:
                        pv(r - 1, prev)
                    prev = cur
                    yield

        import os
        g1, g2 = ssd_gen(), na_gen()
        alive = [g1, g2]
        if os.environ.get("K_M23") == "nossd":
            alive = [g2]
        if os.environ.get("K_M23") == "nona":
            alive = [g1]
        for g in alive:
            for _ in g:
                pass
        ofm = P.psb("ofm", [128, 2, TOK], BF16)
        junk = P.psb("junk", [128, 256], F32)
        ssq = P.psb("ssq", [128, NCH], F32)
        obA = P.psb("obA", [128, NCH, 256], BF16)
        cstart = 0 if ctx_out else 2
        ncc = NCH - cstart
        P.tt("dve", ytot[:, cstart:, :], ytot[:, cstart:, :], XBZ[:, cstart:, 512:768], ALU.mult, [ytot, XBZ], [ytot])
        for c in range(cstart, NCH):
            P.act(junk[:], ytot[:, c, :], AF.Square, [ytot], [junk, (ssq, c)], accum_out=ssq[:, c:c + 1])
        P.act(ssq[:, cstart:], ssq[:, cstart:], AF.Ln, [ssq], [ssq], scale=1.0 / 256, bias=EPS)
        P.act(ssq[:, cstart:], ssq[:, cstart:], AF.Exp, [ssq], [ssq], scale=-0.5)
        P.tt("dve", ytot[:, cstart:, :], ytot[:, cstart:, :], ssq[:, cstart:].unsqueeze(2).broadcast_to([128, ncc, 256]), ALU.mult,
             [ytot, ssq], [ytot])
        P.tt("dve", obA[:, cstart:, :], ytot[:, cstart:, :], sm[:, O_SNG:O_SNG + 256].unsqueeze(1).broadcast_to([128, ncc, 256]),
             ALU.mult, [ytot, sm], [obA])
        gi = 0
        for c4 in range(cstart, NCH, 4):
            cs_ = list(range(c4, min(c4 + 4, NCH)))
            pbk = self.pb[gi % 2]
            gi += 1
            for ci, c in enumerate(cs_):
                for i in range(2):
                    P.tr(pbk[:, (i * 4 + ci) * 128:(i * 4 + ci + 1) * 128], obA[:, c, i * 128:(i + 1) * 128], self.id_b[:],
                         [obA, self.id_b], [pbk])
            nn = len(cs_)
            P.copy("act" if gi % 2 else "dve", ofm[:, :, c4 * 128:(c4 + nn) * 128],
                   pbk[:, :].rearrange("p (i t) -> p i t", i=2)[:, :, 0:nn * 128], [pbk], [ofm])
        t0 = cstart * 128
        P.dma(fm_view(self.o[b], 768, 2, t0, TOK - t0), ofm[:, :, t0:TOK], reads=[ofm], writes=[self.o[b]])
        P.phase_end()

    def c1(self, l):
        P = self.P
        cs = self.cst_s
        moe = (l % 2 == 1)
        last = (l == NL - 1)
        P.phase_begin()
        wo = P.psb("wo", [128, 8, D], BF16)
        P.dma(wo[:], self.w_out_b[l].ap().rearrange("(k p) n -> p k n", p=128), reads=[self.w_out_b[l]], writes=[wo])
        xt2 = [P.psb("xt", [128, 8, 512], F32) for _ in range(2)]
        ot2 = [P.psb("ot", [128, 8, 512], BF16) for _ in range(2)]
        sq = P.psb("sq", [128, 8, 512], BF16)
        rstd = P.psb("rstd", [128, 512], F32)
        tmp = P.psb("tmp", [128, 8, 512], F32)
        hb2 = [P.psb("hb", [128, 8, 512], BF16) for _ in range(2)]
        h32 = P.psb("h32", [128, 8, 512], F32) if moe else None
        if moe:
            wr = self.gsm_s
            lg = P.psb("lg", [128, 32], F32)
            lg2 = P.psb("lg2", [128, 32], F32)
            eq1 = P.psb("eq1", [128, 32], F32)
            eq2 = P.psb("eq2", [128, 32], F32)
            gt = P.psb("gt", [128, 32], F32)
            m1 = P.psb("m1", [128, 4], F32)
            m2 = P.psb("m2", [128, 4], F32)
            dd = P.psb("dd", [128, 4], F32)
            ee = P.psb("ee", [128, 4], F32)
            g1 = P.psb("g1", [128, 4], F32)
            g2 = P.psb("g2", [128, 4], F32)
            gts = P.psb("gts", [8, 512], F32)
        tiles = [(b, t0, n) for b in range(NB) for (t0, n) in TILES if not (t0 == 0 and last)]

        def ldc(i):
            b, t0, n = tiles[i]
            P.dma(xt2[i % 2][:, :, :n], fm_view(self.xs[b], 0, 8, t0, n), reads=[self.xs[b]], writes=[xt2[i % 2]])
            P.dma(ot2[i % 2][:, :, :n], fm_view(self.o[b], 0, 8, t0, n), reads=[self.o[b]], writes=[ot2[i % 2]])

        def outproj(i):
            b, t0, n = tiles[i]
            j = 4 if t0 == 0 else b
            xt, ot = xt2[i % 2], ot2[i % 2]
            for m in range(8):
                ps = self.nps()
                for k in range(8):
                    P.mm(ps[:, :n], wo[:, k, m * 128:(m + 1) * 128], ot[:, k, :n], k == 0, k == 7, [wo, ot], [ps])
                P.stt("dve", xt[:, m, :n], ps[:, :n], self.mod[:, 16 + m, j:j + 1], xt[:, m, :n], ALU.mult, ALU.add,
                      [ps, self.mod, (xt, m)], [(xt, m)])
            P.dma(fm_view(self.xs[b], 0, 8, t0, n), xt[:, :, :n], reads=[xt], writes=[self.xs[b]])

        def norm1(i):
            b, t0, n = tiles[i]
            self.norm_p1(xt2[i % 2], n, sq, rstd, tmp)

        def norm2(i):
            b, t0, n = tiles[i]
            j = 4 if t0 == 0 else b
            hb = hb2[i % 2]
            self.norm_p2(n, self.G2, 24, j, hb, tmp, h32)
            P.dma(fm_view(self.h2[b], 0, 8, t0, n), hb[:, :, :n], reads=[hb], writes=[self.h2[b]])
            if moe:
                ns = n // 128
                ps = self.nps()
                for s_ in range(ns):
                    for k in range(8):
                        P.mm(ps[:, s_ * 8:(s_ + 1) * 8], h32[:, k, s_ * 128:(s_ + 1) * 128], wr[:, 40 + k * 8:40 + (k + 1) * 8],
                             k == 0, k == 7, [h32, wr], [ps])
                bc8 = lambda t: t[:, 0:ns].unsqueeze(2).broadcast_to([128, ns, 8])
                L3 = lambda t: t[:, 0:ns * 8].rearrange("p (s e) -> p s e", e=8)
                P.copy("dve", lg[:, 0:ns * 8], ps[:, 0:ns * 8], [ps], [lg])
                P.op("dve", lambda e: e.reduce_max(out=m1[:, 0:ns], in_=L3(lg), axis=AX.X), [lg], [m1])
                P.tt("dve", L3(eq1), L3(lg), bc8(m1), ALU.is_equal, [lg, m1], [eq1])
                P.stt("dve", lg2[:, 0:ns * 8], eq1[:, 0:ns * 8], -1e30, lg[:, 0:ns * 8], ALU.mult, ALU.add, [eq1, lg], [lg2])
                P.op("dve", lambda e: e.reduce_max(out=m2[:, 0:ns], in_=L3(lg2), axis=AX.X), [lg2], [m2])
                P.tt("dve", L3(eq2), L3(lg2), bc8(m2), ALU.is_equal, [lg2, m2], [eq2])
                P.tt("dve", dd[:, 0:ns], m2[:, 0:ns], m1[:, 0:ns], ALU.subtract, [m1, m2], [dd])
                P.act(ee[:, 0:ns], dd[:, 0:ns], AF.Exp, [dd], [ee])
                P.ts("dve", g1[:, 0:ns], ee[:, 0:ns], 1.0, None, ALU.add, None, [ee], [g1])
                P.op("dve", lambda e: e.reciprocal(out=g1[:, 0:ns], in_=g1[:, 0:ns]), [g1], [g1])
                P.tt("dve", g2[:, 0:ns], ee[:, 0:ns], g1[:, 0:ns], ALU.mult, [ee, g1], [g2])
                P.tt("dve", L3(gt), L3(eq1), bc8(g1), ALU.mult, [eq1, g1], [gt])
                P.tt("dve", L3(eq2), L3(eq2), bc8(g2), ALU.mult, [eq2, g2], [eq2])
                P.tt("dve", gt[:, 0:ns * 8], gt[:, 0:ns * 8], eq2[:, 0:ns * 8], ALU.add, [gt, eq2], [gt])
                pst = self.nps()
                for s_ in range(ns):
                    P.tr(pst[0:8, s_ * 128:(s_ + 1) * 128], gt[:, s_ * 8:(s_ + 1) * 8], cs[:, C_ID:C_ID + 128], [gt, cs], [pst])
                P.copy("act", gts[:, 0:n], pst[0:8, 0:n], [pst], [gts])
                P.dma(self.gT[b].ap()[:, t0 - 256:t0 - 256 + n], gts[:, :n], reads=[gts], writes=[self.gT[b]])

        NT = len(tiles)
        ldc(0)
        outproj(0)
        if NT > 1:
            ldc(1)
        for i in range(NT):
            norm1(i)
            if i + 1 < NT:
                outproj(i + 1)
            norm2(i)
            if i + 2 < NT:
                ldc(i + 2)
        P.phase_end()

    def final_out(self, b, t0, n, xt, yf, sq, rstd, otm):
        P = self.P
        cs = self.cst_s
        g = self.gsm_s
        P.act(sq[:, :, :n], xt[:, :, :n], AF.Square, [xt], [sq])
        ps = self.nps()
        for k in range(8):
            P.mm(ps[:, :n], self.one_b[:], sq[:, k, :n], k == 0, k == 7, [self.one_b, sq], [ps])
        P.act(rstd[:, :n], ps[:, :n], AF.Sqrt, [ps], [rstd], scale=1.0 / D, bias=EPS)
        P.op("dve", lambda e: e.reciprocal(out=rstd[:, :n], in_=rstd[:, :n]), [rstd], [rstd])
        for k in range(8):
            P.stt("dve", yf[:, k, :n], xt[:, k, :n], g[:, 32 + k:33 + k], rstd[:, :n], ALU.mult, ALU.mult, [(xt, k), g, rstd], [(yf, k)])
        for s in range(n // 128):
            ot = otm[s % 2]
            for hf in range(2):
                ps = self.nps()
                for kk in range(4):
                    k = hf * 4 + kk
                    P.tr(ps[:, kk * 128:(kk + 1) * 128], yf[:, k, s * 128:(s + 1) * 128], cs[:, C_ID:C_ID + 128], [(yf, k), cs], [ps])
                P.copy("act" if hf == 0 else "dve", ot[:, hf * 512:(hf + 1) * 512], ps[:, :], [ps], [ot])
            tt0 = t0 - 256 + s * 128
            d = P.dma(self.out[b, tt0:tt0 + 128, :], ot[:], reads=[ot], writes=[self.out])
            self.out_deps.append(d)

    def c2_dense(self, l):
        P = self.P
        last = (l == NL - 1)
        P.phase_begin()
        wg = P.psb("wg", [128, 8, 5632], BF16)
        wd = P.psb("wd", [128, 22, D], BF16)
        P.dma(wg[:, 0:4, :], self.ffn_gu_b.ap()[0:512, :].rearrange("(k p) n -> p k n", p=128), reads=[self.ffn_gu_b], writes=[wg])
        P.dma(wg[:, 4:8, :], self.ffn_gu_b.ap()[512:1024, :].rearrange("(k p) n -> p k n", p=128), reads=[self.ffn_gu_b], writes=[wg])
        P.dma(wd[:], self.ffn_down_b.ap().rearrange("(m p) n -> p m n", p=128), reads=[self.ffn_down_b], writes=[wd])
        NT = 256
        xt2 = [P.psb("xt", [128, 8, NT], F32) for _ in range(2)]
        hb2 = [P.psb("hb", [128, 8, NT], BF16) for _ in range(2)]
        a = P.psb("a", [128, 22, NT], BF16)
        sg = [P.psb("sg", [128, NT], F32) for _ in range(2)]
        bgf = [P.psb("bgf", [128, 1024], F32) for _ in range(2)]
        bgb = [P.psb("bgb", [128, 1024], BF16) for _ in range(2)]
        it = 0
        for b in range(NB):
            for t0 in range(0, TOK, NT):
                if t0 == 0 and last:
                    continue
                n = NT
                if self.nl > 1:
                    self.bg_step(9, bgf, bgb)
                j = 4 if t0 == 0 else b
                xt, hb = xt2[it % 2], hb2[it % 2]
                it += 1
                P.dma(xt[:], fm_view(self.xs[b], 0, 8, t0, n), reads=[self.xs[b]], writes=[xt])
                P.dma(hb[:], fm_view(self.h2[b], 0, 8, t0, n), reads=[self.h2[b]], writes=[hb])
                for m in range(22):
                    pg, pu = self.nps(), self.nps()
                    for k in range(8):
                        P.mm(pg[:, :n], wg[:, k, m * 128:(m + 1) * 128], hb[:, k, :], k == 0, k == 7, [wg, hb], [pg])
                    for k in range(8):
                        P.mm(pu[:, :n], wg[:, k, 2816 + m * 128:2816 + (m + 1) * 128], hb[:, k, :], k == 0, k == 7, [wg, hb], [pu])
                    s_ = sg[m % 2]
                    P.act(s_[:], pg[:, :n], AF.Silu, [pg], [s_])
                    P.tt("dve", a[:, m, :], pu[:, :n], s_[:], ALU.mult, [pu, s_], [(a, m)])
                for f in range(8):
                    ps = self.nps()
                    for m in range(22):
                        P.mm(ps[:, :n], wd[:, m, f * 128:(f + 1) * 128], a[:, m, :], m == 0, m == 21, [wd, a], [ps])
                    P.stt("dve", xt[:, f, :], ps[:, :n], self.mod[:, 40 + f, j:j + 1], xt[:, f, :], ALU.mult, ALU.add,
                          [ps, self.mod, (xt, f)], [(xt, f)])
                P.dma(fm_view(self.xs[b], 0, 8, t0, n), xt[:], reads=[xt], writes=[self.xs[b]])
        if self.nl > 1:
            self.bg_step(10 ** 6, bgf, bgb)
        P.phase_end()

    def c2_moe(self, l):
        P = self.P
        cs = self.cst_s
        last = (l == NL - 1)
        P.phase_begin()
        wgu = [P.psb("wgu", [128, 8, 2, 768], BF16) for _ in range(2)]
        wdn = [P.psb("wdn", [128, 6, D], BF16) for _ in range(2)]
        hb2 = [P.psb("hb", [128, 8, 512], BF16) for _ in range(2)]
        xt = P.psb("xt", [128, 8, 512], F32)
        acc = P.psb("acc", [128, 8, 512], F32)
        gbc = P.psb("gbc", [128, 8, 512], F32)
        gts = P.psb("gts", [8, 512], F32)
        a2 = [P.psb("a", [128, 6, 512], BF16) for _ in range(2)]
        sg = [P.psb("sg", [128, 512], F32) for _ in range(2)]
        sg2 = [P.psb("sg2", [128, 512], F32) for _ in range(2)]
        if last:
            sq = P.psb("sq", [128, 8, 512], BF16)
            rstd = P.psb("rstd", [128, 512], F32)
            otm = [P.psb("otm", [128, D], F32) for _ in range(2)]
        tiles = [(b, t0) for b in range(NB) for t0 in range(256, TOK, 512)]
        units = [(ti, u) for ti in range(len(tiles)) for u in range(16)]

        def load_unit(ui):
            ti, u = units[ui]
            e, half = u // 2, u % 2
            nm = 6 if half == 0 else 5
            wgt, wdt = wgu[ui % 2], wdn[ui % 2]
            for gu in range(2):
                c0 = gu * 1408 + half * 768
                P.dma(wgt[:, :, gu, 0:nm * 128], self.moe_gu_b[e].ap()[:, c0:c0 + nm * 128].rearrange("(k p) n -> p k n", p=128),
                      reads=[self.moe_gu_b[e]], writes=[wgt])
            P.dma(wdt[:, 0:nm, :], self.moe_down_b[e].ap()[half * 768:half * 768 + nm * 128, :].rearrange("(m p) n -> p m n", p=128),
                  reads=[self.moe_down_b[e]], writes=[wdt])

        def load_tile(ti):
            b, t0 = tiles[ti]
            P.dma(hb2[ti % 2][:], fm_view(self.h2[b], 0, 8, t0, 512), reads=[self.h2[b]], writes=[hb2[ti % 2]])

        load_unit(0)
        load_tile(0)
        ui = 0
        for ti, (b, t0) in enumerate(tiles):
            n = 512
            hb = hb2[ti % 2]
            if ti + 1 < len(tiles):
                load_tile(ti + 1)
            P.dma(xt[:], fm_view(self.xs[b], 0, 8, t0, n), reads=[self.xs[b]], writes=[xt])
            P.dma(gts[:], self.gT[b].ap()[:, t0 - 256:t0 - 256 + n], reads=[self.gT[b]], writes=[gts])
            for e in range(8):
                ps = self.nps()
                P.mm(ps[:, :n], cs[0:8, C_SEL + e * 128:C_SEL + (e + 1) * 128], gts[:, :n], True, True, [cs, gts], [ps])
                P.copy("act", gbc[:, e, :], ps[:, :n], [ps], [(gbc, e)])
            for u in range(16):
                if ui + 1 < len(units):
                    load_unit(ui + 1)
                e, half = u // 2, u % 2
                nm = 6 if half == 0 else 5
                wgt, wdt = wgu[ui % 2], wdn[ui % 2]
                a = a2[ui % 2]
                ui += 1
                for mi in range(nm):
                    pg, pu = self.nps(), self.nps()
                    for k in range(8):
                        P.mm(pg[:, :n], wgt[:, k, 0, mi * 128:(mi + 1) * 128], hb[:, k, :], k == 0, k == 7, [wgt, hb], [pg])
                    for k in range(8):
                        P.mm(pu[:, :n], wgt[:, k, 1, mi * 128:(mi + 1) * 128], hb[:, k, :], k == 0, k == 7, [wgt, hb], [pu])
                    s_, s2_ = sg[mi % 2], sg2[mi % 2]
                    P.act(s_[:], pg[:, :n], AF.Silu, [pg], [s_])
                    P.tt("pool", s2_[:], s_[:], gbc[:, e, :], ALU.mult, [s_, (gbc, e)], [s2_])
                    P.tt("dve", a[:, mi, :], pu[:, :n], s2_[:], ALU.mult, [pu, s2_], [(a, mi)])
                for f in range(8):
                    ps = self.nps()
                    for mi in range(nm):
                        P.mm(ps[:, :n], wdt[:, mi, f * 128:(f + 1) * 128], a[:, mi, :], mi == 0, mi == nm - 1, [wdt, a], [ps])
                    if u == 0:
                        P.copy("act", acc[:, f, :], ps[:, :n], [ps], [(acc, f)])
                    else:
                        P.tt("dve", acc[:, f, :], acc[:, f, :], ps[:, :n], ALU.add, [ps, (acc, f)], [(acc, f)])
            for f in range(8):
                P.stt("dve", xt[:, f, :], acc[:, f, :], self.mod[:, 40 + f, b:b + 1], xt[:, f, :], ALU.mult, ALU.add,
                      [(acc, f), self.mod, (xt, f)], [(xt, f)])
            if last:
                self.final_out(b, t0, n, xt, acc, sq, rstd, otm)
            else:
                P.dma(fm_view(self.xs[b], 0, 8, t0, n), xt[:], reads=[xt], writes=[self.xs[b]])
        P.phase_end()

    def build(self):
        P = self.P
        self.out_deps = []
        self.setup()
        self.convert([0])
        self.bg_jobs_init()
        for l in range(self.nl):
            ctx_out = l < NL - 1
            self.adaln(l)
            if self.stop_after == "adaln":
                break
            self.phaseA(l)
            if self.stop_after == "A":
                break
            for b in range(NB):
                self.m1(l, b)
                self.m23(l, b, ctx_out)
            if self.stop_after == "M":
                break
            self.c1(l)
            if self.stop_after == "C1":
                break
            if l % 2 == 0:
                self.c2_dense(l)
            else:
                self.c2_moe(l)
        P.barrier()
        return P.nc


def _consts():
    c = np.zeros((128, NCST), np.float32)
    i = np.arange(128)
    c[:, C_ID:C_ID + 128] = np.eye(128)
    c[:, C_TRI:C_TRI + 128] = (i[:, None] <= i[None, :])
    c[:, C_TRU:C_TRU + 128] = (i[:, None] >= i[None, :])
    c[:, C_SG:C_SG + 128] = (i[:, None] > i[None, :])
    c[:, C_SL:C_SL + 128] = (i[:, None] < i[None, :])
    c[:, C_ONE:C_ONE + 128] = 1.0
    for e in range(8):
        c[e, C_SEL + e * 128:C_SEL + (e + 1) * 128] = 1.0
    invc = np.ones((128, 2, WP), np.float32)
    wins = (2, 4, 8, 16)
    for g, win in enumerate(wins):
        for (L, off) in ((CTXL, 16), (SEQ, 304)):
            t = np.arange(L)
            lo = np.clip(t - win // 2, 0, L)
            hi = np.clip(t - win // 2 + win, 0, L)
            p0 = (g % 2) * 64
            invc[p0:p0 + 64, g // 2, off:off + L] = (1.0 / (hi - lo).astype(np.float32))[None, :]
    return c, invc.reshape(128, 2 * WP)


def _prep_shared(inp):
    f = lambda a: np.ascontiguousarray(np.asarray(a, dtype=np.float32))
    small = np.zeros((NL, 128, NSM), np.float32)
    ttab = np.zeros((NL, 128, 4, 15, 64), np.float32)
    cq = np.arange(64)
    sc = np.clip(cq - 8, 0, 48)
    kc = np.arange(64)
    valid = (kc[None, :] >= sc[:, None]) & (kc[None, :] < sc[:, None] + 16)
    idx = np.clip(kc[None, :] - cq[:, None] + 15, 0, 30)
    for l in range(NL):
        small[l, :, O_BADA:O_BADA + 48] = f(inp["b_ada"])[l].reshape(48, 128).T
        small[l, :, O_PSC:O_PSC + 2] = f(inp["pool_scale"])[l].reshape(2, 128).T
        cw = f(inp["conv_w"])[l]
        small[l, :, O_CW:O_CW + 6] = cw.reshape(3, 2, 128).transpose(2, 1, 0).reshape(128, 6)
        scw = f(inp["ssd_conv_w"])[l]
        small[l, :, O_SCW:O_SCW + 18] = scw.reshape(3, 6, 128).transpose(2, 1, 0).reshape(128, 18)
        small[l, :, O_SCB:O_SCB + 6] = f(inp["ssd_conv_b"])[l].reshape(6, 128).T
        small[l, :, O_DTB:O_DTB + 8] = f(inp["ssd_dt_bias"])[l].reshape(1, 8)
        small[l, :, O_ALOG:O_ALOG + 8] = f(inp["ssd_a_log"])[l].reshape(1, 8)
        small[l, :, O_DSK:O_DSK + 256] = np.repeat(f(inp["ssd_d"])[l], 64)[None, :]
        small[l, :, O_SNG:O_SNG + 256] = f(inp["ssd_norm_g"])[l][None, :]
        pw = f(inp["pool_w"])[l]
        blk = np.zeros((128, 2, 128), np.float32)
        for g in range(4):
            p0 = (g % 2) * 64
            blk[p0:p0 + 64, g // 2, p0:p0 + 64] = pw[g]
        small[l, :, O_PW:O_PW + 256] = blk.reshape(128, 256)
        rpb = f(inp["na_rpb"])[l]
        gath = rpb[:, :, idx]
        tab = np.where(valid[None, None], gath, np.float32(-30000.0))
        tab = tab.transpose(2, 0, 1, 3)
        ttab[l, 0:64] = tab
        ttab[l, 64:128] = tab
    gsm = np.zeros((128, 104), np.float32)
    gsm[:, 0:8] = f(inp["g_mix"])[0].reshape(8, 128).T
    gsm[:, 8:16] = f(inp["g_mix"])[1].reshape(8, 128).T
    gsm[:, 16:24] = f(inp["g_ffn"])[0].reshape(8, 128).T
    gsm[:, 24:32] = f(inp["g_ffn"])[1].reshape(8, 128).T
    gsm[:, 32:40] = f(inp["g_final"]).reshape(8, 128).T
    gsm[:, 40:104] = f(inp["moe_router"])[0].reshape(8, 128, 8).transpose(1, 0, 2).reshape(128, 64)
    cst, invc = _consts()
    return {
        "w_ada": f(inp["w_ada"]), "w_in": f(inp["w_in"]), "w_out": f(inp["w_out"]), "small": small,
        "ttab": ttab.reshape(NL, 128, 3840), "gsm": gsm, "cst": cst, "invc": invc,
        "ffn_gu": f(inp["ffn_w_gu"])[0], "ffn_down": f(inp["ffn_w_down"])[0],
        "moe_gu": f(inp["moe_w_gu"])[0], "moe_down": f(inp["moe_w_down"])[0],
    }


def _core_inputs(inp, shared, core):
    b0 = core * NB
    x = np.asarray(inp["x"], dtype=np.float32)
    c = np.asarray(inp["c"], dtype=np.float32)
    ctx = np.asarray(inp["ctx"], dtype=np.float32)
    cc = np.concatenate([c[b0:b0 + NB], np.asarray(inp["c_ctx"], dtype=np.float32)[None, :]], axis=0).T
    m = dict(shared)
    m["x"] = np.ascontiguousarray(x[b0:b0 + NB])
    m["ctx"] = np.ascontiguousarray(ctx[b0:b0 + NB])
    m["cc"] = np.ascontiguousarray(cc)
    return m


_NC_CACHE = {}


def kernel(**inputs):
    if "nc" not in _NC_CACHE:
        mdl = Model()
        _NC_CACHE["nc"] = mdl.build()
    nc = _NC_CACHE["nc"]
    shared = _prep_shared(inputs)
    in_maps = [_core_inputs(inputs, shared, core) for core in range(8)]
    res = run_bass_kernel_spmd(nc, in_maps, core_ids=list(range(8)))
    out = np.concatenate([np.asarray(r["out"], dtype=np.float32) for r in res.results], axis=0)
    return out
```

```python
import numpy as np
import concourse.bass as bass
import concourse.mybir as mybir

F32 = mybir.dt.float32
BF16 = mybir.dt.bfloat16
ALU = mybir.AluOpType
AF = mybir.ActivationFunctionType
AX = mybir.AxisListType


class T:
    def __init__(self, name, handle, space):
        self.name = name
        self.h = handle
        self.space = space
        self.state = {}

    def ap(self):
        return self.h.ap() if self.space == "dram" else self.h[:]

    def __getitem__(self, idx):
        return self.ap()[idx]


class Prog:
    N_DMA_SEMS = 48

    def __init__(self):
        self.nc = bass.Bass("TRN2", target_bir_lowering=False)
        nc = self.nc
        self.eng = {"pe": nc.tensor, "act": nc.scalar, "dve": nc.vector, "pool": nc.gpsimd, "sp": nc.sync}
        self.sem = {}
        self.cnt = {}
        self._cms = []
        for e in self.eng:
            cm = nc.semaphore("s_" + e)
            self.sem[e] = cm.__enter__()
            self._cms.append(cm)
            self.cnt[e] = 0
        self.dma_sems = []
        for i in range(self.N_DMA_SEMS):
            cm = nc.semaphore("d%d" % i)
            self.dma_sems.append(cm.__enter__())
            self._cms.append(cm)
        self.dma_cnt = [0] * self.N_DMA_SEMS
        self.dma_last = [None] * self.N_DMA_SEMS
        self.dma_i = 0
        self.seen = {e: {} for e in self.eng}
        self.n_ops = 0
        self.n_waits = 0
        self.out_deps = []

    def sbuf(self, name, shape, dtype):
        return T(name, self.nc.alloc_sbuf_tensor(name, list(shape), dtype), "sbuf")

    def psum(self, name, shape, dtype=F32):
        return T(name, self.nc.alloc_psum_tensor(name, list(shape), dtype), "psum")

    def dram(self, name, shape, dtype, kind="Internal"):
        return T(name, self.nc.dram_tensor(name, list(shape), dtype, kind=kind), "dram")

    @staticmethod
    def _norm(acc):
        out = []
        for a in acc:
            if isinstance(a, T):
                out.append((a, None))
            else:
                out.append((a[0], a[1]))
        return out

    def _collect(self, reads, writes):
        deps = []
        for t, k in self._norm(reads):
            keys = list(t.state.keys()) if k is None else [kk for kk in (k, None) if kk in t.state]
            for kk in keys:
                w = t.state[kk][0]
                if w is not None:
                    deps.append(w)
        for t, k in self._norm(writes):
            keys = list(t.state.keys()) if k is None else [kk for kk in (k, None) if kk in t.state]
            for kk in keys:
                w, rs = t.state[kk]
                if w is not None:
                    deps.append(w)
                deps.extend(rs.items())
        return deps

    def _update(self, reads, writes, me):
        for t, k in self._norm(reads):
            if k is None:
                if None not in t.state:
                    t.state[None] = [None, {}]
                for kk in t.state:
                    self._addr(t.state[kk][1], me)
            else:
                if k not in t.state:
                    w = t.state[None][0] if None in t.state else None
                    t.state[k] = [w, {}]
                self._addr(t.state[k][1], me)
        for t, k in self._norm(writes):
            if k is None:
                t.state = {None: [me, {}]}
            else:
                t.state[k] = [me, {}]
                if None in t.state:
                    self._addr(t.state[None][1], me)

    @staticmethod
    def _addr(d, me):
        if d.get(me[0], 0) < me[1]:
            d[me[0]] = me[1]

    def _emit_waits(self, e, deps):
        eng = self.eng[e]
        need = {}
        for d in deps:
            if d is None:
                continue
            key, val = d
            if need.get(key, 0) < val:
                need[key] = val
        for key, val in need.items():
            if self.seen[e].get(key, 0) >= val:
                continue
            if key == e and e == "pe":
                continue
            sem = self.sem[key] if isinstance(key, str) else self.dma_sems[key]
            eng.wait_ge(sem, val)
            self.n_waits += 1
            self.seen[e][key] = val

    @staticmethod
    def _excl(reads, writes):
        r2, w2 = [], []
        for a in reads:
            t = a if isinstance(a, T) else a[0]
            if t.space == "psum":
                w2.append(t)
            else:
                r2.append(a)
        for a in writes:
            t = a if isinstance(a, T) else a[0]
            w2.append(t if t.space == "psum" else a)
        return r2, w2

    def op(self, e, fn, reads=(), writes=()):
        reads, writes = self._excl(reads, writes)
        deps = self._collect(reads, writes)
        self._emit_waits(e, deps)
        ins = fn(self.eng[e])
        self.cnt[e] += 1
        ins.then_inc(self.sem[e], 1)
        me = (e, self.cnt[e])
        self._update(reads, writes, me)
        self.n_ops += 1
        return me

    def dma(self, out_ap, in_ap, reads=(), writes=(), q="sp", **kw):
        deps = self._collect(reads, writes)
        i = self.dma_i % self.N_DMA_SEMS
        self.dma_i += 1
        if self.dma_last[i] is not None:
            deps.append(self.dma_last[i])
        self._emit_waits(q, deps)
        ins = self.eng[q].dma_start(out=out_ap, in_=in_ap, **kw)
        self.dma_cnt[i] += 1
        ins.then_inc(self.dma_sems[i], 16)
        me = (i, 16 * self.dma_cnt[i])
        self.dma_last[i] = me
        self._update(reads, writes, me)
        self.n_ops += 1
        return me

    def finish(self, final_deps):
        self._emit_waits("sp", list(final_deps))
        return self.nc


def _mm(self, out, lhsT, rhs, start, stop, R, W):
    return self.op("pe", lambda e: e.matmul(out, lhsT=lhsT, rhs=rhs, start=start, stop=stop), R, W)


def _tr(self, out, in_, ident, R, W):
    return self.op("pe", lambda e: e.transpose(out=out, in_=in_, identity=ident), R, W)


def _act(self, out, in_, func, R, W, **kw):
    return self.op("act", lambda e: e.activation(out=out, in_=in_, func=func, **kw), R, W)


def _tt(self, eng, out, in0, in1, op, R, W):
    return self.op(eng, lambda e: e.tensor_tensor(out=out, in0=in0, in1=in1, op=op), R, W)


def _ts(self, eng, out, in0, s1, s2, op0, op1, R, W):
    if op1 is None:
        return self.op(eng, lambda e: e.tensor_scalar(out=out, in0=in0, scalar1=s1, scalar2=None, op0=op0), R, W)
    return self.op(eng, lambda e: e.tensor_scalar(out=out, in0=in0, scalar1=s1, scalar2=s2, op0=op0, op1=op1), R, W)


def _stt(self, eng, out, in0, scalar, in1, op0, op1, R, W):
    return self.op(eng, lambda e: e.scalar_tensor_tensor(out=out, in0=in0, scalar=scalar, in1=in1, op0=op0, op1=op1), R, W)


def _copy(self, eng, out, in_, R, W):
    if eng == "act":
        return self.op("act", lambda e: e.copy(out=out, in_=in_), R, W)
    return self.op(eng, lambda e: e.tensor_copy(out=out, in_=in_), R, W)


def _memset(self, eng, t_ap, val, W):
    return self.op(eng, lambda e: e.memset(t_ap, val), (), W)


def _phase_begin(self):
    import contextlib
    if not hasattr(self, "_stacks"):
        self._stacks = []
    self._stacks.append(contextlib.ExitStack())
    self._stack = self._stacks[-1]


def _psb(self, name, shape, dtype):
    self._uid = getattr(self, "_uid", 0) + 1
    name = "%s_%d" % (name, self._uid)
    h = self._stack.enter_context(self.nc.sbuf_tensor(name, list(shape), dtype))
    t = T(name, h, "sbuf")
    return t


def _barrier(self):
    deps = [(e, c) for e, c in self.cnt.items() if c > 0]
    deps += [d for d in self.dma_last if d is not None]
    for e in self.eng:
        self._emit_waits(e, [d for d in deps if d[0] != e])


def _phase_end(self):
    self.barrier()
    self._stacks.pop().close()
    self._stack = self._stacks[-1] if self._stacks else None


Prog.mm = _mm
Prog.tr = _tr
Prog.act = _act
Prog.tt = _tt
Prog.ts = _ts
Prog.stt = _stt
Prog.copy = _copy
Prog.memset = _memset
Prog.phase_begin = _phase_begin
Prog.psb = _psb
Prog.barrier = _barrier
Prog.phase_end = _phase_end

from concourse.bass_utils import run_bass_kernel_spmd

D = 1024
SEQ = 2048
CTXL = 256
TOK = 2304
NB = 4
NL = 2
DIN = 2824
WP = 2368
NCH = 18
TILES = [(0, 256)] + [(256 + i * 512, 512) for i in range(4)]
EPS = 1e-6
O_BADA, O_PSC, O_CW, O_SCW, O_SCB, O_DTB, O_ALOG, O_DSK, O_SNG, O_PW, NSM = 0, 48, 50, 56, 74, 80, 88, 96, 352, 608, 864
C_ID, C_TRI, C_TRU, C_SG, C_SL, C_ONE, C_SEL, NCST = 0, 128, 256, 384, 512, 640, 768, 1792


def pcol(t):
    return 16 + t if t < 256 else t + 48


def ccol(c):
    return pcol(c * 128)


def fm_view(dr, r0, nch, t0, n):
    return dr.ap()[r0:r0 + nch * 128, t0:t0 + n].rearrange("(m p) t -> p m t", p=128)


class Model:
    def __init__(self, dbg=False, nl=NL, stop_after=None):
        self.P = Prog()
        self.dbg = dbg
        self.nl = nl
        self.stop_after = stop_after
        P = self.P
        k_in = "ExternalInput"
        self.x_in = P.dram("x", [NB, SEQ, D], F32, k_in)
        self.ctx_in = P.dram("ctx", [NB, CTXL, D], F32, k_in)
        self.cc = P.dram("cc", [D, 5], F32, k_in)
        self.w_ada = P.dram("w_ada", [NL, D, 6 * D], F32, k_in)
        self.w_in = P.dram("w_in", [NL, D, DIN], F32, k_in)
        self.w_out = P.dram("w_out", [NL, D, D], F32, k_in)
        self.small = P.dram("small", [NL, 128, NSM], F32, k_in)
        self.ttab = P.dram("ttab", [NL, 128, 4 * 960], F32, k_in)
        self.gsm = P.dram("gsm", [128, 104], F32, k_in)
        self.cst = P.dram("cst", [128, NCST], F32, k_in)
        self.invc = P.dram("invc", [128, 2 * WP], F32, k_in)
        self.ffn_gu = P.dram("ffn_gu", [D, 5632], F32, k_in)
        self.ffn_down = P.dram("ffn_down", [2816, D], F32, k_in)
        self.moe_gu = P.dram("moe_gu", [8, D, 2816], F32, k_in)
        self.moe_down = P.dram("moe_down", [8, 1408, D], F32, k_in)
        self.out = P.dram("out", [NB, SEQ, D], F32, "ExternalOutput")
        dk = "ExternalOutput" if dbg else "Internal"
        self.w_in_b = [P.dram("w_in_b%d" % l, [D, DIN], BF16) for l in range(NL)]
        self.w_out_b = [P.dram("w_out_b%d" % l, [D, D], BF16) for l in range(NL)]
        self.ffn_gu_b = P.dram("ffn_gu_b", [D, 5632], BF16)
        self.ffn_down_b = P.dram("ffn_down_b", [2816, D], BF16)
        self.moe_gu_b = [P.dram("moe_gu_b%d" % e, [D, 2816], BF16) for e in range(8)]
        self.moe_down_b = [P.dram("moe_down_b%d" % e, [1408, D], BF16) for e in range(8)]
        self.xs = [P.dram("xs%d" % b, [D, TOK], F32, dk if b == 0 else "Internal") for b in range(NB)]
        self.pr = [P.dram("pr%d" % b, [2816, TOK], BF16, dk if b == 0 else "Internal") for b in range(NB)]
        self.vtm = [P.dram("vtm%d" % b, [TOK, 256], BF16, dk if b == 0 else "Internal") for b in range(NB)]
        self.dtm = [P.dram("dtm%d" % b, [8, TOK], F32, dk if b == 0 else "Internal") for b in range(NB)]
        self.o = [P.dram("o%d" % b, [D, TOK], BF16, dk if b == 0 else "Internal") for b in range(NB)]
        self.h2 = [P.dram("h2_%d" % b, [D, TOK], BF16, dk if b == 0 else "Internal") for b in range(NB)]
        self.gT = [P.dram("gT%d" % b, [8, SEQ], F32, dk if b == 0 else "Internal") for b in range(NB)]
        self.ps = [P.psum("ps%d" % i, [128, 512], F32) for i in range(6)]
        self.pb = [P.psum("pb%d" % i, [128, 1024], BF16) for i in range(2)]
        self.psi = 0
        self.cst_s = P.sbuf("cst_s", [128, NCST], F32)
        self.id_b = P.sbuf("id_b", [128, 128], BF16)
        self.one_b = P.sbuf("one_b", [128, 128], BF16)
        self.cst_b = P.sbuf("cst_b", [128, 640], BF16)
        self.sm_s = [P.sbuf("sm_s%d" % l, [128, NSM], F32) for l in range(NL)]
        self.gsm_s = P.sbuf("gsm_s", [128, 104], F32)
        self.s_t = P.sbuf("s_t", [128, 8, 5], F32)
        self.mod = P.sbuf("mod", [128, 48, 5], F32)
        self.G1 = P.sbuf("G1", [128, 8, 5], F32)
        self.G2 = P.sbuf("G2", [128, 8, 5], F32)
        self.pwb = P.sbuf("pwb", [128, 2, 128], BF16)

    def nps(self):
        p = self.ps[self.psi % 6]
        self.psi += 1
        return p

    def setup(self):
        P = self.P
        c = self.cst_s
        P.dma(c[:], self.cst[:], writes=[c])
        for l in range(NL):
            P.dma(self.sm_s[l][:], self.small[l], writes=[self.sm_s[l]])
        P.dma(self.gsm_s[:], self.gsm[:], writes=[self.gsm_s])
        P.copy("dve", self.id_b[:], c[:, C_ID:C_ID + 128], [c], [self.id_b])
        P.copy("dve", self.one_b[:], c[:, C_ONE:C_ONE + 128], [c], [self.one_b])
        P.copy("dve", self.cst_b[:], c[:, 0:640], [c], [self.cst_b])
        st = self.s_t
        P.dma(st[:], self.cc.ap().rearrange("(k p) j -> p k j", p=128), writes=[st])
        P.act(st[:], st[:], AF.Silu, [st], [st])

    def convert(self, layers):
        P = self.P
        P.phase_begin()
        f32b = [P.psb("cv32", [128, 4096], F32) for _ in range(3)]
        b16b = [P.psb("cv16", [128, 4096], BF16) for _ in range(3)]
        jobs = []

        def add(dst, src_ap):
            sv = src_ap.rearrange("(a p) n -> p a n", p=128)
            dv = dst.ap().rearrange("(a p) n -> p a n", p=128)
            A, N = sv.shape[1], sv.shape[2]
            if N <= 2048:
                ga = 4096 // N
                for a0 in range(0, A, ga):
                    g = min(ga, A - a0)
                    jobs.append((dst, sv[:, a0:a0 + g, :], dv[:, a0:a0 + g, :], g, N))
            else:
                for a0 in range(A):
                    for c0 in range(0, N, 4096):
                        n = min(4096, N - c0)
                        jobs.append((dst, sv[:, a0:a0 + 1, c0:c0 + n], dv[:, a0:a0 + 1, c0:c0 + n], 1, n))
        for l in layers:
            add(self.w_in_b[l], self.w_in[l])
        for i, (dst, sv, dv, g, n) in enumerate(jobs):
            fb, bb = f32b[i % 3], b16b[i % 3]
            fv = fb[:, 0:g * n].rearrange("p (a n) -> p a n", a=g)
            bv = bb[:, 0:g * n].rearrange("p (a n) -> p a n", a=g)
            P.dma(fv, sv, writes=[fb])
            P.copy("pool" if i % 2 == 0 else "act", bv, fv, [fb], [bb])
            P.dma(dv, bv, reads=[bb], writes=[dst])
        P.phase_end()

    def bg_jobs_init(self):
        jobs = []

        def add(dst, src_ap):
            sv = src_ap.rearrange("(a p) n -> p a n", p=128)
            dv = dst.ap().rearrange("(a p) n -> p a n", p=128)
            A, N = sv.shape[1], sv.shape[2]
            for a0 in range(A):
                for c0 in range(0, N, 1024):
                    n = min(1024, N - c0)
                    jobs.append((dst, sv[:, a0, c0:c0 + n], dv[:, a0, c0:c0 + n], n))
        add(self.w_out_b[0], self.w_out[0])
        add(self.ffn_gu_b, self.ffn_gu.ap())
        add(self.ffn_down_b, self.ffn_down.ap())
        self.bg_split = len(jobs)
        if self.nl > 1:
            add(self.w_in_b[1], self.w_in[1])
            add(self.w_out_b[1], self.w_out[1])
            for e in range(8):
                add(self.moe_gu_b[e], self.moe_gu[e])
                add(self.moe_down_b[e], self.moe_down[e])
        self.bg_jobs = jobs
        self.bg_i = 0

    def bg_step(self, k, f32b, b16b, limit=None):
        P = self.P
        limit = len(self.bg_jobs) if limit is None else limit
        for _ in range(k):
            if self.bg_i >= limit:
                return
            i = self.bg_i
            self.bg_i += 1
            dst, sv, dv, n = self.bg_jobs[i]
            fb, bb = f32b[i % len(f32b)], b16b[i % len(b16b)]
            P.dma(fb[:, 0:n], sv, writes=[fb], q="pool")
            P.copy("pool", bb[:, 0:n], fb[:, 0:n], [fb], [bb])
            P.dma(dv, bb[:, 0:n], reads=[bb], writes=[dst], q="pool")

    def adaln(self, l):
        P = self.P
        P.phase_begin()
        wa = [P.psb("wa", [128, 8, 1024], F32) for _ in range(2)]
        sm = self.sm_s[l]
        mod = self.mod

        def ld(part):
            P.dma(wa[part % 2][:], self.w_ada[l][:, part * 1024:(part + 1) * 1024].rearrange("(k p) n -> p k n", p=128),
                  writes=[wa[part % 2]])
        ld(0)
        for part in range(6):
            if part + 1 < 6:
                ld(part + 1)
            w = wa[part % 2]
            for fc in range(8):
                ci = part * 8 + fc
                ps = self.nps()
                for k in range(8):
                    P.mm(ps[:, 0:5], w[:, k, fc * 128:(fc + 1) * 128], self.s_t[:, k, :], k == 0, k == 7, [w, self.s_t], [ps])
                P.ts("dve", mod[:, ci, :], ps[:, 0:5], sm[:, O_BADA + ci:O_BADA + ci + 1], None, ALU.add, None,
                     [ps, sm], [(mod, ci)])
        g = self.gsm_s
        for k in range(8):
            P.ts("dve", self.G1[:, k, :], mod[:, 8 + k, :], 1.0, g[:, l * 8 + k:l * 8 + k + 1], ALU.add, ALU.mult,
                 [mod, g], [(self.G1, k)])
            P.ts("dve", self.G2[:, k, :], mod[:, 32 + k, :], 1.0, g[:, 16 + l * 8 + k:16 + l * 8 + k + 1], ALU.add, ALU.mult,
                 [mod, g], [(self.G2, k)])
        P.copy("dve", self.pwb[:], sm[:, O_PW:O_PW + 256].rearrange("p (c n) -> p c n", c=2), [sm], [self.pwb])
        P.phase_end()

    def norm_p1(self, xt, n, sq, rstd, tmp):
        P = self.P
        P.act(sq[:, :, :n], xt[:, :, :n], AF.Square, [xt], [sq])
        ps = self.nps()
        for k in range(8):
            P.mm(ps[:, :n], self.one_b[:], sq[:, k, :n], k == 0, k == 7, [self.one_b, sq], [ps])
        P.act(rstd[:, :n], ps[:, :n], AF.Ln, [ps], [rstd], scale=1.0 / D, bias=EPS)
        P.act(rstd[:, :n], rstd[:, :n], AF.Exp, [rstd], [rstd], scale=-0.5)
        P.tt("dve", tmp[:, :, :n], xt[:, :, :n], rstd[:, :n].unsqueeze(1).broadcast_to([128, 8, n]), ALU.mult, [xt, rstd], [tmp])

    def norm_p2(self, n, G, shoff, j, h_out, tmp, h32=None):
        P = self.P
        for k in range(8):
            if h32 is not None:
                P.act(h32[:, k, :n], tmp[:, k, :n], AF.Identity, [tmp, self.mod, G], [(h32, k)], scale=G[:, k, j:j + 1],
                      bias=self.mod[:, shoff + k, j:j + 1])
            else:
                P.act(h_out[:, k, :n], tmp[:, k, :n], AF.Identity, [tmp, self.mod, G], [(h_out, k)], scale=G[:, k, j:j + 1],
                      bias=self.mod[:, shoff + k, j:j + 1])
        if h32 is not None:
            P.copy("dve", h_out[:, :, :n], h32[:, :, :n], [h32], [h_out])

    def norm_mod(self, xt, n, G, shoff, j, h_out, sq, rstd, tmp, h32=None):
        self.norm_p1(xt, n, sq, rstd, tmp)
        self.norm_p2(n, G, shoff, j, h_out, tmp, h32)

    def phaseA(self, l):
        P = self.P
        P.phase_begin()
        cs = self.cst_s
        w = P.psb("w_in_s", [128, 8, DIN], BF16)
        P.dma(w[:], self.w_in_b[l].ap().rearrange("(k p) n -> p k n", p=128), reads=[self.w_in_b[l]], writes=[w])
        xt2 = [P.psb("xt", [128, 8, 512], F32) for _ in range(2)]
        xtm = [P.psb("xtm", [128, 1024], F32) for _ in range(2)] if l == 0 else None
        sq2 = [P.psb("sq", [128, 8, 512], BF16) for _ in range(2)]
        rstd2 = [P.psb("rstd", [128, 512], F32) for _ in range(2)]
        tmp = P.psb("tmp", [128, 8, 512], F32)
        h2 = [P.psb("h", [128, 8, 512], BF16) for _ in range(2)]
        stg = [P.psb("stg", [128, 22, 512], BF16) for _ in range(1)]
        vst = P.psb("vst", [128, 4, 256], BF16)
        dst = P.psb("dst", [8, 512], F32)
        tiles = [(b, t0, n) for b in range(NB) for (t0, n) in TILES]
        if l == 0:
            bgf = [P.psb("bgf", [128, 1024], F32) for _ in range(2)]
            bgb = [P.psb("bgb", [128, 1024], BF16) for _ in range(2)]

        def load(i):
            b, t0, n = tiles[i]
            xt = xt2[i % 2]
            if l == 0:
                for s_ in range(n // 128):
                    xm = xtm[s_ % 2]
                    src = self.ctx_in[b, s_ * 128:(s_ + 1) * 128, :] if t0 == 0 else \
                        self.x_in[b, t0 - 256 + s_ * 128:t0 - 256 + (s_ + 1) * 128, :]
                    P.dma(xm[:], src, writes=[xm])
                    for hf in range(2):
                        ps = self.nps()
                        for kk in range(4):
                            k = hf * 4 + kk
                            P.tr(ps[:, kk * 128:(kk + 1) * 128], xm[:, k * 128:(k + 1) * 128], cs[:, C_ID:C_ID + 128],
                                 [xm, cs], [ps])
                        dst_ap = xt[:, hf * 4:(hf + 1) * 4, s_ * 128:(s_ + 1) * 128]
                        src_ap = ps[:, :].rearrange("p (k t) -> p k t", k=4)
                        P.copy("dve", dst_ap, src_ap, [ps], [xt])
                P.dma(fm_view(self.xs[b], 0, 8, t0, n), xt[:, :, :n], reads=[xt], writes=[self.xs[b]])
            else:
                P.dma(xt[:, :, :n], fm_view(self.xs[b], 0, 8, t0, n), reads=[self.xs[b]], writes=[xt])

        def norm(i):
            b, t0, n = tiles[i]
            j = 4 if t0 == 0 else b
            self.norm_mod(xt2[i % 2], n, self.G1, 0, j, h2[i % 2], sq2[i % 2], rstd2[i % 2], tmp)

        def proj(i):
            b, t0, n = tiles[i]
            h = h2[i % 2]
            st = stg[0]
            ns = n // 128
            if l == 0:
                self.bg_step(5, bgf, bgb, limit=self.bg_split)
            for m in range(22):
                ps = self.nps()
                for k in range(8):
                    P.mm(ps[:, :n], w[:, k, m * 128:(m + 1) * 128], h[:, k, :n], k == 0, k == 7, [w, h], [ps])
                if m % 3 == 0:
                    P.copy("act", st[:, m, :n], ps[:, :n], [ps], [(st, m)])
                else:
                    P.copy("dve", st[:, m, :n], ps[:, :n], [ps], [(st, m)])
            P.dma(fm_view(self.pr[b], 0, 22, t0, n), st[:, :, :n], reads=[st], writes=[self.pr[b]])
            for s_ in range(ns):
                ps = self.nps()
                for k in range(8):
                    P.mm(ps[:, 0:256], h[:, k, s_ * 128:(s_ + 1) * 128], w[:, k, 1536:1792], k == 0, k == 7, [w, h], [ps])
                P.copy("dve", vst[:, s_, :], ps[:, 0:256], [ps], [(vst, s_)])
            P.dma(self.vtm[b].ap()[t0:t0 + n, :].rearrange("(s p) c -> p s c", p=128), vst[:, :ns, :],
                  reads=[vst], writes=[self.vtm[b]])
            psd = self.nps()
            for k in range(8):
                P.mm(psd[0:8, :n], w[:, k, 2816:2824], h[:, k, :n], k == 0, k == 7, [w, h], [psd])
            P.copy("dve", dst[:, :n], psd[0:8, :n], [psd], [dst])
            P.dma(self.dtm[b].ap()[:, t0:t0 + n], dst[:, :n], reads=[dst], writes=[self.dtm[b]])

        NT = len(tiles)
        load(0)
        norm(0)
        load(1)
        for i in range(NT):
            if i + 1 < NT:
                norm(i + 1)
            if i + 2 < NT:
                load(i + 2)
            proj(i)
        if l == 0:
            self.bg_step(10 ** 6, bgf, bgb, limit=self.bg_split)
        P.phase_end()

    def zero_pads(self, t):
        P = self.P
        for (c0, c1) in ((0, 16), (272, 304), (2352, WP)):
            P.memset("dve", t[:, :, c0:c1], 0.0, [t])

    def load_padded(self, t, ch, dr, row0, R=None):
        P = self.P
        P.dma(t[:, ch, 16:272], dr.ap()[row0:row0 + 128, 0:256], reads=[dr], writes=[t])
        P.dma(t[:, ch, 304:2352], dr.ap()[row0:row0 + 128, 256:2304], reads=[dr], writes=[t])

    def store_padded(self, dr, row0, src_t, ch):
        P = self.P
        P.dma(dr.ap()[row0:row0 + 128, 0:256], src_t[:, ch, 16:272], reads=[src_t], writes=[dr])
        P.dma(dr.ap()[row0:row0 + 128, 256:2304], src_t[:, ch, 304:2352], reads=[src_t], writes=[dr])

    def m1(self, l, b):
        P = self.P
        sm = self.sm_s[l]
        W = WP
        P.phase_begin()
        up = P.psb("up", [128, 2, WP], BF16)
        hp = P.psb("hp", [128, 2, WP], BF16)
        bp = P.psb("bp", [128, 2, WP], BF16)
        cp = P.psb("cp", [128, 2, WP], BF16)
        invc = P.psb("invc", [128, 2, WP], F32)
        for t in (up, hp, bp, cp):
            self.zero_pads(t)
        for ch in range(2):
            self.load_padded(up, ch, self.pr[b], ch * 128)
        for ch in range(2):
            self.load_padded(cp, ch, self.pr[b], 768 + ch * 128)
            self.load_padded(hp, ch, self.pr[b], 256 + ch * 128)
            self.load_padded(bp, ch, self.pr[b], 512 + ch * 128)
        P.dma(invc[:], self.invc.ap().rearrange("p (c w) -> p c w", c=2), writes=[invc])
        z = P.psb("z", [128, 2, WP], F32)
        y = P.psb("y", [128, 2, WP], BF16)
        y2 = P.psb("y2", [128, 2, WP], BF16)
        A = P.psb("A", [128, WP], F32)
        B = P.psb("B", [128, WP], F32)
        C = P.psb("C", [128, WP], F32)
        Dd = P.psb("Dd", [128, WP], F32)
        tp = P.psb("tp", [128, WP], F32)
        mC = P.psb("mC", [128, WP], F32)
        accC = P.psb("accC", [128, WP], F32)
        for ch in range(2):
            for c0 in range(0, W, 512):
                n = min(512, W - c0)
                ps = self.nps()
                P.mm(ps[:, :n], self.pwb[:, ch, :], up[:, ch, c0:c0 + n], True, True, [self.pwb, up], [ps])
                P.copy("act", z[:, ch, c0:c0 + n], ps[:, :n], [ps], [(z, ch)])
        for ch in range(2):
            zc = z[:, ch, :]
            P.tt("dve", A[:, 4:W - 4], zc[:, 3:W - 5], zc[:, 4:W - 4], ALU.add, [(z, ch)], [A])
            if ch == 0:
                P.tt("dve", B[64:128, 6:W - 6], A[64:128, 5:W - 7], A[64:128, 7:W - 5], ALU.add, [A], [B])
                S0, S1 = A, B
            else:
                P.tt("dve", B[:, 6:W - 6], A[:, 5:W - 7], A[:, 7:W - 5], ALU.add, [A], [B])
                P.tt("dve", C[:, 8:W - 8], B[:, 6:W - 10], B[:, 10:W - 6], ALU.add, [B], [C])
                P.tt("dve", Dd[64:128, 12:W - 12], C[64:128, 8:W - 16], C[64:128, 16:W - 8], ALU.add, [C], [Dd])
                S0, S1 = C, Dd
            for (p0, p1, S) in ((0, 64, S0), (64, 128, S1)):
                P.tt("dve", tp[p0:p1, 16:W - 16], S[p0:p1, 16:W - 16], invc[p0:p1, ch, 16:W - 16], ALU.mult, [S, invc], [tp])
                P.tt("dve", tp[p0:p1, 16:W - 16], tp[p0:p1, 16:W - 16], zc[p0:p1, 16:W - 16], ALU.subtract, [tp, (z, ch)], [tp])
                P.act(y[p0:p1, ch, 16:W - 16], tp[p0:p1, 16:W - 16], AF.Identity, [tp, sm], [(y, ch)],
                      scale=sm[p0:p1, O_PSC + ch:O_PSC + ch + 1])
            self.store_padded(self.o[b], ch * 128, y, ch)
            m, acc = mC, accC
            cw = lambda tap: sm[:, O_CW + ch * 3 + tap:O_CW + ch * 3 + tap + 1]
            P.tt("dve", m[:], cp[:, ch, :], hp[:, ch, :], ALU.mult, [cp, hp], [m])
            P.act(acc[:, 1:W - 1], m[:, 0:W - 2], AF.Identity, [m, sm], [acc], scale=cw(0))
            P.stt("dve", acc[:, 1:W - 1], m[:, 1:W - 1], cw(1), acc[:, 1:W - 1], ALU.mult, ALU.add, [m, sm, acc], [acc])
            P.stt("dve", acc[:, 1:W - 1], m[:, 2:W], cw(2), acc[:, 1:W - 1], ALU.mult, ALU.add, [m, sm, acc], [acc])
            P.tt("dve", y2[:, ch, 1:W - 1], bp[:, ch, 1:W - 1], acc[:, 1:W - 1], ALU.mult, [bp, acc], [(y2, ch)])
            self.store_padded(self.o[b], 256 + ch * 128, y2, ch)
        P.phase_end()

    def m2(self, l, b, ctx_out):
        P = self.P
        sm = self.sm_s[l]
        cs = self.cst_s
        W = WP
        P.phase_begin()
        xp = P.psb("xp", [128, 6, WP], BF16)
        self.zero_pads(xp)
        for ch in range(6):
            self.load_padded(xp, ch, self.pr[b], 2048 + ch * 128)
        xc = P.psb("xc", [128, 6, WP], BF16)
        acc = [P.psb("acc", [128, WP], F32) for _ in range(2)]
        for ch in range(6):
            a = acc[ch % 2]
            cw = lambda tap: sm[:, O_SCW + ch * 3 + tap:O_SCW + ch * 3 + tap + 1]
            P.ts("dve", a[:, 1:W - 1], xp[:, ch, 0:W - 2], cw(0), None, ALU.mult, None, [xp, sm], [a])
            P.stt("dve", a[:, 1:W - 1], xp[:, ch, 1:W - 1], cw(1), a[:, 1:W - 1], ALU.mult, ALU.add, [xp, sm, a], [a])
            P.stt("dve", a[:, 1:W - 1], xp[:, ch, 2:W], cw(2), a[:, 1:W - 1], ALU.mult, ALU.add, [xp, sm, a], [a])
            P.act(xc[:, ch, 1:W - 1], a[:, 1:W - 1], AF.Silu, [a, sm], [(xc, ch)], bias=sm[:, O_SCB + ch:O_SCB + ch + 1])
        zf = P.psb("zf", [128, 2, TOK], BF16)
        P.dma(zf[:], fm_view(self.pr[b], 1792, 2, 0, TOK), reads=[self.pr[b]], writes=[zf])
        P.act(zf[:], zf[:], AF.Silu, [zf], [zf])
        XBZ = P.psb("XBZ", [128, NCH, 768], BF16)
        for c in range(NCH):
            pbk = self.pb[c % 2]
            c0 = ccol(c)
            for i in range(4):
                P.tr(pbk[:, i * 128:(i + 1) * 128], xc[:, i, c0:c0 + 128], self.id_b[:], [(xc, i), self.id_b], [pbk])
            for i in range(2):
                P.tr(pbk[:, 512 + i * 128:512 + (i + 1) * 128], zf[:, i, c * 128:(c + 1) * 128], self.id_b[:], [zf, self.id_b], [pbk])
            P.copy("dve" if c % 2 else "act", XBZ[:, c, :], pbk[:, 0:768], [pbk], [(XBZ, c)])
        def t3(name):
            return P.psb(name, [128, NCH, 8], F32)
        dt, la, acum, tot, eac, wdec, dA = [t3(n) for n in ("dt", "la", "acum", "tot", "eac", "wdec", "dA")]
        ea = P.psb("ea", [128, 8], F32)
        dtf = P.psb("dtf", [8, TOK], F32)
        P.dma(dtf[:], self.dtm[b].ap(), reads=[self.dtm[b]], writes=[dtf])
        psq = self.nps()
        for c in range(NCH):
            P.tr(psq[:, c * 8:(c + 1) * 8], dtf[0:8, c * 128:(c + 1) * 128], cs[0:8, C_ID:C_ID + 8], [dtf, cs], [psq])
        P.copy("dve", dt[:].rearrange("p c j -> p (c j)"), psq[:, 0:144], [psq], [dt])
        bc = lambda off: sm[:, off:off + 8].unsqueeze(1).broadcast_to([128, NCH, 8])
        P.tt("dve", dt[:], dt[:], bc(O_DTB), ALU.add, [dt, sm], [dt])
        P.act(dt[:], dt[:], AF.Exp, [dt], [dt])
        P.act(dt[:], dt[:], AF.Ln, [dt], [dt], bias=1.0)
        P.act(ea[:], sm[:, O_ALOG:O_ALOG + 8], AF.Exp, [sm], [ea])
        P.stt("dve", la[:], dt[:], -1.0, ea[:, 0:8].unsqueeze(1).broadcast_to([128, NCH, 8]), ALU.mult, ALU.mult, [dt, ea], [la])
        laf = la[:].rearrange("p c j -> p (c j)")
        psA, psB, psT = self.nps(), self.nps(), self.nps()
        P.mm(psA[:, 0:144], cs[:, C_TRI:C_TRI + 128], laf, True, True, [cs, la], [psA])
        P.mm(psB[:, 0:144], cs[:, C_TRU:C_TRU + 128], laf, True, True, [cs, la], [psB])
        P.mm(psT[:, 0:144], cs[:, C_ONE:C_ONE + 128], laf, True, True, [cs, la], [psT])
        v3 = lambda ps: ps[:, 0:144].rearrange("p (c j) -> p c j", j=8)
        P.copy("dve", acum[:, :, 0:4], v3(psA)[:, :, 0:4], [psA], [acum])
        P.copy("dve", acum[:, :, 4:8], v3(psB)[:, :, 4:8], [psB], [acum])
        P.copy("dve", tot[:], v3(psT), [psT], [tot])
        P.act(eac[:], acum[:], AF.Exp, [acum], [eac])
        P.act(dA[:], tot[:], AF.Exp, [tot], [dA])
        P.tt("dve", wdec[:], tot[:], acum[:], ALU.subtract, [tot, acum], [wdec])
        P.act(wdec[:], wdec[:], AF.Exp, [wdec], [wdec])
        H32 = P.psb("H32", [128, 8, 64], F32)
        Hb = P.psb("Hb", [128, 8, 64], BF16)
        P.memset("dve", H32[:], 0.0, [H32])
        P.memset("dve", Hb[:], 0.0, [Hb])
        ytot = P.psb("ytot", [128, NCH, 256], F32)
        for c in range(NCH):
            P.tt("pool", ytot[:, c, :], XBZ[:, c, 0:256], sm[:, O_DSK:O_DSK + 256], ALU.mult, [(XBZ, c), sm], [(ytot, c)])
        Gm = [[P.psb("Gm", [128, 128], F32) for _ in range(2)] for _ in range(2)]
        LD = [P.psb("LD", [128, 128], F32) for _ in range(2)]
        E = [P.psb("E", [128, 128], F32) for _ in range(2)]
        M = [P.psb("M", [128, 128], BF16) for _ in range(2)]
        xdt = [P.psb("xdt", [128, 64], BF16) for _ in range(2)]
        xdw = [P.psb("xdw", [128, 64], BF16) for _ in range(2)]
        y1 = [P.psb("y1", [128, 64], F32) for _ in range(2)]
        y2 = [P.psb("y2", [128, 64], F32) for _ in range(2)]
        it = 0
        for d in range(2):
            order = list(range(NCH)) if d == 0 else [1, 0] + list(range(NCH - 1, 1, -1))
            cmask = C_TRI if d == 0 else C_TRU
            cstr = C_SG if d == 0 else C_SL
            for c in order:
                need_y = ctx_out or c >= 2
                c0 = ccol(c)
                for g in range(2):
                    gm = Gm[it % 2][g]
                    if need_y:
                        psG = self.nps()
                        P.mm(psG[:, 0:128], xc[:, 2 + g, c0:c0 + 128], xc[:, 4 + g, c0:c0 + 128], True, True, [xc], [psG])
                        P.tt("dve", gm[:], psG[:, 0:128], cs[:, cmask:cmask + 128], ALU.mult, [psG, cs], [gm])
                    for hh in range(2):
                        h = 2 * g + hh
                        j = d * 4 + h
                        i2 = it % 2
                        it += 1
                        if need_y:
                            P.ts("pool", LD[i2][:], cs[:, cstr:cstr + 128], la[:, c, j:j + 1], None, ALU.mult, None, [cs, la], [LD[i2]])
                            psD = self.nps()
                            P.mm(psD[:, 0:128], LD[i2][:], cs[:, cmask:cmask + 128], True, True, [LD[i2], cs], [psD])
                            P.act(E[i2][:], psD[:, 0:128], AF.Exp, [psD], [E[i2]])
                            P.tt("dve", M[i2][:], gm[:], E[i2][:], ALU.mult, [gm, E[i2]], [M[i2]])
                        P.ts("pool", xdt[i2][:], XBZ[:, c, h * 64:(h + 1) * 64], dt[:, c, j:j + 1], None, ALU.mult, None,
                             [(XBZ, c), dt], [xdt[i2]])
                        P.ts("pool", xdw[i2][:], xdt[i2][:], wdec[:, c, j:j + 1], None, ALU.mult, None, [xdt[i2], wdec], [xdw[i2]])
                        if need_y:
                            psY = self.nps()
                            P.mm(psY[:, 0:64], M[i2][:], xdt[i2][:], True, True, [M[i2], xdt[i2]], [psY])
                            psI = self.nps()
                            P.mm(psI[:, 0:64], xc[:, 4 + g, c0:c0 + 128], Hb[:, j, :], True, True, [xc, (Hb, j)], [psI])
                            P.copy("act", y1[i2][:], psY[:, 0:64], [psY], [y1[i2]])
                            P.stt("dve", y2[i2][:], psI[:, 0:64], eac[:, c, j:j + 1], y1[i2][:], ALU.mult, ALU.add,
                                  [psI, eac, y1[i2]], [y2[i2]])
                            P.tt("pool", ytot[:, c, h * 64:(h + 1) * 64], ytot[:, c, h * 64:(h + 1) * 64], y2[i2][:], ALU.add,
                                 [(ytot, c), y2[i2]], [(ytot, c)])
                        psS = self.nps()
                        P.mm(psS[:, 0:64], XBZ[:, c, 256 + g * 128:256 + (g + 1) * 128], xdw[i2][:], True, True,
                             [(XBZ, c), xdw[i2]], [psS])
                        P.stt("dve", H32[:, j, :], H32[:, j, :], dA[:, c, j:j + 1], psS[:, 0:64], ALU.mult, ALU.add,
                              [(H32, j), dA, psS], [(H32, j)])
                        P.copy("act", Hb[:, j, :], H32[:, j, :], [(H32, j)], [(Hb, j)])
        ofm = P.psb("ofm", [128, 2, TOK], BF16)
        gte = [P.psb("gte", [128, 256], F32) for _ in range(2)]
        junk = P.psb("junk", [128, 256], F32)
        ssq = [P.psb("ssq", [128, 1], F32) for _ in range(2)]
        ob = [P.psb("ob", [128, 256], BF16) for _ in range(2)]
        cstart = 0 if ctx_out else 2
        for c in range(cstart, NCH):
            i2 = c % 2
            P.tt("dve", gte[i2][:], ytot[:, c, :], XBZ[:, c, 512:768], ALU.mult, [(ytot, c), (XBZ, c)], [gte[i2]])
            P.act(junk[:], gte[i2][:], AF.Square, [gte[i2]], [junk, ssq[i2]], accum_out=ssq[i2][:])
            P.act(ssq[i2][:], ssq[i2][:], AF.Sqrt, [ssq[i2]], [ssq[i2]], scale=1.0 / 256, bias=EPS)
            P.op("dve", lambda e: e.reciprocal(out=ssq[i2][:], in_=ssq[i2][:]), [ssq[i2]], [ssq[i2]])
            P.stt("dve", ob[i2][:], gte[i2][:], ssq[i2][:, 0:1], sm[:, O_SNG:O_SNG + 256], ALU.mult, ALU.mult,
                  [gte[i2], ssq[i2], sm], [ob[i2]])
            pbk = self.pb[c % 2]
            for i in range(2):
                P.tr(pbk[:, i * 128:(i + 1) * 128], ob[i2][:, i * 128:(i + 1) * 128], self.id_b[:], [ob[i2], self.id_b], [pbk])
            P.copy("act", ofm[:, :, c * 128:(c + 1) * 128], pbk[:, 0:256].rearrange("p (i t) -> p i t", i=2), [pbk], [ofm])
        t0 = cstart * 128
        P.dma(fm_view(self.o[b], 768, 2, t0, TOK - t0), ofm[:, :, t0:TOK], reads=[ofm], writes=[self.o[b]])
        P.phase_end()

    def m3(self, l, b, ctx_out):
        P = self.P
        P.phase_begin()
        q = P.psb("q", [128, 2, TOK], BF16)
        k = P.psb("k", [128, 2, TOK], BF16)
        v = P.psb("v", [128, NCH, 256], BF16)
        vs = P.psb("vs", [128, NCH - 1, 256], BF16)
        P.dma(q[:], fm_view(self.pr[b], 1024, 2, 0, TOK), reads=[self.pr[b]], writes=[q])
        P.dma(k[:], fm_view(self.pr[b], 1280, 2, 0, TOK), reads=[self.pr[b]], writes=[k])
        P.dma(v[:], self.vtm[b].ap().rearrange("(c p) d -> p c d", p=128), reads=[self.vtm[b]], writes=[v])
        P.dma(vs[:], self.vtm[b].ap()[64:64 + (NCH - 1) * 128, :].rearrange("(c p) d -> p c d", p=128),
              reads=[self.vtm[b]], writes=[vs])
        t32 = P.psb("t32", [128, 3840], F32)
        T8 = P.psb("T8", [128, 4, 960], BF16)
        P.dma(t32[:], self.ttab[l], writes=[t32])
        P.act(T8[:].rearrange("p h n -> p (h n)"), t32[:], AF.Copy, [t32], [T8], scale=8.0)
        pT = [P.psb("pT", [128, 6, 64], BF16) for _ in range(2)]
        rec = [P.psb("rec", [64, 512], F32) for _ in range(2)]
        obf = [P.psb("obf", [64, 512], BF16) for _ in range(2)]
        it = 0
        for h in range(4):
            p0 = (h % 2) * 64
            ch = h // 2
            if ctx_out:
                psS = self.nps()
                for jc in range(2):
                    P.mm(psS[:, jc * 256:(jc + 1) * 256], k[p0:p0 + 64, ch, jc * 128:(jc + 1) * 128], q[p0:p0 + 64, ch, 0:256],
                         True, True, [k, q], [psS])
                pt2 = P.psb("pt2", [128, 512], BF16)
                P.act(pt2[:], psS[:, :], AF.Exp, [psS], [pt2], scale=0.125)
                pn, pd = self.nps(), self.nps()
                for jc in range(2):
                    P.mm(pn[0:64, 0:256], v[:, jc, h * 64:(h + 1) * 64], pt2[:, jc * 256:(jc + 1) * 256], jc == 0, jc == 1, [v, pt2], [pn])
                for jc in range(2):
                    P.mm(pd[0:64, 0:256], self.one_b[:, 0:64], pt2[:, jc * 256:(jc + 1) * 256], jc == 0, jc == 1, [self.one_b, pt2], [pd])
                i2 = it % 2
                it += 1
                P.op("dve", lambda e: e.reciprocal(out=rec[i2][:, 0:256], in_=pd[0:64, 0:256]), [pd], [rec[i2]])
                P.tt("dve", obf[i2][:, 0:256], pn[0:64, 0:256], rec[i2][:, 0:256], ALU.mult, [pn, rec[i2]], [obf[i2]])
                P.dma(self.o[b].ap()[512 + h * 64:512 + (h + 1) * 64, 0:256], obf[i2][:, 0:256], reads=[obf[i2]], writes=[self.o[b]])
            for r0 in range(0, 32, 8):
                gi = (r0 // 8) % 2
                pn, pd = self.ps[gi * 2], self.ps[gi * 2 + 1]
                for r in range(r0, r0 + 8):
                    sr = min(max(r - 4, 0), 24)
                    psS = self.ps[4 + r % 2]
                    qs = q[p0:p0 + 64, ch, 256 + r * 64:256 + (r + 1) * 64]
                    kts = []
                    for jc in range(6):
                        kt0 = 256 + (sr + 2 * jc) * 64 if jc < 4 else (jc - 4) * 128
                        kts.append(kt0)
                        P.mm(psS[:, jc * 64:(jc + 1) * 64], k[p0:p0 + 64, ch, kt0:kt0 + 128], qs, True, jc >= 4, [k, q], [psS])
                        if jc < 4:
                            off = (sr - r + 7 + 2 * jc) * 64
                            P.mm(psS[:, jc * 64:(jc + 1) * 64], T8[p0:p0 + 64, h, off:off + 128], self.id_b[p0:p0 + 64, p0:p0 + 64],
                                 False, True, [T8, self.id_b], [psS])
                    pt = pT[r % 2]
                    P.act(pt[:].rearrange("p a b -> p (a b)"), psS[:, 0:384], AF.Exp, [psS], [pt], scale=0.125)
                    col = (r - r0) * 64
                    for jc in range(6):
                        kt0 = kts[jc]
                        if kt0 % 128 == 0:
                            vv = v[:, kt0 // 128, h * 64:(h + 1) * 64]
                        else:
                            vv = vs[:, (kt0 - 64) // 128, h * 64:(h + 1) * 64]
                        P.mm(pn[0:64, col:col + 64], vv, pt[:, jc, :], jc == 0, jc == 5, [v, vs, pt], [pn])
                    for jc in range(6):
                        P.mm(pd[0:64, col:col + 64], self.one_b[:, 0:64], pt[:, jc, :], jc == 0, jc == 5, [self.one_b, pt], [pd])
                i2 = it % 2
                it += 1
                P.op("dve", lambda e: e.reciprocal(out=rec[i2][:], in_=pd[0:64, :]), [pd], [rec[i2]])
                P.tt("dve", obf[i2][:], pn[0:64, :], rec[i2][:], ALU.mult, [pn, rec[i2]], [obf[i2]])
                P.dma(self.o[b].ap()[512 + h * 64:512 + (h + 1) * 64, 256 + r0 * 64:256 + r0 * 64 + 512], obf[i2][:],
                      reads=[obf[i2]], writes=[self.o[b]])
        P.phase_end()

    def m23(self, l, b, ctx_out):
        P = self.P
        sm = self.sm_s[l]
        cs = self.cst_s
        W = WP
        P.phase_begin()
        xc = P.psb("xc", [128, 6, WP], BF16)
        XBZ = P.psb("XBZ", [128, NCH, 768], BF16)
        T8 = P.psb("T8", [128, 4, 960], BF16)
        P.phase_begin()
        xp = P.psb("xp", [128, 6, WP], BF16)
        self.zero_pads(xp)
        for ch in range(6):
            self.load_padded(xp, ch, self.pr[b], 2048 + ch * 128)
        zf = P.psb("zf", [128, 2, TOK], BF16)
        P.dma(zf[:], fm_view(self.pr[b], 1792, 2, 0, TOK), reads=[self.pr[b]], writes=[zf])
        t32 = P.psb("t32", [128, 3840], F32)
        P.dma(t32[:], self.ttab[l], writes=[t32])
        acc = [P.psb("acc", [128, WP], F32) for _ in range(2)]
        for ch in range(6):
            a = acc[ch % 2]
            cw = lambda tap: sm[:, O_SCW + ch * 3 + tap:O_SCW + ch * 3 + tap + 1]
            eng = "dve"
            P.act(a[:, 1:W - 1], xp[:, ch, 0:W - 2], AF.Identity, [xp, sm], [a], scale=cw(0))
            P.stt(eng, a[:, 1:W - 1], xp[:, ch, 1:W - 1], cw(1), a[:, 1:W - 1], ALU.mult, ALU.add, [xp, sm, a], [a])
            P.stt(eng, a[:, 1:W - 1], xp[:, ch, 2:W], cw(2), a[:, 1:W - 1], ALU.mult, ALU.add, [xp, sm, a], [a])
            P.act(xc[:, ch, 1:W - 1], a[:, 1:W - 1], AF.Silu, [a, sm], [(xc, ch)], bias=sm[:, O_SCB + ch:O_SCB + ch + 1])
        P.act(zf[:], zf[:], AF.Silu, [zf], [zf])
        P.act(T8[:].rearrange("p h n -> p (h n)"), t32[:], AF.Copy, [t32], [T8], scale=8.0)
        for c in range(NCH):
            pbk = self.pb[c % 2]
            c0 = ccol(c)
            for i in range(4):
                P.tr(pbk[:, i * 128:(i + 1) * 128], xc[:, i, c0:c0 + 128], self.id_b[:], [(xc, i), self.id_b], [pbk])
            for i in range(2):
                P.tr(pbk[:, 512 + i * 128:512 + (i + 1) * 128], zf[:, i, c * 128:(c + 1) * 128], self.id_b[:], [zf, self.id_b], [pbk])
            P.copy("dve" if c % 2 else "act", XBZ[:, c, :], pbk[:, 0:768], [pbk], [(XBZ, c)])
        P.phase_end()
        q = P.psb("q", [128, 2, TOK], BF16)
        k = P.psb("k", [128, 2, TOK], BF16)
        v = P.psb("v", [128, NCH, 256], BF16)
        vs = P.psb("vs", [128, NCH - 1, 256], BF16)
        P.dma(q[:], fm_view(self.pr[b], 1024, 2, 0, TOK), reads=[self.pr[b]], writes=[q])
        P.dma(k[:], fm_view(self.pr[b], 1280, 2, 0, TOK), reads=[self.pr[b]], writes=[k])
        P.dma(v[:], self.vtm[b].ap().rearrange("(c p) d -> p c d", p=128), reads=[self.vtm[b]], writes=[v])
        P.dma(vs[:], self.vtm[b].ap()[64:64 + (NCH - 1) * 128, :].rearrange("(c p) d -> p c d", p=128),
              reads=[self.vtm[b]], writes=[vs])

        def t3(name):
            return P.psb(name, [128, NCH, 8], F32)
        dt, la, acum, tot, eac, wdec, dA, dtw = [t3(n) for n in ("dt", "la", "acum", "tot", "eac", "wdec", "dA", "dtw")]
        ea = P.psb("ea", [128, 8], F32)
        dtf = P.psb("dtf", [8, TOK], F32)
        P.dma(dtf[:], self.dtm[b].ap(), reads=[self.dtm[b]], writes=[dtf])
        psq = self.ps[4]
        for c in range(NCH):
            P.tr(psq[:, c * 8:(c + 1) * 8], dtf[0:8, c * 128:(c + 1) * 128], cs[0:8, C_ID:C_ID + 8], [dtf, cs], [psq])
        P.copy("dve", dt[:].rearrange("p c j -> p (c j)"), psq[:, 0:144], [psq], [dt])
        bc = lambda off: sm[:, off:off + 8].unsqueeze(1).broadcast_to([128, NCH, 8])
        P.tt("dve", dt[:], dt[:], bc(O_DTB), ALU.add, [dt, sm], [dt])
        P.act(dt[:], dt[:], AF.Exp, [dt], [dt])
        P.act(dt[:], dt[:], AF.Ln, [dt], [dt], bias=1.0)
        P.act(ea[:], sm[:, O_ALOG:O_ALOG + 8], AF.Exp, [sm], [ea])
        P.stt("dve", la[:], dt[:], -1.0, ea[:, 0:8].unsqueeze(1).broadcast_to([128, NCH, 8]), ALU.mult, ALU.mult, [dt, ea], [la])
        la_hb = P.psb("la_hb", [128, NCH, 8], BF16)
        la_hi = t3("la_hi")
        la_lo = t3("la_lo")
        P.copy("dve", la_hb[:], la[:], [la], [la_hb])
        P.copy("dve", la_hi[:], la_hb[:], [la_hb], [la_hi])
        P.tt("dve", la_lo[:], la[:], la_hi[:], ALU.subtract, [la, la_hi], [la_lo])
        laf = la[:].rearrange("p c j -> p (c j)")
        psA, psB, psT = self.ps[4], self.ps[5], self.ps[0]
        P.mm(psA[:, 0:144], cs[:, C_TRI:C_TRI + 128], laf, True, True, [cs, la], [psA])
        P.mm(psB[:, 0:144], cs[:, C_TRU:C_TRU + 128], laf, True, True, [cs, la], [psB])
        P.mm(psT[:, 0:144], cs[:, C_ONE:C_ONE + 128], laf, True, True, [cs, la], [psT])
        v3 = lambda ps: ps[:, 0:144].rearrange("p (c j) -> p c j", j=8)
        P.copy("dve", acum[:, :, 0:4], v3(psA)[:, :, 0:4], [psA], [acum])
        P.copy("dve", acum[:, :, 4:8], v3(psB)[:, :, 4:8], [psB], [acum])
        P.copy("dve", tot[:], v3(psT), [psT], [tot])
        P.act(eac[:], acum[:], AF.Exp, [acum], [eac])
        P.act(dA[:], tot[:], AF.Exp, [tot], [dA])
        P.tt("dve", wdec[:], tot[:], acum[:], ALU.subtract, [tot, acum], [wdec])
        P.act(wdec[:], wdec[:], AF.Exp, [wdec], [wdec])
        P.tt("dve", dtw[:], dt[:], wdec[:], ALU.mult, [dt, wdec], [dtw])
        H32 = P.psb("H32", [128, 8, 64], F32)
        Hb = P.psb("Hb", [128, 8, 64], BF16)
        P.memset("dve", H32[:], 0.0, [H32])
        P.memset("dve", Hb[:], 0.0, [Hb])
        ytot = P.psb("ytot", [128, NCH, 256], F32)
        P.tt("dve", ytot[:], XBZ[:, :, 0:256], sm[:, O_DSK:O_DSK + 256].unsqueeze(1).broadcast_to([128, NCH, 256]), ALU.mult,
             [XBZ, sm], [ytot])
        NBUF = 6
        Gm = [P.psb("Gm", [128, 128], F32) for _ in range(4)]
        LDf = [P.psb("LDf", [128, 128], F32) for _ in range(NBUF)]
        E = [P.psb("E", [128, 128], F32) for _ in range(NBUF)]
        M = [P.psb("M", [128, 128], BF16) for _ in range(NBUF)]
        xdw = [P.psb("xdw", [128, 64], BF16) for _ in range(NBUF)]
        subs = []
        gcount = 0
        for d in range(2):
            order = list(range(NCH)) if d == 0 else [1, 0] + list(range(NCH - 1, 1, -1))
            for c in order:
                for g in range(2):
                    for hh in range(2):
                        subs.append(dict(d=d, c=c, g=g, hh=hh, h=2 * g + hh, j=d * 4 + 2 * g + hh, gi=gcount,
                                         need_y=(ctx_out or c >= 2), c0=ccol(c),
                                         cmask=(C_TRI if d == 0 else C_TRU), cstr=(C_SG if d == 0 else C_SL)))
                    gcount += 1
        NS = len(subs)
        bank = {}
        _nps = self.nps

        def nps_safe():
            for _ in range(12):
                p = _nps()
                if all(p is not q_ for q_ in bank.values()):
                    return p
            raise RuntimeError("no free PSUM bank")

        def S0(i):
            u = subs[i]; c, g, h, j, c0 = u["c"], u["g"], u["h"], u["j"], u["c0"]; ib = i % NBUF
            if u["need_y"]:
                if u["hh"] == 0:
                    bG = nps_safe()
                    gm = Gm[u["gi"] % 4]
                    P.mm(bG[:, 0:128], xc[:, 2 + g, c0:c0 + 128], xc[:, 4 + g, c0:c0 + 128], True, True, [xc], [bG])
                    P.tt("dve", gm[:], bG[:, 0:128], cs[:, u["cmask"]:u["cmask"] + 128], ALU.mult, [bG, cs], [gm])
                P.act(LDf[ib][:], cs[:, u["cstr"]:u["cstr"] + 128], AF.Identity, [cs, la], [LDf[ib]], scale=la[:, c, j:j + 1])
            P.act(xdw[ib][:], XBZ[:, c, h * 64:(h + 1) * 64], AF.Identity, [(XBZ, c), dtw], [xdw[ib]], scale=dtw[:, c, j:j + 1])

        def S1(i):
            u = subs[i]; c, g, h, j, c0 = u["c"], u["g"], u["h"], u["j"], u["c0"]; ib = i % NBUF
            if u["need_y"]:
                bD = nps_safe()
                P.mm(bD[:, 0:128], LDf[ib][:], cs[:, u["cmask"]:u["cmask"] + 128], True, True, [LDf[ib], cs], [bD])
                bI = nps_safe()
                P.mm(bI[:, 0:64], xc[:, 4 + g, c0:c0 + 128], Hb[:, j, :], True, True, [xc, (Hb, j)], [bI])
                bank[(i, "D")] = bD
                bank[(i, "I")] = bI
            bS = nps_safe()
            P.mm(bS[:, 0:64], XBZ[:, c, 256 + g * 128:256 + (g + 1) * 128], xdw[ib][:], True, True, [(XBZ, c), xdw[ib]], [bS])
            bank[(i, "S")] = bS

        def S2(i):
            u = subs[i]; c, g, h, j = u["c"], u["g"], u["h"], u["j"]; ib = i % NBUF
            ysl = ytot[:, c, h * 64:(h + 1) * 64]
            if u["need_y"]:
                bD = bank.pop((i, "D"))
                bI = bank.pop((i, "I"))
                P.act(E[ib][:], bD[:, 0:128], AF.Exp, [bD], [E[ib]])
                P.stt("dve", ysl, bI[:, 0:64], eac[:, c, j:j + 1], ysl, ALU.mult, ALU.add, [bI, eac, (ytot, c)], [(ytot, c)])
            bS = bank.pop((i, "S"))
            P.stt("dve", H32[:, j, :], H32[:, j, :], dA[:, c, j:j + 1], bS[:, 0:64], ALU.mult, ALU.add,
                  [(H32, j), dA, bS], [(H32, j)])
            P.copy("pool", Hb[:, j, :], H32[:, j, :], [(H32, j)], [(Hb, j)])

        def S3(i):
            u = subs[i]; c, h, j = u["c"], u["h"], u["j"]; ib = i % NBUF
            if u["need_y"]:
                gm = Gm[u["gi"] % 4]
                P.stt("dve", M[ib][:], gm[:], dt[:, c, j:j + 1], E[ib][:], ALU.mult, ALU.mult, [gm, dt, E[ib]], [M[ib]])
                bY = nps_safe()
                P.mm(bY[:, 0:64], M[ib][:], XBZ[:, c, h * 64:(h + 1) * 64], True, True, [M[ib], (XBZ, c)], [bY])
                bank[(i, "Y")] = bY

        def S4(i):
            u = subs[i]; c, h = u["c"], u["h"]
            if u["need_y"]:
                ysl = ytot[:, c, h * 64:(h + 1) * 64]
                bY = bank.pop((i, "Y"))
                P.tt("dve", ysl, bY[:, 0:64], ysl, ALU.add, [bY, (ytot, c)], [(ytot, c)])

        def ssd_gen():
            stages = [S0, S1, S2, S3, S4]
            for t in range(NS + len(stages) - 1):
                for k in reversed(range(len(stages))):
                    i = t - k
                    if 0 <= i < NS:
                        stages[k](i)
                yield

        pT = [P.psb("pT", [128, 6, 64], BF16) for _ in range(3)]
        rec = [P.psb("rec", [64, 512], F32) for _ in range(2)]
        obf = [P.psb("obf", [64, 512], BF16) for _ in range(2)]
        pt2 = P.psb("pt2", [128, 512], BF16) if ctx_out else None

        def na_gen():
            it = 0
            for h in range(4):
                p0 = (h % 2) * 64
                ch = h // 2
                if ctx_out:
                    psS, pn, pd = self.ps[4], self.ps[0], self.ps[1]
                    for jc in range(2):
                        P.mm(psS[:, jc * 256:(jc + 1) * 256], k[p0:p0 + 64, ch, jc * 128:(jc + 1) * 128], q[p0:p0 + 64, ch, 0:256],
                             True, True, [k, q], [psS])
                    P.act(pt2[:], psS[:, :], AF.Exp, [psS], [pt2], scale=0.125)
                    for jc in range(2):
                        P.mm(pn[0:64, 0:256], v[:, jc, h * 64:(h + 1) * 64], pt2[:, jc * 256:(jc + 1) * 256], jc == 0, jc == 1, [v, pt2], [pn])
                    for jc in range(2):
                        P.mm(pd[0:64, 0:256], self.one_b[:, 0:64], pt2[:, jc * 256:(jc + 1) * 256], jc == 0, jc == 1, [self.one_b, pt2], [pd])
                    i2 = it % 2
                    it += 1
                    P.op("dve", lambda e: e.reciprocal(out=rec[i2][:, 0:256], in_=pd[0:64, 0:256]), [pd], [rec[i2]])
                    P.tt("dve", obf[i2][:, 0:256], pn[0:64, 0:256], rec[i2][:, 0:256], ALU.mult, [pn, rec[i2]], [obf[i2]])
                    P.dma(self.o[b].ap()[512 + h * 64:512 + (h + 1) * 64, 0:256], obf[i2][:, 0:256], reads=[obf[i2]], writes=[self.o[b]])
                    yield
                def scores(r):
                    sr = min(max(r - 4, 0), 24)
                    psS = self.ps[4 + r % 2]
                    qs = q[p0:p0 + 64, ch, 256 + r * 64:256 + (r + 1) * 64]
                    kts = []
                    for jc in range(6):
                        kt0 = 256 + (sr + 2 * jc) * 64 if jc < 4 else (jc - 4) * 128
                        kts.append(kt0)
                        P.mm(psS[:, jc * 64:(jc + 1) * 64], k[p0:p0 + 64, ch, kt0:kt0 + 128], qs, True, jc >= 4, [k, q], [psS])
                        if jc < 4:
                            off = (sr - r + 7 + 2 * jc) * 64
                            P.mm(psS[:, jc * 64:(jc + 1) * 64], T8[p0:p0 + 64, h, off:off + 128], self.id_b[p0:p0 + 64, p0:p0 + 64],
                                 False, True, [T8, self.id_b], [psS])
                    pt = pT[r % 3]
                    P.act(pt[:].rearrange("p a b -> p (a b)"), psS[:, 0:384], AF.Exp, [psS], [pt], scale=0.125)
                    return kts

                def pv(r, kts):
                    nonlocal it
                    r0 = (r // 8) * 8
                    gi_ = (r0 // 8) % 2
                    pn, pd = self.ps[gi_ * 2], self.ps[gi_ * 2 + 1]
                    pt = pT[r % 3]
                    col = (r - r0) * 64
                    for jc in range(6):
                        kt0 = kts[jc]
                        if kt0 % 128 == 0:
                            vv = v[:, kt0 // 128, h * 64:(h + 1) * 64]
                        else:
                            vv = vs[:, (kt0 - 64) // 128, h * 64:(h + 1) * 64]
                        P.mm(pn[0:64, col:col + 64], vv, pt[:, jc, :], jc == 0, jc == 5, [v, vs, pt], [pn])
                    for jc in range(6):
                        P.mm(pd[0:64, col:col + 64], self.one_b[:, 0:64], pt[:, jc, :], jc == 0, jc == 5, [self.one_b, pt], [pd])
                    if r == r0 + 7:
                        i2 = it % 2
                        it += 1
                        P.op("dve", lambda e: e.reciprocal(out=rec[i2][:], in_=pd[0:64, :]), [pd], [rec[i2]])
                        P.tt("dve", obf[i2][:], pn[0:64, :], rec[i2][:], ALU.mult, [pn, rec[i2]], [obf[i2]])
                        P.dma(self.o[b].ap()[512 + h * 64:512 + (h + 1) * 64, 256 + r0 * 64:256 + r0 * 64 + 512], obf[i2][:],
                              reads=[obf[i2]], writes=[self.o[b]])

                prev = None
                for r in range(33):
                    cur = scores(r) if r < 32 else None
                    if prev is not None:
                        pv(r - 1, prev)
                    prev = cur
                    yield

        import os
        g1, g2 = ssd_gen(), na_gen()
        alive = [g1, g2]
        if os.environ.get("K_M23") == "nossd":
            alive = [g2]
        if os.environ.get("K_M23") == "nona":
            alive = [g1]
        for g in alive:
            for _ in g:
                pass
        ofm = P.psb("ofm", [128, 2, TOK], BF16)
        junk = P.psb("junk", [128, 256], F32)
        ssq = P.psb("ssq", [128, NCH], F32)
        obA = P.psb("obA", [128, NCH, 256], BF16)
        cstart = 0 if ctx_out else 2
        ncc = NCH - cstart
        P.tt("dve", ytot[:, cstart:, :], ytot[:, cstart:, :], XBZ[:, cstart:, 512:768], ALU.mult, [ytot, XBZ], [ytot])
        for c in range(cstart, NCH):
            P.act(junk[:], ytot[:, c, :], AF.Square, [ytot], [junk, (ssq, c)], accum_out=ssq[:, c:c + 1])
        P.act(ssq[:, cstart:], ssq[:, cstart:], AF.Ln, [ssq], [ssq], scale=1.0 / 256, bias=EPS)
        P.act(ssq[:, cstart:], ssq[:, cstart:], AF.Exp, [ssq], [ssq], scale=-0.5)
        P.tt("dve", ytot[:, cstart:, :], ytot[:, cstart:, :], ssq[:, cstart:].unsqueeze(2).broadcast_to([128, ncc, 256]), ALU.mult,
             [ytot, ssq], [ytot])
        P.tt("dve", obA[:, cstart:, :], ytot[:, cstart:, :], sm[:, O_SNG:O_SNG + 256].unsqueeze(1).broadcast_to([128, ncc, 256]),
             ALU.mult, [ytot, sm], [obA])
        gi = 0
        for c4 in range(cstart, NCH, 4):
            cs_ = list(range(c4, min(c4 + 4, NCH)))
            pbk = self.pb[gi % 2]
            gi += 1
            for ci, c in enumerate(cs_):
                for i in range(2):
                    P.tr(pbk[:, (i * 4 + ci) * 128:(i * 4 + ci + 1) * 128], obA[:, c, i * 128:(i + 1) * 128], self.id_b[:],
                         [obA, self.id_b], [pbk])
            nn = len(cs_)
            P.copy("act" if gi % 2 else "dve", ofm[:, :, c4 * 128:(c4 + nn) * 128],
                   pbk[:, :].rearrange("p (i t) -> p i t", i=2)[:, :, 0:nn * 128], [pbk], [ofm])
        t0 = cstart * 128
        P.dma(fm_view(self.o[b], 768, 2, t0, TOK - t0), ofm[:, :, t0:TOK], reads=[ofm], writes=[self.o[b]])
        P.phase_end()

    def c1(self, l):
        P = self.P
        cs = self.cst_s
        moe = (l % 2 == 1)
        last = (l == NL - 1)
        P.phase_begin()
        wo = P.psb("wo", [128, 8, D], BF16)
        P.dma(wo[:], self.w_out_b[l].ap().rearrange("(k p) n -> p k n", p=128), reads=[self.w_out_b[l]], writes=[wo])
        xt2 = [P.psb("xt", [128, 8, 512], F32) for _ in range(2)]
        ot2 = [P.psb("ot", [128, 8, 512], BF16) for _ in range(2)]
        sq = P.psb("sq", [128, 8, 512], BF16)
        rstd = P.psb("rstd", [128, 512], F32)
        tmp = P.psb("tmp", [128, 8, 512], F32)
        hb2 = [P.psb("hb", [128, 8, 512], BF16) for _ in range(2)]
        h32 = P.psb("h32", [128, 8, 512], F32) if moe else None
        if moe:
            wr = self.gsm_s
            lg = P.psb("lg", [128, 32], F32)
            lg2 = P.psb("lg2", [128, 32], F32)
            eq1 = P.psb("eq1", [128, 32], F32)
            eq2 = P.psb("eq2", [128, 32], F32)
            gt = P.psb("gt", [128, 32], F32)
            m1 = P.psb("m1", [128, 4], F32)
            m2 = P.psb("m2", [128, 4], F32)
            dd = P.psb("dd", [128, 4], F32)
            ee = P.psb("ee", [128, 4], F32)
            g1 = P.psb("g1", [128, 4], F32)
            g2 = P.psb("g2", [128, 4], F32)
            gts = P.psb("gts", [8, 512], F32)
        tiles = [(b, t0, n) for b in range(NB) for (t0, n) in TILES if not (t0 == 0 and last)]

        def ldc(i):
            b, t0, n = tiles[i]
            P.dma(xt2[i % 2][:, :, :n], fm_view(self.xs[b], 0, 8, t0, n), reads=[self.xs[b]], writes=[xt2[i % 2]])
            P.dma(ot2[i % 2][:, :, :n], fm_view(self.o[b], 0, 8, t0, n), reads=[self.o[b]], writes=[ot2[i % 2]])

        def outproj(i):
            b, t0, n = tiles[i]
            j = 4 if t0 == 0 else b
            xt, ot = xt2[i % 2], ot2[i % 2]
            for m in range(8):
                ps = self.nps()
                for k in range(8):
                    P.mm(ps[:, :n], wo[:, k, m * 128:(m + 1) * 128], ot[:, k, :n], k == 0, k == 7, [wo, ot], [ps])
                P.stt("dve", xt[:, m, :n], ps[:, :n], self.mod[:, 16 + m, j:j + 1], xt[:, m, :n], ALU.mult, ALU.add,
                      [ps, self.mod, (xt, m)], [(xt, m)])
            P.dma(fm_view(self.xs[b], 0, 8, t0, n), xt[:, :, :n], reads=[xt], writes=[self.xs[b]])

        def norm1(i):
            b, t0, n = tiles[i]
            self.norm_p1(xt2[i % 2], n, sq, rstd, tmp)

        def norm2(i):
            b, t0, n = tiles[i]
            j = 4 if t0 == 0 else b
            hb = hb2[i % 2]
            self.norm_p2(n, self.G2, 24, j, hb, tmp, h32)
            P.dma(fm_view(self.h2[b], 0, 8, t0, n), hb[:, :, :n], reads=[hb], writes=[self.h2[b]])
            if moe:
                ns = n // 128
                ps = self.nps()
                for s_ in range(ns):
                    for k in range(8):
                        P.mm(ps[:, s_ * 8:(s_ + 1) * 8], h32[:, k, s_ * 128:(s_ + 1) * 128], wr[:, 40 + k * 8:40 + (k + 1) * 8],
                             k == 0, k == 7, [h32, wr], [ps])
                bc8 = lambda t: t[:, 0:ns].unsqueeze(2).broadcast_to([128, ns, 8])
                L3 = lambda t: t[:, 0:ns * 8].rearrange("p (s e) -> p s e", e=8)
                P.copy("dve", lg[:, 0:ns * 8], ps[:, 0:ns * 8], [ps], [lg])
                P.op("dve", lambda e: e.reduce_max(out=m1[:, 0:ns], in_=L3(lg), axis=AX.X), [lg], [m1])
                P.tt("dve", L3(eq1), L3(lg), bc8(m1), ALU.is_equal, [lg, m1], [eq1])
                P.stt("dve", lg2[:, 0:ns * 8], eq1[:, 0:ns * 8], -1e30, lg[:, 0:ns * 8], ALU.mult, ALU.add, [eq1, lg], [lg2])
                P.op("dve", lambda e: e.reduce_max(out=m2[:, 0:ns], in_=L3(lg2), axis=AX.X), [lg2], [m2])
                P.tt("dve", L3(eq2), L3(lg2), bc8(m2), ALU.is_equal, [lg2, m2], [eq2])
                P.tt("dve", dd[:, 0:ns], m2[:, 0:ns], m1[:, 0:ns], ALU.subtract, [m1, m2], [dd])
                P.act(ee[:, 0:ns], dd[:, 0:ns], AF.Exp, [dd], [ee])
                P.ts("dve", g1[:, 0:ns], ee[:, 0:ns], 1.0, None, ALU.add, None, [ee], [g1])
                P.op("dve", lambda e: e.reciprocal(out=g1[:, 0:ns], in_=g1[:, 0:ns]), [g1], [g1])
                P.tt("dve", g2[:, 0:ns], ee[:, 0:ns], g1[:, 0:ns], ALU.mult, [ee, g1], [g2])
                P.tt("dve", L3(gt), L3(eq1), bc8(g1), ALU.mult, [eq1, g1], [gt])
                P.tt("dve", L3(eq2), L3(eq2), bc8(g2), ALU.mult, [eq2, g2], [eq2])
                P.tt("dve", gt[:, 0:ns * 8], gt[:, 0:ns * 8], eq2[:, 0:ns * 8], ALU.add, [gt, eq2], [gt])
                pst = self.nps()
                for s_ in range(ns):
                    P.tr(pst[0:8, s_ * 128:(s_ + 1) * 128], gt[:, s_ * 8:(s_ + 1) * 8], cs[:, C_ID:C_ID + 128], [gt, cs], [pst])
                P.copy("act", gts[:, 0:n], pst[0:8, 0:n], [pst], [gts])
                P.dma(self.gT[b].ap()[:, t0 - 256:t0 - 256 + n], gts[:, :n], reads=[gts], writes=[self.gT[b]])

        NT = len(tiles)
        ldc(0)
        outproj(0)
        if NT > 1:
            ldc(1)
        for i in range(NT):
            norm1(i)
            if i + 1 < NT:
                outproj(i + 1)
            norm2(i)
            if i + 2 < NT:
                ldc(i + 2)
        P.phase_end()

    def final_out(self, b, t0, n, xt, yf, sq, rstd, otm):
        P = self.P
        cs = self.cst_s
        g = self.gsm_s
        P.act(sq[:, :, :n], xt[:, :, :n], AF.Square, [xt], [sq])
        ps = self.nps()
        for k in range(8):
            P.mm(ps[:, :n], self.one_b[:], sq[:, k, :n], k == 0, k == 7, [self.one_b, sq], [ps])
        P.act(rstd[:, :n], ps[:, :n], AF.Sqrt, [ps], [rstd], scale=1.0 / D, bias=EPS)
        P.op("dve", lambda e: e.reciprocal(out=rstd[:, :n], in_=rstd[:, :n]), [rstd], [rstd])
        for k in range(8):
            P.stt("dve", yf[:, k, :n], xt[:, k, :n], g[:, 32 + k:33 + k], rstd[:, :n], ALU.mult, ALU.mult, [(xt, k), g, rstd], [(yf, k)])
        for s in range(n // 128):
            ot = otm[s % 2]
            for hf in range(2):
                ps = self.nps()
                for kk in range(4):
                    k = hf * 4 + kk
                    P.tr(ps[:, kk * 128:(kk + 1) * 128], yf[:, k, s * 128:(s + 1) * 128], cs[:, C_ID:C_ID + 128], [(yf, k), cs], [ps])
                P.copy("act" if hf == 0 else "dve", ot[:, hf * 512:(hf + 1) * 512], ps[:, :], [ps], [ot])
            tt0 = t0 - 256 + s * 128
            d = P.dma(self.out[b, tt0:tt0 + 128, :], ot[:], reads=[ot], writes=[self.out])
            self.out_deps.append(d)

    def c2_dense(self, l):
        P = self.P
        last = (l == NL - 1)
        P.phase_begin()
        wg = P.psb("wg", [128, 8, 5632], BF16)
        wd = P.psb("wd", [128, 22, D], BF16)
        P.dma(wg[:, 0:4, :], self.ffn_gu_b.ap()[0:512, :].rearrange("(k p) n -> p k n", p=128), reads=[self.ffn_gu_b], writes=[wg])
        P.dma(wg[:, 4:8, :], self.ffn_gu_b.ap()[512:1024, :].rearrange("(k p) n -> p k n", p=128), reads=[self.ffn_gu_b], writes=[wg])
        P.dma(wd[:], self.ffn_down_b.ap().rearrange("(m p) n -> p m n", p=128), reads=[self.ffn_down_b], writes=[wd])
        NT = 256
        xt2 = [P.psb("xt", [128, 8, NT], F32) for _ in range(2)]
        hb2 = [P.psb("hb", [128, 8, NT], BF16) for _ in range(2)]
        a = P.psb("a", [128, 22, NT], BF16)
        sg = [P.psb("sg", [128, NT], F32) for _ in range(2)]
        bgf = [P.psb("bgf", [128, 1024], F32) for _ in range(2)]
        bgb = [P.psb("bgb", [128, 1024], BF16) for _ in range(2)]
        it = 0
        for b in range(NB):
            for t0 in range(0, TOK, NT):
                if t0 == 0 and last:
                    continue
                n = NT
                if self.nl > 1:
                    self.bg_step(9, bgf, bgb)
                j = 4 if t0 == 0 else b
                xt, hb = xt2[it % 2], hb2[it % 2]
                it += 1
                P.dma(xt[:], fm_view(self.xs[b], 0, 8, t0, n), reads=[self.xs[b]], writes=[xt])
                P.dma(hb[:], fm_view(self.h2[b], 0, 8, t0, n), reads=[self.h2[b]], writes=[hb])
                for m in range(22):
                    pg, pu = self.nps(), self.nps()
                    for k in range(8):
                        P.mm(pg[:, :n], wg[:, k, m * 128:(m + 1) * 128], hb[:, k, :], k == 0, k == 7, [wg, hb], [pg])
                    for k in range(8):
                        P.mm(pu[:, :n], wg[:, k, 2816 + m * 128:2816 + (m + 1) * 128], hb[:, k, :], k == 0, k == 7, [wg, hb], [pu])
                    s_ = sg[m % 2]
                    P.act(s_[:], pg[:, :n], AF.Silu, [pg], [s_])
                    P.tt("dve", a[:, m, :], pu[:, :n], s_[:], ALU.mult, [pu, s_], [(a, m)])
                for f in range(8):
                    ps = self.nps()
                    for m in range(22):
                        P.mm(ps[:, :n], wd[:, m, f * 128:(f + 1) * 128], a[:, m, :], m == 0, m == 21, [wd, a], [ps])
                    P.stt("dve", xt[:, f, :], ps[:, :n], self.mod[:, 40 + f, j:j + 1], xt[:, f, :], ALU.mult, ALU.add,
                          [ps, self.mod, (xt, f)], [(xt, f)])
                P.dma(fm_view(self.xs[b], 0, 8, t0, n), xt[:], reads=[xt], writes=[self.xs[b]])
        if self.nl > 1:
            self.bg_step(10 ** 6, bgf, bgb)
        P.phase_end()

    def c2_moe(self, l):
        P = self.P
        cs = self.cst_s
        last = (l == NL - 1)
        P.phase_begin()
        wgu = [P.psb("wgu", [128, 8, 2, 768], BF16) for _ in range(2)]
        wdn = [P.psb("wdn", [128, 6, D], BF16) for _ in range(2)]
        hb2 = [P.psb("hb", [128, 8, 512], BF16) for _ in range(2)]
        xt = P.psb("xt", [128, 8, 512], F32)
        acc = P.psb("acc", [128, 8, 512], F32)
        gbc = P.psb("gbc", [128, 8, 512], F32)
        gts = P.psb("gts", [8, 512], F32)
        a2 = [P.psb("a", [128, 6, 512], BF16) for _ in range(2)]
        sg = [P.psb("sg", [128, 512], F32) for _ in range(2)]
        sg2 = [P.psb("sg2", [128, 512], F32) for _ in range(2)]
        if last:
            sq = P.psb("sq", [128, 8, 512], BF16)
            rstd = P.psb("rstd", [128, 512], F32)
            otm = [P.psb("otm", [128, D], F32) for _ in range(2)]
        tiles = [(b, t0) for b in range(NB) for t0 in range(256, TOK, 512)]
        units = [(ti, u) for ti in range(len(tiles)) for u in range(16)]

        def load_unit(ui):
            ti, u = units[ui]
            e, half = u // 2, u % 2
            nm = 6 if half == 0 else 5
            wgt, wdt = wgu[ui % 2], wdn[ui % 2]
            for gu in range(2):
                c0 = gu * 1408 + half * 768
                P.dma(wgt[:, :, gu, 0:nm * 128], self.moe_gu_b[e].ap()[:, c0:c0 + nm * 128].rearrange("(k p) n -> p k n", p=128),
                      reads=[self.moe_gu_b[e]], writes=[wgt])
            P.dma(wdt[:, 0:nm, :], self.moe_down_b[e].ap()[half * 768:half * 768 + nm * 128, :].rearrange("(m p) n -> p m n", p=128),
                  reads=[self.moe_down_b[e]], writes=[wdt])

        def load_tile(ti):
            b, t0 = tiles[ti]
            P.dma(hb2[ti % 2][:], fm_view(self.h2[b], 0, 8, t0, 512), reads=[self.h2[b]], writes=[hb2[ti % 2]])

        load_unit(0)
        load_tile(0)
        ui = 0
        for ti, (b, t0) in enumerate(tiles):
            n = 512
            hb = hb2[ti % 2]
            if ti + 1 < len(tiles):
                load_tile(ti + 1)
            P.dma(xt[:], fm_view(self.xs[b], 0, 8, t0, n), reads=[self.xs[b]], writes=[xt])
            P.dma(gts[:], self.gT[b].ap()[:, t0 - 256:t0 - 256 + n], reads=[self.gT[b]], writes=[gts])
            for e in range(8):
                ps = self.nps()
                P.mm(ps[:, :n], cs[0:8, C_SEL + e * 128:C_SEL + (e + 1) * 128], gts[:, :n], True, True, [cs, gts], [ps])
                P.copy("act", gbc[:, e, :], ps[:, :n], [ps], [(gbc, e)])
            for u in range(16):
                if ui + 1 < len(units):
                    load_unit(ui + 1)
                e, half = u // 2, u % 2
                nm = 6 if half == 0 else 5
                wgt, wdt = wgu[ui % 2], wdn[ui % 2]
                a = a2[ui % 2]
                ui += 1
                for mi in range(nm):
                    pg, pu = self.nps(), self.nps()
                    for k in range(8):
                        P.mm(pg[:, :n], wgt[:, k, 0, mi * 128:(mi + 1) * 128], hb[:, k, :], k == 0, k == 7, [wgt, hb], [pg])
                    for k in range(8):
                        P.mm(pu[:, :n], wgt[:, k, 1, mi * 128:(mi + 1) * 128], hb[:, k, :], k == 0, k == 7, [wgt, hb], [pu])
                    s_, s2_ = sg[mi % 2], sg2[mi % 2]
                    P.act(s_[:], pg[:, :n], AF.Silu, [pg], [s_])
                    P.tt("pool", s2_[:], s_[:], gbc[:, e, :], ALU.mult, [s_, (gbc, e)], [s2_])
                    P.tt("dve", a[:, mi, :], pu[:, :n], s2_[:], ALU.mult, [pu, s2_], [(a, mi)])
                for f in range(8):
                    ps = self.nps()
                    for mi in range(nm):
                        P.mm(ps[:, :n], wdt[:, mi, f * 128:(f + 1) * 128], a[:, mi, :], mi == 0, mi == nm - 1, [wdt, a], [ps])
                    if u == 0:
                        P.copy("act", acc[:, f, :], ps[:, :n], [ps], [(acc, f)])
                    else:
                        P.tt("dve", acc[:, f, :], acc[:, f, :], ps[:, :n], ALU.add, [ps, (acc, f)], [(acc, f)])
            for f in range(8):
                P.stt("dve", xt[:, f, :], acc[:, f, :], self.mod[:, 40 + f, b:b + 1], xt[:, f, :], ALU.mult, ALU.add,
                      [(acc, f), self.mod, (xt, f)], [(xt, f)])
            if last:
                self.final_out(b, t0, n, xt, acc, sq, rstd, otm)
            else:
                P.dma(fm_view(self.xs[b], 0, 8, t0, n), xt[:], reads=[xt], writes=[self.xs[b]])
        P.phase_end()

    def build(self):
        P = self.P
        self.out_deps = []
        self.setup()
        self.bg_jobs_init()
        self.convert([0])
        for l in range(self.nl):
            ctx_out = l < NL - 1
            self.adaln(l)
            if self.stop_after == "adaln":
                break
            self.phaseA(l)
            if self.stop_after == "A":
                break
            for b in range(NB):
                self.m1(l, b)
                self.m23(l, b, ctx_out)
            if self.stop_after == "M":
                break
            self.c1(l)
            if self.stop_after == "C1":
                break
            if l % 2 == 0:
                self.c2_dense(l)
            else:
                self.c2_moe(l)
        P.barrier()
        return P.nc


def _consts():
    c = np.zeros((128, NCST), np.float32)
    i = np.arange(128)
    c[:, C_ID:C_ID + 128] = np.eye(128)
    c[:, C_TRI:C_TRI + 128] = (i[:, None] <= i[None, :])
    c[:, C_TRU:C_TRU + 128] = (i[:, None] >= i[None, :])
    c[:, C_SG:C_SG + 128] = (i[:, None] > i[None, :])
    c[:, C_SL:C_SL + 128] = (i[:, None] < i[None, :])
    c[:, C_ONE:C_ONE + 128] = 1.0
    for e in range(8):
        c[e, C_SEL + e * 128:C_SEL + (e + 1) * 128] = 1.0
    invc = np.ones((128, 2, WP), np.float32)
    wins = (2, 4, 8, 16)
    for g, win in enumerate(wins):
        for (L, off) in ((CTXL, 16), (SEQ, 304)):
            t = np.arange(L)
            lo = np.clip(t - win // 2, 0, L)
            hi = np.clip(t - win // 2 + win, 0, L)
            p0 = (g % 2) * 64
            invc[p0:p0 + 64, g // 2, off:off + L] = (1.0 / (hi - lo).astype(np.float32))[None, :]
    return c, invc.reshape(128, 2 * WP)


def _prep_shared(inp):
    f = lambda a: np.ascontiguousarray(np.asarray(a, dtype=np.float32))
    small = np.zeros((NL, 128, NSM), np.float32)
    ttab = np.zeros((NL, 128, 4, 15, 64), np.float32)
    cq = np.arange(64)
    sc = np.clip(cq - 8, 0, 48)
    kc = np.arange(64)
    valid = (kc[None, :] >= sc[:, None]) & (kc[None, :] < sc[:, None] + 16)
    idx = np.clip(kc[None, :] - cq[:, None] + 15, 0, 30)
    for l in range(NL):
        small[l, :, O_BADA:O_BADA + 48] = f(inp["b_ada"])[l].reshape(48, 128).T
        small[l, :, O_PSC:O_PSC + 2] = f(inp["pool_scale"])[l].reshape(2, 128).T
        cw = f(inp["conv_w"])[l]
        small[l, :, O_CW:O_CW + 6] = cw.reshape(3, 2, 128).transpose(2, 1, 0).reshape(128, 6)
        scw = f(inp["ssd_conv_w"])[l]
        small[l, :, O_SCW:O_SCW + 18] = scw.reshape(3, 6, 128).transpose(2, 1, 0).reshape(128, 18)
        small[l, :, O_SCB:O_SCB + 6] = f(inp["ssd_conv_b"])[l].reshape(6, 128).T
        small[l, :, O_DTB:O_DTB + 8] = f(inp["ssd_dt_bias"])[l].reshape(1, 8)
        small[l, :, O_ALOG:O_ALOG + 8] = f(inp["ssd_a_log"])[l].reshape(1, 8)
        small[l, :, O_DSK:O_DSK + 256] = np.repeat(f(inp["ssd_d"])[l], 64)[None, :]
        small[l, :, O_SNG:O_SNG + 256] = f(inp["ssd_norm_g"])[l][None, :]
        pw = f(inp["pool_w"])[l]
        blk = np.zeros((128, 2, 128), np.float32)
        for g in range(4):
            p0 = (g % 2) * 64
            blk[p0:p0 + 64, g // 2, p0:p0 + 64] = pw[g]
        small[l, :, O_PW:O_PW + 256] = blk.reshape(128, 256)
        rpb = f(inp["na_rpb"])[l]
        gath = rpb[:, :, idx]
        tab = np.where(valid[None, None], gath, np.float32(-30000.0))
        tab = tab.transpose(2, 0, 1, 3)
        ttab[l, 0:64] = tab
        ttab[l, 64:128] = tab
    gsm = np.zeros((128, 104), np.float32)
    gsm[:, 0:8] = f(inp["g_mix"])[0].reshape(8, 128).T
    gsm[:, 8:16] = f(inp["g_mix"])[1].reshape(8, 128).T
    gsm[:, 16:24] = f(inp["g_ffn"])[0].reshape(8, 128).T
    gsm[:, 24:32] = f(inp["g_ffn"])[1].reshape(8, 128).T
    gsm[:, 32:40] = f(inp["g_final"]).reshape(8, 128).T
    gsm[:, 40:104] = f(inp["moe_router"])[0].reshape(8, 128, 8).transpose(1, 0, 2).reshape(128, 64)
    cst, invc = _consts()
    return {
        "w_ada": f(inp["w_ada"]), "w_in": f(inp["w_in"]), "w_out": f(inp["w_out"]), "small": small,
        "ttab": ttab.reshape(NL, 128, 3840), "gsm": gsm, "cst": cst, "invc": invc,
        "ffn_gu": f(inp["ffn_w_gu"])[0], "ffn_down": f(inp["ffn_w_down"])[0],
        "moe_gu": f(inp["moe_w_gu"])[0], "moe_down": f(inp["moe_w_down"])[0],
    }


def _core_inputs(inp, shared, core):
    b0 = core * NB
    x = np.asarray(inp["x"], dtype=np.float32)
    c = np.asarray(inp["c"], dtype=np.float32)
    ctx = np.asarray(inp["ctx"], dtype=np.float32)
    cc = np.concatenate([c[b0:b0 + NB], np.asarray(inp["c_ctx"], dtype=np.float32)[None, :]], axis=0).T
    m = dict(shared)
    m["x"] = np.ascontiguousarray(x[b0:b0 + NB])
    m["ctx"] = np.ascontiguousarray(ctx[b0:b0 + NB])
    m["cc"] = np.ascontiguousarray(cc)
    return m


_NC_CACHE = {}


def kernel(**inputs):
    if "nc" not in _NC_CACHE:
        mdl = Model()
        _NC_CACHE["nc"] = mdl.build()
    nc = _NC_CACHE["nc"]
    shared = _prep_shared(inputs)
    in_maps = [_core_inputs(inputs, shared, core) for core in range(8)]
    res = run_bass_kernel_spmd(nc, in_maps, core_ids=list(range(8)))
    out = np.concatenate([np.asarray(r["out"], dtype=np.float32) for r in res.results], axis=0)
    return out
```

```python
import numpy as np
import concourse.bass as bass
import concourse.mybir as mybir

F32 = mybir.dt.float32
BF16 = mybir.dt.bfloat16
ALU = mybir.AluOpType
AF = mybir.ActivationFunctionType
AX = mybir.AxisListType


class T:
    def __init__(self, name, handle, space):
        self.name = name
        self.h = handle
        self.space = space
        self.state = {}

    def ap(self):
        return self.h.ap() if self.space == "dram" else self.h[:]

    def __getitem__(self, idx):
        return self.ap()[idx]


class Prog:
    N_DMA_SEMS = 48

    def __init__(self):
        self.nc = bass.Bass("TRN2", target_bir_lowering=False)
        nc = self.nc
        self.eng = {"pe": nc.tensor, "act": nc.scalar, "dve": nc.vector, "pool": nc.gpsimd, "sp": nc.sync}
        self.sem = {}
        self.cnt = {}
        self._cms = []
        for e in self.eng:
            cm = nc.semaphore("s_" + e)
            self.sem[e] = cm.__enter__()
            self._cms.append(cm)
            self.cnt[e] = 0
        self.dma_sems = []
        for i in range(self.N_DMA_SEMS):
            cm = nc.semaphore("d%d" % i)
            self.dma_sems.append(cm.__enter__())
            self._cms.append(cm)
        self.dma_cnt = [0] * self.N_DMA_SEMS
        self.dma_last = [None] * self.N_DMA_SEMS
        self.dma_i = 0
        self.seen = {e: {} for e in self.eng}
        self.n_ops = 0
        self.n_waits = 0
        self.out_deps = []

    def sbuf(self, name, shape, dtype):
        return T(name, self.nc.alloc_sbuf_tensor(name, list(shape), dtype), "sbuf")

    def psum(self, name, shape, dtype=F32):
        return T(name, self.nc.alloc_psum_tensor(name, list(shape), dtype), "psum")

    def dram(self, name, shape, dtype, kind="Internal"):
        return T(name, self.nc.dram_tensor(name, list(shape), dtype, kind=kind), "dram")

    @staticmethod
    def _norm(acc):
        out = []
        for a in acc:
            if isinstance(a, T):
                out.append((a, None))
            else:
                out.append((a[0], a[1]))
        return out

    def _collect(self, reads, writes):
        deps = []
        for t, k in self._norm(reads):
            keys = list(t.state.keys()) if k is None else [kk for kk in (k, None) if kk in t.state]
            for kk in keys:
                w = t.state[kk][0]
                if w is not None:
                    deps.append(w)
        for t, k in self._norm(writes):
            keys = list(t.state.keys()) if k is None else [kk for kk in (k, None) if kk in t.state]
            for kk in keys:
                w, rs = t.state[kk]
                if w is not None:
                    deps.append(w)
                deps.extend(rs.items())
        return deps

    def _update(self, reads, writes, me):
        for t, k in self._norm(reads):
            if k is None:
                if None not in t.state:
                    t.state[None] = [None, {}]
                for kk in t.state:
                    self._addr(t.state[kk][1], me)
            else:
                if k not in t.state:
                    w = t.state[None][0] if None in t.state else None
                    t.state[k] = [w, {}]
                self._addr(t.state[k][1], me)
        for t, k in self._norm(writes):
            if k is None:
                t.state = {None: [me, {}]}
            else:
                t.state[k] = [me, {}]
                if None in t.state:
                    self._addr(t.state[None][1], me)

    @staticmethod
    def _addr(d, me):
        if d.get(me[0], 0) < me[1]:
            d[me[0]] = me[1]

    def _emit_waits(self, e, deps):
        eng = self.eng[e]
        need = {}
        for d in deps:
            if d is None:
                continue
            key, val = d
            if need.get(key, 0) < val:
                need[key] = val
        for key, val in need.items():
            if self.seen[e].get(key, 0) >= val:
                continue
            if key == e and e == "pe":
                continue
            sem = self.sem[key] if isinstance(key, str) else self.dma_sems[key]
            eng.wait_ge(sem, val)
            self.n_waits += 1
            self.seen[e][key] = val

    @staticmethod
    def _excl(reads, writes):
        r2, w2 = [], []
        for a in reads:
            t = a if isinstance(a, T) else a[0]
            if t.space == "psum":
                w2.append(t)
            else:
                r2.append(a)
        for a in writes:
            t = a if isinstance(a, T) else a[0]
            w2.append(t if t.space == "psum" else a)
        return r2, w2

    def op(self, e, fn, reads=(), writes=()):
        reads, writes = self._excl(reads, writes)
        deps = self._collect(reads, writes)
        self._emit_waits(e, deps)
        ins = fn(self.eng[e])
        self.cnt[e] += 1
        ins.then_inc(self.sem[e], 1)
        me = (e, self.cnt[e])
        self._update(reads, writes, me)
        self.n_ops += 1
        return me

    def dma(self, out_ap, in_ap, reads=(), writes=(), q="sp", **kw):
        deps = self._collect(reads, writes)
        i = self.dma_i % self.N_DMA_SEMS
        self.dma_i += 1
        if self.dma_last[i] is not None:
            deps.append(self.dma_last[i])
        self._emit_waits(q, deps)
        ins = self.eng[q].dma_start(out=out_ap, in_=in_ap, **kw)
        self.dma_cnt[i] += 1
        ins.then_inc(self.dma_sems[i], 16)
        me = (i, 16 * self.dma_cnt[i])
        self.dma_last[i] = me
        self._update(reads, writes, me)
        self.n_ops += 1
        return me

    def finish(self, final_deps):
        self._emit_waits("sp", list(final_deps))
        return self.nc


def _mm(self, out, lhsT, rhs, start, stop, R, W):
    return self.op("pe", lambda e: e.matmul(out, lhsT=lhsT, rhs=rhs, start=start, stop=stop), R, W)


def _tr(self, out, in_, ident, R, W):
    return self.op("pe", lambda e: e.transpose(out=out, in_=in_, identity=ident), R, W)


def _act(self, out, in_, func, R, W, **kw):
    return self.op("act", lambda e: e.activation(out=out, in_=in_, func=func, **kw), R, W)


def _tt(self, eng, out, in0, in1, op, R, W):
    return self.op(eng, lambda e: e.tensor_tensor(out=out, in0=in0, in1=in1, op=op), R, W)


def _ts(self, eng, out, in0, s1, s2, op0, op1, R, W):
    if op1 is None:
        return self.op(eng, lambda e: e.tensor_scalar(out=out, in0=in0, scalar1=s1, scalar2=None, op0=op0), R, W)
    return self.op(eng, lambda e: e.tensor_scalar(out=out, in0=in0, scalar1=s1, scalar2=s2, op0=op0, op1=op1), R, W)


def _stt(self, eng, out, in0, scalar, in1, op0, op1, R, W):
    return self.op(eng, lambda e: e.scalar_tensor_tensor(out=out, in0=in0, scalar=scalar, in1=in1, op0=op0, op1=op1), R, W)


def _copy(self, eng, out, in_, R, W):
    if eng == "act":
        return self.op("act", lambda e: e.copy(out=out, in_=in_), R, W)
    return self.op(eng, lambda e: e.tensor_copy(out=out, in_=in_), R, W)


def _memset(self, eng, t_ap, val, W):
    return self.op(eng, lambda e: e.memset(t_ap, val), (), W)


def _phase_begin(self):
    import contextlib
    if not hasattr(self, "_stacks"):
        self._stacks = []
    self._stacks.append(contextlib.ExitStack())
    self._stack = self._stacks[-1]


def _psb(self, name, shape, dtype):
    self._uid = getattr(self, "_uid", 0) + 1
    name = "%s_%d" % (name, self._uid)
    h = self._stack.enter_context(self.nc.sbuf_tensor(name, list(shape), dtype))
    t = T(name, h, "sbuf")
    return t


def _barrier(self):
    deps = [(e, c) for e, c in self.cnt.items() if c > 0]
    deps += [d for d in self.dma_last if d is not None]
    for e in self.eng:
        self._emit_waits(e, [d for d in deps if d[0] != e])


def _phase_end(self):
    self.barrier()
    self._stacks.pop().close()
    self._stack = self._stacks[-1] if self._stacks else None


Prog.mm = _mm
Prog.tr = _tr
Prog.act = _act
Prog.tt = _tt
Prog.ts = _ts
Prog.stt = _stt
Prog.copy = _copy
Prog.memset = _memset
Prog.phase_begin = _phase_begin
Prog.psb = _psb
Prog.barrier = _barrier
Prog.phase_end = _phase_end

from concourse.bass_utils import run_bass_kernel_spmd

D = 1024
SEQ = 2048
CTXL = 256
TOK = 2304
NB = 4
NL = 2
DIN = 2824
WP = 2368
NCH = 18
TILES = [(0, 256)] + [(256 + i * 512, 512) for i in range(4)]
EPS = 1e-6
O_BADA, O_PSC, O_CW, O_SCW, O_SCB, O_DTB, O_ALOG, O_DSK, O_SNG, O_PW, NSM = 0, 48, 50, 56, 74, 80, 88, 96, 352, 608, 864
C_ID, C_TRI, C_TRU, C_SG, C_SL, C_ONE, C_SEL, NCST = 0, 128, 256, 384, 512, 640, 768, 1792


def pcol(t):
    return 16 + t if t < 256 else t + 48


def ccol(c):
    return pcol(c * 128)


def fm_view(dr, r0, nch, t0, n):
    return dr.ap()[r0:r0 + nch * 128, t0:t0 + n].rearrange("(m p) t -> p m t", p=128)


class Model:
    def __init__(self, dbg=False, nl=NL, stop_after=None):
        self.P = Prog()
        self.dbg = dbg
        self.nl = nl
        self.stop_after = stop_after
        P = self.P
        k_in = "ExternalInput"
        self.x_in = P.dram("x", [NB, SEQ, D], F32, k_in)
        self.ctx_in = P.dram("ctx", [NB, CTXL, D], F32, k_in)
        self.cc = P.dram("cc", [D, 5], F32, k_in)
        self.w_ada = P.dram("w_ada", [NL, D, 6 * D], F32, k_in)
        self.w_in = P.dram("w_in", [NL, D, DIN], F32, k_in)
        self.w_out = P.dram("w_out", [NL, D, D], F32, k_in)
        self.small = P.dram("small", [NL, 128, NSM], F32, k_in)
        self.ttab = P.dram("ttab", [NL, 128, 4 * 960], F32, k_in)
        self.gsm = P.dram("gsm", [128, 104], F32, k_in)
        self.cst = P.dram("cst", [128, NCST], F32, k_in)
        self.invc = P.dram("invc", [128, 2 * WP], F32, k_in)
        self.ffn_gu = P.dram("ffn_gu", [D, 5632], F32, k_in)
        self.ffn_down = P.dram("ffn_down", [2816, D], F32, k_in)
        self.moe_gu = P.dram("moe_gu", [8, D, 2816], F32, k_in)
        self.moe_down = P.dram("moe_down", [8, 1408, D], F32, k_in)
        self.out = P.dram("out", [NB, SEQ, D], F32, "ExternalOutput")
        dk = "ExternalOutput" if dbg else "Internal"
        self.w_in_b = [P.dram("w_in_b%d" % l, [D, DIN], BF16) for l in range(NL)]
        self.w_out_b = [P.dram("w_out_b%d" % l, [D, D], BF16) for l in range(NL)]
        self.ffn_gu_b = P.dram("ffn_gu_b", [D, 5632], BF16)
        self.ffn_down_b = P.dram("ffn_down_b", [2816, D], BF16)
        self.moe_gu_b = [P.dram("moe_gu_b%d" % e, [D, 2816], BF16) for e in range(8)]
        self.moe_down_b = [P.dram("moe_down_b%d" % e, [1408, D], BF16) for e in range(8)]
        self.xs = [P.dram("xs%d" % b, [D, TOK], F32, dk if b == 0 else "Internal") for b in range(NB)]
        self.pr = [P.dram("pr%d" % b, [2816, TOK], BF16, dk if b == 0 else "Internal") for b in range(NB)]
        self.vtm = [P.dram("vtm%d" % b, [TOK, 256], BF16, dk if b == 0 else "Internal") for b in range(NB)]
        self.dtm = [P.dram("dtm%d" % b, [8, TOK], F32, dk if b == 0 else "Internal") for b in range(NB)]
        self.o = [P.dram("o%d" % b, [D, TOK], BF16, dk if b == 0 else "Internal") for b in range(NB)]
        self.h2 = [P.dram("h2_%d" % b, [D, TOK], BF16, dk if b == 0 else "Internal") for b in range(NB)]
        self.gT = [P.dram("gT%d" % b, [8, SEQ], F32, dk if b == 0 else "Internal") for b in range(NB)]
        self.ps = [P.psum("ps%d" % i, [128, 512], F32) for i in range(6)]
        self.pb = [P.psum("pb%d" % i, [128, 1024], BF16) for i in range(2)]
        self.psi = 0
        self.cst_s = P.sbuf("cst_s", [128, NCST], F32)
        self.id_b = P.sbuf("id_b", [128, 128], BF16)
        self.one_b = P.sbuf("one_b", [128, 128], BF16)
        self.cst_b = P.sbuf("cst_b", [128, 640], BF16)
        self.sm_s = [P.sbuf("sm_s%d" % l, [128, NSM], F32) for l in range(NL)]
        self.gsm_s = P.sbuf("gsm_s", [128, 104], F32)
        self.s_t = P.sbuf("s_t", [128, 8, 5], F32)
        self.mod = P.sbuf("mod", [128, 48, 5], F32)
        self.G1 = P.sbuf("G1", [128, 8, 5], F32)
        self.G2 = P.sbuf("G2", [128, 8, 5], F32)
        self.pwb = P.sbuf("pwb", [128, 2, 128], BF16)

    def nps(self):
        p = self.ps[self.psi % 6]
        self.psi += 1
        return p

    def setup(self):
        P = self.P
        c = self.cst_s
        P.dma(c[:], self.cst[:], writes=[c])
        for l in range(NL):
            P.dma(self.sm_s[l][:], self.small[l], writes=[self.sm_s[l]])
        P.dma(self.gsm_s[:], self.gsm[:], writes=[self.gsm_s])
        P.copy("dve", self.id_b[:], c[:, C_ID:C_ID + 128], [c], [self.id_b])
        P.copy("dve", self.one_b[:], c[:, C_ONE:C_ONE + 128], [c], [self.one_b])
        P.copy("dve", self.cst_b[:], c[:, 0:640], [c], [self.cst_b])
        st = self.s_t
        P.dma(st[:], self.cc.ap().rearrange("(k p) j -> p k j", p=128), writes=[st])
        P.act(st[:], st[:], AF.Silu, [st], [st])

    def convert(self, layers):
        P = self.P
        P.phase_begin()
        f32b = [P.psb("cv32", [128, 4096], F32) for _ in range(3)]
        b16b = [P.psb("cv16", [128, 4096], BF16) for _ in range(3)]
        jobs = []

        def add(dst, src_ap):
            sv = src_ap.rearrange("(a p) n -> p a n", p=128)
            dv = dst.ap().rearrange("(a p) n -> p a n", p=128)
            A, N = sv.shape[1], sv.shape[2]
            if N <= 2048:
                ga = 4096 // N
                for a0 in range(0, A, ga):
                    g = min(ga, A - a0)
                    jobs.append((dst, sv[:, a0:a0 + g, :], dv[:, a0:a0 + g, :], g, N))
            else:
                for a0 in range(A):
                    for c0 in range(0, N, 4096):
                        n = min(4096, N - c0)
                        jobs.append((dst, sv[:, a0:a0 + 1, c0:c0 + n], dv[:, a0:a0 + 1, c0:c0 + n], 1, n))
        for l in layers:
            add(self.w_in_b[l], self.w_in[l])
        for i, (dst, sv, dv, g, n) in enumerate(jobs):
            fb, bb = f32b[i % 3], b16b[i % 3]
            fv = fb[:, 0:g * n].rearrange("p (a n) -> p a n", a=g)
            bv = bb[:, 0:g * n].rearrange("p (a n) -> p a n", a=g)
            P.dma(fv, sv, writes=[fb])
            P.copy("pool" if i % 2 == 0 else "act", bv, fv, [fb], [bb])
            P.dma(dv, bv, reads=[bb], writes=[dst])
        P.phase_end()

    def bg_jobs_init(self):
        jobs = []

        def add(dst, src_ap):
            sv = src_ap.rearrange("(a p) n -> p a n", p=128)
            dv = dst.ap().rearrange("(a p) n -> p a n", p=128)
            A, N = sv.shape[1], sv.shape[2]
            for a0 in range(A):
                for c0 in range(0, N, 1024):
                    n = min(1024, N - c0)
                    jobs.append((dst, sv[:, a0, c0:c0 + n], dv[:, a0, c0:c0 + n], n))
        add(self.w_out_b[0], self.w_out[0])
        add(self.ffn_gu_b, self.ffn_gu.ap())
        add(self.ffn_down_b, self.ffn_down.ap())
        self.bg_split = len(jobs)
        if self.nl > 1:
            add(self.w_in_b[1], self.w_in[1])
            add(self.w_out_b[1], self.w_out[1])
            for e in range(8):
                add(self.moe_gu_b[e], self.moe_gu[e])
                add(self.moe_down_b[e], self.moe_down[e])
        self.bg_jobs = jobs
        self.bg_i = 0

    def bg_step(self, k, f32b, b16b, limit=None):
        P = self.P
        limit = len(self.bg_jobs) if limit is None else limit
        for _ in range(k):
            if self.bg_i >= limit:
                return
            i = self.bg_i
            self.bg_i += 1
            dst, sv, dv, n = self.bg_jobs[i]
            fb, bb = f32b[i % len(f32b)], b16b[i % len(b16b)]
            P.dma(fb[:, 0:n], sv, writes=[fb], q="pool")
            P.copy("pool", bb[:, 0:n], fb[:, 0:n], [fb], [bb])
            P.dma(dv, bb[:, 0:n], reads=[bb], writes=[dst], q="pool")

    def adaln(self, l):
        P = self.P
        P.phase_begin()
        wa = [P.psb("wa", [128, 8, 1024], F32) for _ in range(2)]
        sm = self.sm_s[l]
        mod = self.mod

        def ld(part):
            P.dma(wa[part % 2][:], self.w_ada[l][:, part * 1024:(part + 1) * 1024].rearrange("(k p) n -> p k n", p=128),
                  writes=[wa[part % 2]])
        ld(0)
        for part in range(6):
            if part + 1 < 6:
                ld(part + 1)
            w = wa[part % 2]
            for fc in range(8):
                ci = part * 8 + fc
                ps = self.nps()
                for k in range(8):
                    P.mm(ps[:, 0:5], w[:, k, fc * 128:(fc + 1) * 128], self.s_t[:, k, :], k == 0, k == 7, [w, self.s_t], [ps])
                P.ts("dve", mod[:, ci, :], ps[:, 0:5], sm[:, O_BADA + ci:O_BADA + ci + 1], None, ALU.add, None,
                     [ps, sm], [(mod, ci)])
        g = self.gsm_s
        for k in range(8):
            P.ts("dve", self.G1[:, k, :], mod[:, 8 + k, :], 1.0, g[:, l * 8 + k:l * 8 + k + 1], ALU.add, ALU.mult,
                 [mod, g], [(self.G1, k)])
            P.ts("dve", self.G2[:, k, :], mod[:, 32 + k, :], 1.0, g[:, 16 + l * 8 + k:16 + l * 8 + k + 1], ALU.add, ALU.mult,
                 [mod, g], [(self.G2, k)])
        P.copy("dve", self.pwb[:], sm[:, O_PW:O_PW + 256].rearrange("p (c n) -> p c n", c=2), [sm], [self.pwb])
        P.phase_end()

    def norm_p1(self, xt, n, sq, rstd, tmp):
        P = self.P
        P.act(sq[:, :, :n], xt[:, :, :n], AF.Square, [xt], [sq])
        ps = self.nps()
        for k in range(8):
            P.mm(ps[:, :n], self.one_b[:], sq[:, k, :n], k == 0, k == 7, [self.one_b, sq], [ps])
        P.act(rstd[:, :n], ps[:, :n], AF.Ln, [ps], [rstd], scale=1.0 / D, bias=EPS)
        P.act(rstd[:, :n], rstd[:, :n], AF.Exp, [rstd], [rstd], scale=-0.5)
        P.tt("dve", tmp[:, :, :n], xt[:, :, :n], rstd[:, :n].unsqueeze(1).broadcast_to([128, 8, n]), ALU.mult, [xt, rstd], [tmp])

    def norm_p2(self, n, G, shoff, j, h_out, tmp, h32=None):
        P = self.P
        for k in range(8):
            if h32 is not None:
                P.act(h32[:, k, :n], tmp[:, k, :n], AF.Identity, [tmp, self.mod, G], [(h32, k)], scale=G[:, k, j:j + 1],
                      bias=self.mod[:, shoff + k, j:j + 1])
            else:
                P.act(h_out[:, k, :n], tmp[:, k, :n], AF.Identity, [tmp, self.mod, G], [(h_out, k)], scale=G[:, k, j:j + 1],
                      bias=self.mod[:, shoff + k, j:j + 1])
        if h32 is not None:
            P.copy("dve", h_out[:, :, :n], h32[:, :, :n], [h32], [h_out])

    def norm_mod(self, xt, n, G, shoff, j, h_out, sq, rstd, tmp, h32=None):
        self.norm_p1(xt, n, sq, rstd, tmp)
        self.norm_p2(n, G, shoff, j, h_out, tmp, h32)

    def phaseA(self, l):
        P = self.P
        P.phase_begin()
        cs = self.cst_s
        w = P.psb("w_in_s", [128, 8, DIN], BF16)
        P.dma(w[:], self.w_in_b[l].ap().rearrange("(k p) n -> p k n", p=128), reads=[self.w_in_b[l]], writes=[w])
        xt2 = [P.psb("xt", [128, 8, 512], F32) for _ in range(2)]
        xtm = [P.psb("xtm", [128, 1024], F32) for _ in range(2)] if l == 0 else None
        sq2 = [P.psb("sq", [128, 8, 512], BF16) for _ in range(2)]
        rstd2 = [P.psb("rstd", [128, 512], F32) for _ in range(2)]
        tmp = P.psb("tmp", [128, 8, 512], F32)
        h2 = [P.psb("h", [128, 8, 512], BF16) for _ in range(2)]
        stg = [P.psb("stg", [128, 22, 512], BF16) for _ in range(1)]
        vst = P.psb("vst", [128, 4, 256], BF16)
        dst = P.psb("dst", [8, 512], F32)
        tiles = [(b, t0, n) for b in range(NB) for (t0, n) in TILES]
        if l == 0:
            bgf = [P.psb("bgf", [128, 1024], F32) for _ in range(2)]
            bgb = [P.psb("bgb", [128, 1024], BF16) for _ in range(2)]

        def load(i):
            b, t0, n = tiles[i]
            xt = xt2[i % 2]
            if l == 0:
                for s_ in range(n // 128):
                    xm = xtm[s_ % 2]
                    src = self.ctx_in[b, s_ * 128:(s_ + 1) * 128, :] if t0 == 0 else \
                        self.x_in[b, t0 - 256 + s_ * 128:t0 - 256 + (s_ + 1) * 128, :]
                    P.dma(xm[:], src, writes=[xm])
                    for hf in range(2):
                        ps = self.nps()
                        for kk in range(4):
                            k = hf * 4 + kk
                            P.tr(ps[:, kk * 128:(kk + 1) * 128], xm[:, k * 128:(k + 1) * 128], cs[:, C_ID:C_ID + 128],
                                 [xm, cs], [ps])
                        dst_ap = xt[:, hf * 4:(hf + 1) * 4, s_ * 128:(s_ + 1) * 128]
                        src_ap = ps[:, :].rearrange("p (k t) -> p k t", k=4)
                        P.copy("dve", dst_ap, src_ap, [ps], [xt])
                P.dma(fm_view(self.xs[b], 0, 8, t0, n), xt[:, :, :n], reads=[xt], writes=[self.xs[b]])
            else:
                P.dma(xt[:, :, :n], fm_view(self.xs[b], 0, 8, t0, n), reads=[self.xs[b]], writes=[xt])

        def norm(i):
            b, t0, n = tiles[i]
            j = 4 if t0 == 0 else b
            self.norm_mod(xt2[i % 2], n, self.G1, 0, j, h2[i % 2], sq2[i % 2], rstd2[i % 2], tmp)

        def proj(i):
            b, t0, n = tiles[i]
            h = h2[i % 2]
            st = stg[0]
            ns = n // 128
            if l == 0:
                self.bg_step(5, bgf, bgb, limit=self.bg_split)
            for m in range(22):
                ps = self.nps()
                for k in range(8):
                    P.mm(ps[:, :n], w[:, k, m * 128:(m + 1) * 128], h[:, k, :n], k == 0, k == 7, [w, h], [ps])
                if m % 3 == 0:
                    P.copy("act", st[:, m, :n], ps[:, :n], [ps], [(st, m)])
                else:
                    P.copy("dve", st[:, m, :n], ps[:, :n], [ps], [(st, m)])
            P.dma(fm_view(self.pr[b], 0, 22, t0, n), st[:, :, :n], reads=[st], writes=[self.pr[b]])
            for s_ in range(ns):
                ps = self.nps()
                for k in range(8):
                    P.mm(ps[:, 0:256], h[:, k, s_ * 128:(s_ + 1) * 128], w[:, k, 1536:1792], k == 0, k == 7, [w, h], [ps])
                P.copy("dve", vst[:, s_, :], ps[:, 0:256], [ps], [(vst, s_)])
            P.dma(self.vtm[b].ap()[t0:t0 + n, :].rearrange("(s p) c -> p s c", p=128), vst[:, :ns, :],
                  reads=[vst], writes=[self.vtm[b]])
            psd = self.nps()
            for k in range(8):
                P.mm(psd[0:8, :n], w[:, k, 2816:2824], h[:, k, :n], k == 0, k == 7, [w, h], [psd])
            P.copy("dve", dst[:, :n], psd[0:8, :n], [psd], [dst])
            P.dma(self.dtm[b].ap()[:, t0:t0 + n], dst[:, :n], reads=[dst], writes=[self.dtm[b]])

        NT = len(tiles)
        load(0)
        norm(0)
        load(1)
        for i in range(NT):
            if i + 1 < NT:
                norm(i + 1)
            if i + 2 < NT:
                load(i + 2)
            proj(i)
        if l == 0:
            self.bg_step(10 ** 6, bgf, bgb, limit=self.bg_split)
        P.phase_end()

    def zero_pads(self, t):
        P = self.P
        for (c0, c1) in ((0, 16), (272, 304), (2352, WP)):
            P.memset("dve", t[:, :, c0:c1], 0.0, [t])

    def load_padded(self, t, ch, dr, row0, R=None):
        P = self.P
        P.dma(t[:, ch, 16:272], dr.ap()[row0:row0 + 128, 0:256], reads=[dr], writes=[t])
        P.dma(t[:, ch, 304:2352], dr.ap()[row0:row0 + 128, 256:2304], reads=[dr], writes=[t])

    def store_padded(self, dr, row0, src_t, ch):
        P = self.P
        P.dma(dr.ap()[row0:row0 + 128, 0:256], src_t[:, ch, 16:272], reads=[src_t], writes=[dr])
        P.dma(dr.ap()[row0:row0 + 128, 256:2304], src_t[:, ch, 304:2352], reads=[src_t], writes=[dr])

    def m1(self, l, b):
        P = self.P
        sm = self.sm_s[l]
        P.phase_begin()
        invc = P.psb("invc", [128, 2, WP], F32)
        P.dma(invc[:], self.invc.ap().rearrange("p (c w) -> p c w", c=2), writes=[invc])
        up = P.psb("up", [128, 2, WP], BF16)
        self.zero_pads(up)
        for ch in range(2):
            self.load_padded(up, ch, self.pr[b], ch * 128)
        z = P.psb("z", [128, 2, WP], F32)
        y = P.psb("y", [128, 2, WP], BF16)
        A = P.psb("A", [128, WP], F32)
        B = P.psb("B", [128, WP], F32)
        C = P.psb("C", [128, WP], F32)
        Dd = P.psb("Dd", [128, WP], F32)
        tp = P.psb("tp", [128, WP], F32)
        W = WP
        for ch in range(2):
            for c0 in range(0, W, 512):
                n = min(512, W - c0)
                ps = self.nps()
                P.mm(ps[:, :n], self.pwb[:, ch, :], up[:, ch, c0:c0 + n], True, True, [self.pwb, up], [ps])
                P.copy("act", z[:, ch, c0:c0 + n], ps[:, :n], [ps], [z])
            zc = z[:, ch, :]
            P.tt("dve", A[:, 4:W - 4], zc[:, 3:W - 5], zc[:, 4:W - 4], ALU.add, [z], [A])
            if ch == 0:
                P.tt("dve", B[64:128, 6:W - 6], A[64:128, 5:W - 7], A[64:128, 7:W - 5], ALU.add, [A], [B])
                S0, S1 = A, B
            else:
                P.tt("dve", B[:, 6:W - 6], A[:, 5:W - 7], A[:, 7:W - 5], ALU.add, [A], [B])
                P.tt("dve", C[:, 8:W - 8], B[:, 6:W - 10], B[:, 10:W - 6], ALU.add, [B], [C])
                P.tt("dve", Dd[64:128, 12:W - 12], C[64:128, 8:W - 16], C[64:128, 16:W - 8], ALU.add, [C], [Dd])
                S0, S1 = C, Dd
            for (p0, p1, S) in ((0, 64, S0), (64, 128, S1)):
                P.tt("dve", tp[p0:p1, 16:W - 16], S[p0:p1, 16:W - 16], invc[p0:p1, ch, 16:W - 16], ALU.mult, [S, invc], [tp])
                P.tt("dve", tp[p0:p1, 16:W - 16], tp[p0:p1, 16:W - 16], zc[p0:p1, 16:W - 16], ALU.subtract, [tp, z], [tp])
                P.act(y[p0:p1, ch, 16:W - 16], tp[p0:p1, 16:W - 16], AF.Identity, [tp, sm], [y],
                      scale=sm[p0:p1, O_PSC + ch:O_PSC + ch + 1])
            self.store_padded(self.o[b], ch * 128, y, ch)
        hp = P.psb("hp", [128, 2, WP], BF16)
        bp = P.psb("bp", [128, 2, WP], BF16)
        cp = P.psb("cp", [128, 2, WP], BF16)
        for t in (hp, bp, cp):
            self.zero_pads(t)
        y2 = P.psb("y2", [128, 2, WP], BF16)
        for ch in range(2):
            self.load_padded(hp, ch, self.pr[b], 256 + ch * 128)
            self.load_padded(bp, ch, self.pr[b], 512 + ch * 128)
            self.load_padded(cp, ch, self.pr[b], 768 + ch * 128)
        mC = P.psb("mC", [128, WP], F32)
        accC = P.psb("accC", [128, WP], F32)
        for ch in range(2):
            m, acc = mC, accC
            cw = lambda tap: sm[:, O_CW + ch * 3 + tap:O_CW + ch * 3 + tap + 1]
            P.tt("dve", m[:], cp[:, ch, :], hp[:, ch, :], ALU.mult, [cp, hp], [m])
            P.ts("dve", acc[:, 1:W - 1], m[:, 0:W - 2], cw(0), None, ALU.mult, None, [m, sm], [acc])
            P.stt("dve", acc[:, 1:W - 1], m[:, 1:W - 1], cw(1), acc[:, 1:W - 1], ALU.mult, ALU.add, [m, sm, acc], [acc])
            P.stt("dve", acc[:, 1:W - 1], m[:, 2:W], cw(2), acc[:, 1:W - 1], ALU.mult, ALU.add, [m, sm, acc], [acc])
            P.tt("dve", y2[:, ch, 1:W - 1], bp[:, ch, 1:W - 1], acc[:, 1:W - 1], ALU.mult, [bp, acc], [y2])
            self.store_padded(self.o[b], 256 + ch * 128, y2, ch)
        P.phase_end()

    def m2(self, l, b, ctx_out):
        P = self.P
        sm = self.sm_s[l]
        cs = self.cst_s
        W = WP
        P.phase_begin()
        xp = P.psb("xp", [128, 6, WP], BF16)
        self.zero_pads(xp)
        for ch in range(6):
            self.load_padded(xp, ch, self.pr[b], 2048 + ch * 128)
        xc = P.psb("xc", [128, 6, WP], BF16)
        acc = [P.psb("acc", [128, WP], F32) for _ in range(2)]
        for ch in range(6):
            a = acc[ch % 2]
            cw = lambda tap: sm[:, O_SCW + ch * 3 + tap:O_SCW + ch * 3 + tap + 1]
            P.ts("dve", a[:, 1:W - 1], xp[:, ch, 0:W - 2], cw(0), None, ALU.mult, None, [xp, sm], [a])
            P.stt("dve", a[:, 1:W - 1], xp[:, ch, 1:W - 1], cw(1), a[:, 1:W - 1], ALU.mult, ALU.add, [xp, sm, a], [a])
            P.stt("dve", a[:, 1:W - 1], xp[:, ch, 2:W], cw(2), a[:, 1:W - 1], ALU.mult, ALU.add, [xp, sm, a], [a])
            P.act(xc[:, ch, 1:W - 1], a[:, 1:W - 1], AF.Silu, [a, sm], [(xc, ch)], bias=sm[:, O_SCB + ch:O_SCB + ch + 1])
        zf = P.psb("zf", [128, 2, TOK], BF16)
        P.dma(zf[:], fm_view(self.pr[b], 1792, 2, 0, TOK), reads=[self.pr[b]], writes=[zf])
        P.act(zf[:], zf[:], AF.Silu, [zf], [zf])
        XBZ = P.psb("XBZ", [128, NCH, 768], BF16)
        for c in range(NCH):
            pbk = self.pb[c % 2]
            c0 = ccol(c)
            for i in range(4):
                P.tr(pbk[:, i * 128:(i + 1) * 128], xc[:, i, c0:c0 + 128], self.id_b[:], [(xc, i), self.id_b], [pbk])
            for i in range(2):
                P.tr(pbk[:, 512 + i * 128:512 + (i + 1) * 128], zf[:, i, c * 128:(c + 1) * 128], self.id_b[:], [zf, self.id_b], [pbk])
            P.copy("dve" if c % 2 else "act", XBZ[:, c, :], pbk[:, 0:768], [pbk], [(XBZ, c)])
        def t3(name):
            return P.psb(name, [128, NCH, 8], F32)
        dt, la, acum, tot, eac, wdec, dA = [t3(n) for n in ("dt", "la", "acum", "tot", "eac", "wdec", "dA")]
        ea = P.psb("ea", [128, 8], F32)
        dtf = P.psb("dtf", [8, TOK], F32)
        P.dma(dtf[:], self.dtm[b].ap(), reads=[self.dtm[b]], writes=[dtf])
        psq = self.nps()
        for c in range(NCH):
            P.tr(psq[:, c * 8:(c + 1) * 8], dtf[0:8, c * 128:(c + 1) * 128], cs[0:8, C_ID:C_ID + 8], [dtf, cs], [psq])
        P.copy("dve", dt[:].rearrange("p c j -> p (c j)"), psq[:, 0:144], [psq], [dt])
        bc = lambda off: sm[:, off:off + 8].unsqueeze(1).broadcast_to([128, NCH, 8])
        P.tt("dve", dt[:], dt[:], bc(O_DTB), ALU.add, [dt, sm], [dt])
        P.act(dt[:], dt[:], AF.Exp, [dt], [dt])
        P.act(dt[:], dt[:], AF.Ln, [dt], [dt], bias=1.0)
        P.act(ea[:], sm[:, O_ALOG:O_ALOG + 8], AF.Exp, [sm], [ea])
        P.stt("dve", la[:], dt[:], -1.0, ea[:, 0:8].unsqueeze(1).broadcast_to([128, NCH, 8]), ALU.mult, ALU.mult, [dt, ea], [la])
        laf = la[:].rearrange("p c j -> p (c j)")
        psA, psB, psT = self.nps(), self.nps(), self.nps()
        P.mm(psA[:, 0:144], cs[:, C_TRI:C_TRI + 128], laf, True, True, [cs, la], [psA])
        P.mm(psB[:, 0:144], cs[:, C_TRU:C_TRU + 128], laf, True, True, [cs, la], [psB])
        P.mm(psT[:, 0:144], cs[:, C_ONE:C_ONE + 128], laf, True, True, [cs, la], [psT])
        v3 = lambda ps: ps[:, 0:144].rearrange("p (c j) -> p c j", j=8)
        P.copy("dve", acum[:, :, 0:4], v3(psA)[:, :, 0:4], [psA], [acum])
        P.copy("dve", acum[:, :, 4:8], v3(psB)[:, :, 4:8], [psB], [acum])
        P.copy("dve", tot[:], v3(psT), [psT], [tot])
        P.act(eac[:], acum[:], AF.Exp, [acum], [eac])
        P.act(dA[:], tot[:], AF.Exp, [tot], [dA])
        P.tt("dve", wdec[:], tot[:], acum[:], ALU.subtract, [tot, acum], [wdec])
        P.act(wdec[:], wdec[:], AF.Exp, [wdec], [wdec])
        H32 = P.psb("H32", [128, 8, 64], F32)
        Hb = P.psb("Hb", [128, 8, 64], BF16)
        P.memset("dve", H32[:], 0.0, [H32])
        P.memset("dve", Hb[:], 0.0, [Hb])
        ytot = P.psb("ytot", [128, NCH, 256], F32)
        for c in range(NCH):
            P.tt("pool", ytot[:, c, :], XBZ[:, c, 0:256], sm[:, O_DSK:O_DSK + 256], ALU.mult, [(XBZ, c), sm], [(ytot, c)])
        Gm = [[P.psb("Gm", [128, 128], F32) for _ in range(2)] for _ in range(2)]
        LD = [P.psb("LD", [128, 128], F32) for _ in range(2)]
        E = [P.psb("E", [128, 128], F32) for _ in range(2)]
        M = [P.psb("M", [128, 128], BF16) for _ in range(2)]
        xdt = [P.psb("xdt", [128, 64], BF16) for _ in range(2)]
        xdw = [P.psb("xdw", [128, 64], BF16) for _ in range(2)]
        y1 = [P.psb("y1", [128, 64], F32) for _ in range(2)]
        y2 = [P.psb("y2", [128, 64], F32) for _ in range(2)]
        it = 0
        for d in range(2):
            order = list(range(NCH)) if d == 0 else [1, 0] + list(range(NCH - 1, 1, -1))
            cmask = C_TRI if d == 0 else C_TRU
            cstr = C_SG if d == 0 else C_SL
            for c in order:
                need_y = ctx_out or c >= 2
                c0 = ccol(c)
                for g in range(2):
                    gm = Gm[it % 2][g]
                    if need_y:
                        psG = self.nps()
                        P.mm(psG[:, 0:128], xc[:, 2 + g, c0:c0 + 128], xc[:, 4 + g, c0:c0 + 128], True, True, [xc], [psG])
                        P.tt("dve", gm[:], psG[:, 0:128], cs[:, cmask:cmask + 128], ALU.mult, [psG, cs], [gm])
                    for hh in range(2):
                        h = 2 * g + hh
                        j = d * 4 + h
                        i2 = it % 2
                        it += 1
                        if need_y:
                            P.ts("pool", LD[i2][:], cs[:, cstr:cstr + 128], la[:, c, j:j + 1], None, ALU.mult, None, [cs, la], [LD[i2]])
                            psD = self.nps()
                            P.mm(psD[:, 0:128], LD[i2][:], cs[:, cmask:cmask + 128], True, True, [LD[i2], cs], [psD])
                            P.act(E[i2][:], psD[:, 0:128], AF.Exp, [psD], [E[i2]])
                            P.tt("dve", M[i2][:], gm[:], E[i2][:], ALU.mult, [gm, E[i2]], [M[i2]])
                        P.ts("pool", xdt[i2][:], XBZ[:, c, h * 64:(h + 1) * 64], dt[:, c, j:j + 1], None, ALU.mult, None,
                             [(XBZ, c), dt], [xdt[i2]])
                        P.ts("pool", xdw[i2][:], xdt[i2][:], wdec[:, c, j:j + 1], None, ALU.mult, None, [xdt[i2], wdec], [xdw[i2]])
                        if need_y:
                            psY = self.nps()
                            P.mm(psY[:, 0:64], M[i2][:], xdt[i2][:], True, True, [M[i2], xdt[i2]], [psY])
                            psI = self.nps()
                            P.mm(psI[:, 0:64], xc[:, 4 + g, c0:c0 + 128], Hb[:, j, :], True, True, [xc, (Hb, j)], [psI])
                            P.copy("act", y1[i2][:], psY[:, 0:64], [psY], [y1[i2]])
                            P.stt("dve", y2[i2][:], psI[:, 0:64], eac[:, c, j:j + 1], y1[i2][:], ALU.mult, ALU.add,
                                  [psI, eac, y1[i2]], [y2[i2]])
                            P.tt("pool", ytot[:, c, h * 64:(h + 1) * 64], ytot[:, c, h * 64:(h + 1) * 64], y2[i2][:], ALU.add,
                                 [(ytot, c), y2[i2]], [(ytot, c)])
                        psS = self.nps()
                        P.mm(psS[:, 0:64], XBZ[:, c, 256 + g * 128:256 + (g + 1) * 128], xdw[i2][:], True, True,
                             [(XBZ, c), xdw[i2]], [psS])
                        P.stt("dve", H32[:, j, :], H32[:, j, :], dA[:, c, j:j + 1], psS[:, 0:64], ALU.mult, ALU.add,
                              [(H32, j), dA, psS], [(H32, j)])
                        P.copy("act", Hb[:, j, :], H32[:, j, :], [(H32, j)], [(Hb, j)])
        ofm = P.psb("ofm", [128, 2, TOK], BF16)
        gte = [P.psb("gte", [128, 256], F32) for _ in range(2)]
        junk = P.psb("junk", [128, 256], F32)
        ssq = [P.psb("ssq", [128, 1], F32) for _ in range(2)]
        ob = [P.psb("ob", [128, 256], BF16) for _ in range(2)]
        cstart = 0 if ctx_out else 2
        for c in range(cstart, NCH):
            i2 = c % 2
            P.tt("dve", gte[i2][:], ytot[:, c, :], XBZ[:, c, 512:768], ALU.mult, [(ytot, c), (XBZ, c)], [gte[i2]])
            P.act(junk[:], gte[i2][:], AF.Square, [gte[i2]], [junk, ssq[i2]], accum_out=ssq[i2][:])
            P.act(ssq[i2][:], ssq[i2][:], AF.Sqrt, [ssq[i2]], [ssq[i2]], scale=1.0 / 256, bias=EPS)
            P.op("dve", lambda e: e.reciprocal(out=ssq[i2][:], in_=ssq[i2][:]), [ssq[i2]], [ssq[i2]])
            P.stt("dve", ob[i2][:], gte[i2][:], ssq[i2][:, 0:1], sm[:, O_SNG:O_SNG + 256], ALU.mult, ALU.mult,
                  [gte[i2], ssq[i2], sm], [ob[i2]])
            pbk = self.pb[c % 2]
            for i in range(2):
                P.tr(pbk[:, i * 128:(i + 1) * 128], ob[i2][:, i * 128:(i + 1) * 128], self.id_b[:], [ob[i2], self.id_b], [pbk])
            P.copy("act", ofm[:, :, c * 128:(c + 1) * 128], pbk[:, 0:256].rearrange("p (i t) -> p i t", i=2), [pbk], [ofm])
        t0 = cstart * 128
        P.dma(fm_view(self.o[b], 768, 2, t0, TOK - t0), ofm[:, :, t0:TOK], reads=[ofm], writes=[self.o[b]])
        P.phase_end()

    def m3(self, l, b, ctx_out):
        P = self.P
        P.phase_begin()
        q = P.psb("q", [128, 2, TOK], BF16)
        k = P.psb("k", [128, 2, TOK], BF16)
        v = P.psb("v", [128, NCH, 256], BF16)
        vs = P.psb("vs", [128, NCH - 1, 256], BF16)
        P.dma(q[:], fm_view(self.pr[b], 1024, 2, 0, TOK), reads=[self.pr[b]], writes=[q])
        P.dma(k[:], fm_view(self.pr[b], 1280, 2, 0, TOK), reads=[self.pr[b]], writes=[k])
        P.dma(v[:], self.vtm[b].ap().rearrange("(c p) d -> p c d", p=128), reads=[self.vtm[b]], writes=[v])
        P.dma(vs[:], self.vtm[b].ap()[64:64 + (NCH - 1) * 128, :].rearrange("(c p) d -> p c d", p=128),
              reads=[self.vtm[b]], writes=[vs])
        t32 = P.psb("t32", [128, 3840], F32)
        T8 = P.psb("T8", [128, 4, 960], BF16)
        P.dma(t32[:], self.ttab[l], writes=[t32])
        P.act(T8[:].rearrange("p h n -> p (h n)"), t32[:], AF.Copy, [t32], [T8], scale=8.0)
        pT = [P.psb("pT", [128, 6, 64], BF16) for _ in range(2)]
        rec = [P.psb("rec", [64, 512], F32) for _ in range(2)]
        obf = [P.psb("obf", [64, 512], BF16) for _ in range(2)]
        it = 0
        for h in range(4):
            p0 = (h % 2) * 64
            ch = h // 2
            if ctx_out:
                psS = self.nps()
                for jc in range(2):
                    P.mm(psS[:, jc * 256:(jc + 1) * 256], k[p0:p0 + 64, ch, jc * 128:(jc + 1) * 128], q[p0:p0 + 64, ch, 0:256],
                         True, True, [k, q], [psS])
                pt2 = P.psb("pt2", [128, 512], BF16)
                P.act(pt2[:], psS[:, :], AF.Exp, [psS], [pt2], scale=0.125)
                pn, pd = self.nps(), self.nps()
                for jc in range(2):
                    P.mm(pn[0:64, 0:256], v[:, jc, h * 64:(h + 1) * 64], pt2[:, jc * 256:(jc + 1) * 256], jc == 0, jc == 1, [v, pt2], [pn])
                for jc in range(2):
                    P.mm(pd[0:64, 0:256], self.one_b[:, 0:64], pt2[:, jc * 256:(jc + 1) * 256], jc == 0, jc == 1, [self.one_b, pt2], [pd])
                i2 = it % 2
                it += 1
                P.op("dve", lambda e: e.reciprocal(out=rec[i2][:, 0:256], in_=pd[0:64, 0:256]), [pd], [rec[i2]])
                P.tt("dve", obf[i2][:, 0:256], pn[0:64, 0:256], rec[i2][:, 0:256], ALU.mult, [pn, rec[i2]], [obf[i2]])
                P.dma(self.o[b].ap()[512 + h * 64:512 + (h + 1) * 64, 0:256], obf[i2][:, 0:256], reads=[obf[i2]], writes=[self.o[b]])
            for r0 in range(0, 32, 8):
                gi = (r0 // 8) % 2
                pn, pd = self.ps[gi * 2], self.ps[gi * 2 + 1]
                for r in range(r0, r0 + 8):
                    sr = min(max(r - 4, 0), 24)
                    psS = self.ps[4 + r % 2]
                    qs = q[p0:p0 + 64, ch, 256 + r * 64:256 + (r + 1) * 64]
                    kts = []
                    for jc in range(6):
                        kt0 = 256 + (sr + 2 * jc) * 64 if jc < 4 else (jc - 4) * 128
                        kts.append(kt0)
                        P.mm(psS[:, jc * 64:(jc + 1) * 64], k[p0:p0 + 64, ch, kt0:kt0 + 128], qs, True, jc >= 4, [k, q], [psS])
                        if jc < 4:
                            off = (sr - r + 7 + 2 * jc) * 64
                            P.mm(psS[:, jc * 64:(jc + 1) * 64], T8[p0:p0 + 64, h, off:off + 128], self.id_b[p0:p0 + 64, p0:p0 + 64],
                                 False, True, [T8, self.id_b], [psS])
                    pt = pT[r % 2]
                    P.act(pt[:].rearrange("p a b -> p (a b)"), psS[:, 0:384], AF.Exp, [psS], [pt], scale=0.125)
                    col = (r - r0) * 64
                    for jc in range(6):
                        kt0 = kts[jc]
                        if kt0 % 128 == 0:
                            vv = v[:, kt0 // 128, h * 64:(h + 1) * 64]
                        else:
                            vv = vs[:, (kt0 - 64) // 128, h * 64:(h + 1) * 64]
                        P.mm(pn[0:64, col:col + 64], vv, pt[:, jc, :], jc == 0, jc == 5, [v, vs, pt], [pn])
                    for jc in range(6):
                        P.mm(pd[0:64, col:col + 64], self.one_b[:, 0:64], pt[:, jc, :], jc == 0, jc == 5, [self.one_b, pt], [pd])
                i2 = it % 2
                it += 1
                P.op("dve", lambda e: e.reciprocal(out=rec[i2][:], in_=pd[0:64, :]), [pd], [rec[i2]])
                P.tt("dve", obf[i2][:], pn[0:64, :], rec[i2][:], ALU.mult, [pn, rec[i2]], [obf[i2]])
                P.dma(self.o[b].ap()[512 + h * 64:512 + (h + 1) * 64, 256 + r0 * 64:256 + r0 * 64 + 512], obf[i2][:],
                      reads=[obf[i2]], writes=[self.o[b]])
        P.phase_end()

    def m23(self, l, b, ctx_out):
        P = self.P
        sm = self.sm_s[l]
        cs = self.cst_s
        W = WP
        P.phase_begin()
        xc = P.psb("xc", [128, 6, WP], BF16)
        XBZ = P.psb("XBZ", [128, NCH, 768], BF16)
        T8 = P.psb("T8", [128, 4, 960], BF16)
        P.phase_begin()
        xp = P.psb("xp", [128, 6, WP], BF16)
        self.zero_pads(xp)
        for ch in range(6):
            self.load_padded(xp, ch, self.pr[b], 2048 + ch * 128)
        zf = P.psb("zf", [128, 2, TOK], BF16)
        P.dma(zf[:], fm_view(self.pr[b], 1792, 2, 0, TOK), reads=[self.pr[b]], writes=[zf])
        t32 = P.psb("t32", [128, 3840], F32)
        P.dma(t32[:], self.ttab[l], writes=[t32])
        acc = [P.psb("acc", [128, WP], F32) for _ in range(2)]
        for ch in range(6):
            a = acc[ch % 2]
            cw = lambda tap: sm[:, O_SCW + ch * 3 + tap:O_SCW + ch * 3 + tap + 1]
            eng = "dve"
            P.ts(eng, a[:, 1:W - 1], xp[:, ch, 0:W - 2], cw(0), None, ALU.mult, None, [xp, sm], [a])
            P.stt(eng, a[:, 1:W - 1], xp[:, ch, 1:W - 1], cw(1), a[:, 1:W - 1], ALU.mult, ALU.add, [xp, sm, a], [a])
            P.stt(eng, a[:, 1:W - 1], xp[:, ch, 2:W], cw(2), a[:, 1:W - 1], ALU.mult, ALU.add, [xp, sm, a], [a])
            P.act(xc[:, ch, 1:W - 1], a[:, 1:W - 1], AF.Silu, [a, sm], [(xc, ch)], bias=sm[:, O_SCB + ch:O_SCB + ch + 1])
        P.act(zf[:], zf[:], AF.Silu, [zf], [zf])
        P.act(T8[:].rearrange("p h n -> p (h n)"), t32[:], AF.Copy, [t32], [T8], scale=8.0)
        for c in range(NCH):
            pbk = self.pb[c % 2]
            c0 = ccol(c)
            for i in range(4):
                P.tr(pbk[:, i * 128:(i + 1) * 128], xc[:, i, c0:c0 + 128], self.id_b[:], [(xc, i), self.id_b], [pbk])
            for i in range(2):
                P.tr(pbk[:, 512 + i * 128:512 + (i + 1) * 128], zf[:, i, c * 128:(c + 1) * 128], self.id_b[:], [zf, self.id_b], [pbk])
            P.copy("dve" if c % 2 else "act", XBZ[:, c, :], pbk[:, 0:768], [pbk], [(XBZ, c)])
        P.phase_end()
        q = P.psb("q", [128, 2, TOK], BF16)
        k = P.psb("k", [128, 2, TOK], BF16)
        v = P.psb("v", [128, NCH, 256], BF16)
        vs = P.psb("vs", [128, NCH - 1, 256], BF16)
        P.dma(q[:], fm_view(self.pr[b], 1024, 2, 0, TOK), reads=[self.pr[b]], writes=[q])
        P.dma(k[:], fm_view(self.pr[b], 1280, 2, 0, TOK), reads=[self.pr[b]], writes=[k])
        P.dma(v[:], self.vtm[b].ap().rearrange("(c p) d -> p c d", p=128), reads=[self.vtm[b]], writes=[v])
        P.dma(vs[:], self.vtm[b].ap()[64:64 + (NCH - 1) * 128, :].rearrange("(c p) d -> p c d", p=128),
              reads=[self.vtm[b]], writes=[vs])

        def t3(name):
            return P.psb(name, [128, NCH, 8], F32)
        dt, la, acum, tot, eac, wdec, dA, dtw = [t3(n) for n in ("dt", "la", "acum", "tot", "eac", "wdec", "dA", "dtw")]
        ea = P.psb("ea", [128, 8], F32)
        dtf = P.psb("dtf", [8, TOK], F32)
        P.dma(dtf[:], self.dtm[b].ap(), reads=[self.dtm[b]], writes=[dtf])
        psq = self.ps[4]
        for c in range(NCH):
            P.tr(psq[:, c * 8:(c + 1) * 8], dtf[0:8, c * 128:(c + 1) * 128], cs[0:8, C_ID:C_ID + 8], [dtf, cs], [psq])
        P.copy("dve", dt[:].rearrange("p c j -> p (c j)"), psq[:, 0:144], [psq], [dt])
        bc = lambda off: sm[:, off:off + 8].unsqueeze(1).broadcast_to([128, NCH, 8])
        P.tt("dve", dt[:], dt[:], bc(O_DTB), ALU.add, [dt, sm], [dt])
        P.act(dt[:], dt[:], AF.Exp, [dt], [dt])
        P.act(dt[:], dt[:], AF.Ln, [dt], [dt], bias=1.0)
        P.act(ea[:], sm[:, O_ALOG:O_ALOG + 8], AF.Exp, [sm], [ea])
        P.stt("dve", la[:], dt[:], -1.0, ea[:, 0:8].unsqueeze(1).broadcast_to([128, NCH, 8]), ALU.mult, ALU.mult, [dt, ea], [la])
        la_hb = P.psb("la_hb", [128, NCH, 8], BF16)
        la_hi = t3("la_hi")
        la_lo = t3("la_lo")
        P.copy("dve", la_hb[:], la[:], [la], [la_hb])
        P.copy("dve", la_hi[:], la_hb[:], [la_hb], [la_hi])
        P.tt("dve", la_lo[:], la[:], la_hi[:], ALU.subtract, [la, la_hi], [la_lo])
        laf = la[:].rearrange("p c j -> p (c j)")
        psA, psB, psT = self.ps[4], self.ps[5], self.ps[0]
        P.mm(psA[:, 0:144], cs[:, C_TRI:C_TRI + 128], laf, True, True, [cs, la], [psA])
        P.mm(psB[:, 0:144], cs[:, C_TRU:C_TRU + 128], laf, True, True, [cs, la], [psB])
        P.mm(psT[:, 0:144], cs[:, C_ONE:C_ONE + 128], laf, True, True, [cs, la], [psT])
        v3 = lambda ps: ps[:, 0:144].rearrange("p (c j) -> p c j", j=8)
        P.copy("dve", acum[:, :, 0:4], v3(psA)[:, :, 0:4], [psA], [acum])
        P.copy("dve", acum[:, :, 4:8], v3(psB)[:, :, 4:8], [psB], [acum])
        P.copy("dve", tot[:], v3(psT), [psT], [tot])
        P.act(eac[:], acum[:], AF.Exp, [acum], [eac])
        P.act(dA[:], tot[:], AF.Exp, [tot], [dA])
        P.tt("dve", wdec[:], tot[:], acum[:], ALU.subtract, [tot, acum], [wdec])
        P.act(wdec[:], wdec[:], AF.Exp, [wdec], [wdec])
        P.tt("dve", dtw[:], dt[:], wdec[:], ALU.mult, [dt, wdec], [dtw])
        H32 = P.psb("H32", [128, 8, 64], F32)
        Hb = P.psb("Hb", [128, 8, 64], BF16)
        P.memset("dve", H32[:], 0.0, [H32])
        P.memset("dve", Hb[:], 0.0, [Hb])
        ytot = P.psb("ytot", [128, NCH, 256], F32)
        P.tt("dve", ytot[:], XBZ[:, :, 0:256], sm[:, O_DSK:O_DSK + 256].unsqueeze(1).broadcast_to([128, NCH, 256]), ALU.mult,
             [XBZ, sm], [ytot])
        NBUF = 6
        Gm = [P.psb("Gm", [128, 128], F32) for _ in range(4)]
        LDf = [P.psb("LDf", [128, 128], F32) for _ in range(NBUF)]
        E = [P.psb("E", [128, 128], F32) for _ in range(NBUF)]
        M = [P.psb("M", [128, 128], BF16) for _ in range(NBUF)]
        xdw = [P.psb("xdw", [128, 64], BF16) for _ in range(NBUF)]
        subs = []
        gcount = 0
        for d in range(2):
            order = list(range(NCH)) if d == 0 else [1, 0] + list(range(NCH - 1, 1, -1))
            for c in order:
                for g in range(2):
                    for hh in range(2):
                        subs.append(dict(d=d, c=c, g=g, hh=hh, h=2 * g + hh, j=d * 4 + 2 * g + hh, gi=gcount,
                                         need_y=(ctx_out or c >= 2), c0=ccol(c),
                                         cmask=(C_TRI if d == 0 else C_TRU), cstr=(C_SG if d == 0 else C_SL)))
                    gcount += 1
        NS = len(subs)
        bank = {}
        _nps = self.nps

        def nps_safe():
            for _ in range(12):
                p = _nps()
                if all(p is not q_ for q_ in bank.values()):
                    return p
            raise RuntimeError("no free PSUM bank")

        def S0(i):
            u = subs[i]; c, g, h, j, c0 = u["c"], u["g"], u["h"], u["j"], u["c0"]; ib = i % NBUF
            if u["need_y"]:
                if u["hh"] == 0:
                    bG = nps_safe()
                    gm = Gm[u["gi"] % 4]
                    P.mm(bG[:, 0:128], xc[:, 2 + g, c0:c0 + 128], xc[:, 4 + g, c0:c0 + 128], True, True, [xc], [bG])
                    P.tt("dve", gm[:], bG[:, 0:128], cs[:, u["cmask"]:u["cmask"] + 128], ALU.mult, [bG, cs], [gm])
                P.act(LDf[ib][:], cs[:, u["cstr"]:u["cstr"] + 128], AF.Identity, [cs, la], [LDf[ib]], scale=la[:, c, j:j + 1])
            P.act(xdw[ib][:], XBZ[:, c, h * 64:(h + 1) * 64], AF.Identity, [(XBZ, c), dtw], [xdw[ib]], scale=dtw[:, c, j:j + 1])

        def S1(i):
            u = subs[i]; c, g, h, j, c0 = u["c"], u["g"], u["h"], u["j"], u["c0"]; ib = i % NBUF
            if u["need_y"]:
                bD = nps_safe()
                P.mm(bD[:, 0:128], LDf[ib][:], cs[:, u["cmask"]:u["cmask"] + 128], True, True, [LDf[ib], cs], [bD])
                bI = nps_safe()
                P.mm(bI[:, 0:64], xc[:, 4 + g, c0:c0 + 128], Hb[:, j, :], True, True, [xc, (Hb, j)], [bI])
                bank[(i, "D")] = bD
                bank[(i, "I")] = bI
            bS = nps_safe()
            P.mm(bS[:, 0:64], XBZ[:, c, 256 + g * 128:256 + (g + 1) * 128], xdw[ib][:], True, True, [(XBZ, c), xdw[ib]], [bS])
            bank[(i, "S")] = bS

        def S2(i):
            u = subs[i]; c, g, h, j = u["c"], u["g"], u["h"], u["j"]; ib = i % NBUF
            ysl = ytot[:, c, h * 64:(h + 1) * 64]
            if u["need_y"]:
                bD = bank.pop((i, "D"))
                bI = bank.pop((i, "I"))
                P.act(E[ib][:], bD[:, 0:128], AF.Exp, [bD], [E[ib]])
                P.stt("dve", ysl, bI[:, 0:64], eac[:, c, j:j + 1], ysl, ALU.mult, ALU.add, [bI, eac, (ytot, c)], [(ytot, c)])
            bS = bank.pop((i, "S"))
            P.stt("dve", H32[:, j, :], H32[:, j, :], dA[:, c, j:j + 1], bS[:, 0:64], ALU.mult, ALU.add,
                  [(H32, j), dA, bS], [(H32, j)])
            P.copy("pool", Hb[:, j, :], H32[:, j, :], [(H32, j)], [(Hb, j)])

        def S3(i):
            u = subs[i]; c, h, j = u["c"], u["h"], u["j"]; ib = i % NBUF
            if u["need_y"]:
                gm = Gm[u["gi"] % 4]
                P.stt("dve", M[ib][:], gm[:], dt[:, c, j:j + 1], E[ib][:], ALU.mult, ALU.mult, [gm, dt, E[ib]], [M[ib]])
                bY = nps_safe()
                P.mm(bY[:, 0:64], M[ib][:], XBZ[:, c, h * 64:(h + 1) * 64], True, True, [M[ib], (XBZ, c)], [bY])
                bank[(i, "Y")] = bY

        def S4(i):
            u = subs[i]; c, h = u["c"], u["h"]
            if u["need_y"]:
                ysl = ytot[:, c, h * 64:(h + 1) * 64]
                bY = bank.pop((i, "Y"))
                P.tt("dve", ysl, bY[:, 0:64], ysl, ALU.add, [bY, (ytot, c)], [(ytot, c)])

        def ssd_gen():
            stages = [S0, S1, S2, S3, S4]
            for t in range(NS + len(stages) - 1):
                for k in reversed(range(len(stages))):
                    i = t - k
                    if 0 <= i < NS:
                        stages[k](i)
                yield

        pT = [P.psb("pT", [128, 6, 64], BF16) for _ in range(3)]
        rec = [P.psb("rec", [64, 512], F32) for _ in range(2)]
        obf = [P.psb("obf", [64, 512], BF16) for _ in range(2)]
        pt2 = P.psb("pt2", [128, 512], BF16) if ctx_out else None

        def na_gen():
            it = 0
            for h in range(4):
                p0 = (h % 2) * 64
                ch = h // 2
                if ctx_out:
                    psS, pn, pd = self.ps[4], self.ps[0], self.ps[1]
                    for jc in range(2):
                        P.mm(psS[:, jc * 256:(jc + 1) * 256], k[p0:p0 + 64, ch, jc * 128:(jc + 1) * 128], q[p0:p0 + 64, ch, 0:256],
                             True, True, [k, q], [psS])
                    P.act(pt2[:], psS[:, :], AF.Exp, [psS], [pt2], scale=0.125)
                    for jc in range(2):
                        P.mm(pn[0:64, 0:256], v[:, jc, h * 64:(h + 1) * 64], pt2[:, jc * 256:(jc + 1) * 256], jc == 0, jc == 1, [v, pt2], [pn])
                    for jc in range(2):
                        P.mm(pd[0:64, 0:256], self.one_b[:, 0:64], pt2[:, jc * 256:(jc + 1) * 256], jc == 0, jc == 1, [self.one_b, pt2], [pd])
                    i2 = it % 2
                    it += 1
                    P.op("dve", lambda e: e.reciprocal(out=rec[i2][:, 0:256], in_=pd[0:64, 0:256]), [pd], [rec[i2]])
                    P.tt("dve", obf[i2][:, 0:256], pn[0:64, 0:256], rec[i2][:, 0:256], ALU.mult, [pn, rec[i2]], [obf[i2]])
                    P.dma(self.o[b].ap()[512 + h * 64:512 + (h + 1) * 64, 0:256], obf[i2][:, 0:256], reads=[obf[i2]], writes=[self.o[b]])
                    yield
                def scores(r):
                    sr = min(max(r - 4, 0), 24)
                    psS = self.ps[4 + r % 2]
                    qs = q[p0:p0 + 64, ch, 256 + r * 64:256 + (r + 1) * 64]
                    kts = []
                    for jc in range(6):
                        kt0 = 256 + (sr + 2 * jc) * 64 if jc < 4 else (jc - 4) * 128
                        kts.append(kt0)
                        P.mm(psS[:, jc * 64:(jc + 1) * 64], k[p0:p0 + 64, ch, kt0:kt0 + 128], qs, True, jc >= 4, [k, q], [psS])
                        if jc < 4:
                            off = (sr - r + 7 + 2 * jc) * 64
                            P.mm(psS[:, jc * 64:(jc + 1) * 64], T8[p0:p0 + 64, h, off:off + 128], self.id_b[p0:p0 + 64, p0:p0 + 64],
                                 False, True, [T8, self.id_b], [psS])
                    pt = pT[r % 3]
                    P.act(pt[:].rearrange("p a b -> p (a b)"), psS[:, 0:384], AF.Exp, [psS], [pt], scale=0.125)
                    return kts

                def pv(r, kts):
                    nonlocal it
                    r0 = (r // 8) * 8
                    gi_ = (r0 // 8) % 2
                    pn, pd = self.ps[gi_ * 2], self.ps[gi_ * 2 + 1]
                    pt = pT[r % 3]
                    col = (r - r0) * 64
                    for jc in range(6):
                        kt0 = kts[jc]
                        if kt0 % 128 == 0:
                            vv = v[:, kt0 // 128, h * 64:(h + 1) * 64]
                        else:
                            vv = vs[:, (kt0 - 64) // 128, h * 64:(h + 1) * 64]
                        P.mm(pn[0:64, col:col + 64], vv, pt[:, jc, :], jc == 0, jc == 5, [v, vs, pt], [pn])
                    for jc in range(6):
                        P.mm(pd[0:64, col:col + 64], self.one_b[:, 0:64], pt[:, jc, :], jc == 0, jc == 5, [self.one_b, pt], [pd])
                    if r == r0 + 7:
                        i2 = it % 2
                        it += 1
                        P.op("dve", lambda e: e.reciprocal(out=rec[i2][:], in_=pd[0:64, :]), [pd], [rec[i2]])
                        P.tt("dve", obf[i2][:], pn[0:64, :], rec[i2][:], ALU.mult, [pn, rec[i2]], [obf[i2]])
                        P.dma(self.o[b].ap()[512 + h * 64:512 + (h + 1) * 64, 256 + r0 * 64:256 + r0 * 64 + 512], obf[i2][:],
                              reads=[obf[i2]], writes=[self.o[b]])

                prev = None
                for r in range(33):
                    cur = scores(r) if r < 32 else None
                    if prev is not None:
                        pv(r - 1, prev)
                    prev = cur
                    yield

        import os
        g1, g2 = ssd_gen(), na_gen()
        alive = [g1, g2]
        if os.environ.get("K_M23") == "nossd":
            alive = [g2]
        if os.environ.get("K_M23") == "nona":
            alive = [g1]
        for g in alive:
            for _ in g:
                pass
        ofm = P.psb("ofm", [128, 2, TOK], BF16)
        junk = P.psb("junk", [128, 256], F32)
        ssq = P.psb("ssq", [128, NCH], F32)
        obA = P.psb("obA", [128, NCH, 256], BF16)
        cstart = 0 if ctx_out else 2
        ncc = NCH - cstart
        P.tt("dve", ytot[:, cstart:, :], ytot[:, cstart:, :], XBZ[:, cstart:, 512:768], ALU.mult, [ytot, XBZ], [ytot])
        for c in range(cstart, NCH):
            P.act(junk[:], ytot[:, c, :], AF.Square, [ytot], [junk, (ssq, c)], accum_out=ssq[:, c:c + 1])
        P.act(ssq[:, cstart:], ssq[:, cstart:], AF.Ln, [ssq], [ssq], scale=1.0 / 256, bias=EPS)
        P.act(ssq[:, cstart:], ssq[:, cstart:], AF.Exp, [ssq], [ssq], scale=-0.5)
        P.tt("dve", ytot[:, cstart:, :], ytot[:, cstart:, :], ssq[:, cstart:].unsqueeze(2).broadcast_to([128, ncc, 256]), ALU.mult,
             [ytot, ssq], [ytot])
        P.tt("dve", obA[:, cstart:, :], ytot[:, cstart:, :], sm[:, O_SNG:O_SNG + 256].unsqueeze(1).broadcast_to([128, ncc, 256]),
             ALU.mult, [ytot, sm], [obA])
        gi = 0
        for c4 in range(cstart, NCH, 4):
            cs_ = list(range(c4, min(c4 + 4, NCH)))
            pbk = self.pb[gi % 2]
            gi += 1
            for ci, c in enumerate(cs_):
                for i in range(2):
                    P.tr(pbk[:, (i * 4 + ci) * 128:(i * 4 + ci + 1) * 128], obA[:, c, i * 128:(i + 1) * 128], self.id_b[:],
                         [obA, self.id_b], [pbk])
            nn = len(cs_)
            P.copy("act" if gi % 2 else "dve", ofm[:, :, c4 * 128:(c4 + nn) * 128],
                   pbk[:, :].rearrange("p (i t) -> p i t", i=2)[:, :, 0:nn * 128], [pbk], [ofm])
        t0 = cstart * 128
        P.dma(fm_view(self.o[b], 768, 2, t0, TOK - t0), ofm[:, :, t0:TOK], reads=[ofm], writes=[self.o[b]])
        P.phase_end()

    def c1(self, l):
        P = self.P
        cs = self.cst_s
        moe = (l % 2 == 1)
        last = (l == NL - 1)
        P.phase_begin()
        wo = P.psb("wo", [128, 8, D], BF16)
        P.dma(wo[:], self.w_out_b[l].ap().rearrange("(k p) n -> p k n", p=128), reads=[self.w_out_b[l]], writes=[wo])
        xt2 = [P.psb("xt", [128, 8, 512], F32) for _ in range(2)]
        ot2 = [P.psb("ot", [128, 8, 512], BF16) for _ in range(2)]
        sq = P.psb("sq", [128, 8, 512], BF16)
        rstd = P.psb("rstd", [128, 512], F32)
        tmp = P.psb("tmp", [128, 8, 512], F32)
        hb2 = [P.psb("hb", [128, 8, 512], BF16) for _ in range(2)]
        h32 = P.psb("h32", [128, 8, 512], F32) if moe else None
        if moe:
            wr = self.gsm_s
            lg = P.psb("lg", [128, 32], F32)
            lg2 = P.psb("lg2", [128, 32], F32)
            eq1 = P.psb("eq1", [128, 32], F32)
            eq2 = P.psb("eq2", [128, 32], F32)
            gt = P.psb("gt", [128, 32], F32)
            m1 = P.psb("m1", [128, 4], F32)
            m2 = P.psb("m2", [128, 4], F32)
            dd = P.psb("dd", [128, 4], F32)
            ee = P.psb("ee", [128, 4], F32)
            g1 = P.psb("g1", [128, 4], F32)
            g2 = P.psb("g2", [128, 4], F32)
            gts = P.psb("gts", [8, 512], F32)
        tiles = [(b, t0, n) for b in range(NB) for (t0, n) in TILES if not (t0 == 0 and last)]
        do_bg = (l == 0 and self.nl > 1)
        if do_bg:
            bgf = [P.psb("bgf", [128, 1024], F32) for _ in range(2)]
            bgb = [P.psb("bgb", [128, 1024], BF16) for _ in range(2)]

        def ldc(i):
            b, t0, n = tiles[i]
            P.dma(xt2[i % 2][:, :, :n], fm_view(self.xs[b], 0, 8, t0, n), reads=[self.xs[b]], writes=[xt2[i % 2]])
            P.dma(ot2[i % 2][:, :, :n], fm_view(self.o[b], 0, 8, t0, n), reads=[self.o[b]], writes=[ot2[i % 2]])

        def outproj(i):
            b, t0, n = tiles[i]
            j = 4 if t0 == 0 else b
            xt, ot = xt2[i % 2], ot2[i % 2]
            for m in range(8):
                ps = self.nps()
                for k in range(8):
                    P.mm(ps[:, :n], wo[:, k, m * 128:(m + 1) * 128], ot[:, k, :n], k == 0, k == 7, [wo, ot], [ps])
                P.stt("dve", xt[:, m, :n], ps[:, :n], self.mod[:, 16 + m, j:j + 1], xt[:, m, :n], ALU.mult, ALU.add,
                      [ps, self.mod, (xt, m)], [(xt, m)])
            P.dma(fm_view(self.xs[b], 0, 8, t0, n), xt[:, :, :n], reads=[xt], writes=[self.xs[b]])

        def norm1(i):
            b, t0, n = tiles[i]
            self.norm_p1(xt2[i % 2], n, sq, rstd, tmp)

        def norm2(i):
            b, t0, n = tiles[i]
            j = 4 if t0 == 0 else b
            hb = hb2[i % 2]
            self.norm_p2(n, self.G2, 24, j, hb, tmp, h32)
            P.dma(fm_view(self.h2[b], 0, 8, t0, n), hb[:, :, :n], reads=[hb], writes=[self.h2[b]])
            if moe:
                ns = n // 128
                ps = self.nps()
                for s_ in range(ns):
                    for k in range(8):
                        P.mm(ps[:, s_ * 8:(s_ + 1) * 8], h32[:, k, s_ * 128:(s_ + 1) * 128], wr[:, 40 + k * 8:40 + (k + 1) * 8],
                             k == 0, k == 7, [h32, wr], [ps])
                bc8 = lambda t: t[:, 0:ns].unsqueeze(2).broadcast_to([128, ns, 8])
                L3 = lambda t: t[:, 0:ns * 8].rearrange("p (s e) -> p s e", e=8)
                P.copy("dve", lg[:, 0:ns * 8], ps[:, 0:ns * 8], [ps], [lg])
                P.op("dve", lambda e: e.reduce_max(out=m1[:, 0:ns], in_=L3(lg), axis=AX.X), [lg], [m1])
                P.tt("dve", L3(eq1), L3(lg), bc8(m1), ALU.is_equal, [lg, m1], [eq1])
                P.stt("dve", lg2[:, 0:ns * 8], eq1[:, 0:ns * 8], -1e30, lg[:, 0:ns * 8], ALU.mult, ALU.add, [eq1, lg], [lg2])
                P.op("dve", lambda e: e.reduce_max(out=m2[:, 0:ns], in_=L3(lg2), axis=AX.X), [lg2], [m2])
                P.tt("dve", L3(eq2), L3(lg2), bc8(m2), ALU.is_equal, [lg2, m2], [eq2])
                P.tt("dve", dd[:, 0:ns], m2[:, 0:ns], m1[:, 0:ns], ALU.subtract, [m1, m2], [dd])
                P.act(ee[:, 0:ns], dd[:, 0:ns], AF.Exp, [dd], [ee])
                P.ts("dve", g1[:, 0:ns], ee[:, 0:ns], 1.0, None, ALU.add, None, [ee], [g1])
                P.op("dve", lambda e: e.reciprocal(out=g1[:, 0:ns], in_=g1[:, 0:ns]), [g1], [g1])
                P.tt("dve", g2[:, 0:ns], ee[:, 0:ns], g1[:, 0:ns], ALU.mult, [ee, g1], [g2])
                P.tt("dve", L3(gt), L3(eq1), bc8(g1), ALU.mult, [eq1, g1], [gt])
                P.tt("dve", L3(eq2), L3(eq2), bc8(g2), ALU.mult, [eq2, g2], [eq2])
                P.tt("dve", gt[:, 0:ns * 8], gt[:, 0:ns * 8], eq2[:, 0:ns * 8], ALU.add, [gt, eq2], [gt])
                pst = self.nps()
                for s_ in range(ns):
                    P.tr(pst[0:8, s_ * 128:(s_ + 1) * 128], gt[:, s_ * 8:(s_ + 1) * 8], cs[:, C_ID:C_ID + 128], [gt, cs], [pst])
                P.copy("act", gts[:, 0:n], pst[0:8, 0:n], [pst], [gts])
                P.dma(self.gT[b].ap()[:, t0 - 256:t0 - 256 + n], gts[:, :n], reads=[gts], writes=[self.gT[b]])

        NT = len(tiles)
        ldc(0)
        outproj(0)
        if NT > 1:
            ldc(1)
        for i in range(NT):
            if do_bg:
                self.bg_step(4, bgf, bgb)
            norm1(i)
            if i + 1 < NT:
                outproj(i + 1)
            norm2(i)
            if i + 2 < NT:
                ldc(i + 2)
        P.phase_end()

    def final_out(self, b, t0, n, xt, yf, sq, rstd, otm):
        P = self.P
        cs = self.cst_s
        g = self.gsm_s
        P.act(sq[:, :, :n], xt[:, :, :n], AF.Square, [xt], [sq])
        ps = self.nps()
        for k in range(8):
            P.mm(ps[:, :n], self.one_b[:], sq[:, k, :n], k == 0, k == 7, [self.one_b, sq], [ps])
        P.act(rstd[:, :n], ps[:, :n], AF.Sqrt, [ps], [rstd], scale=1.0 / D, bias=EPS)
        P.op("dve", lambda e: e.reciprocal(out=rstd[:, :n], in_=rstd[:, :n]), [rstd], [rstd])
        for k in range(8):
            P.stt("dve", yf[:, k, :n], xt[:, k, :n], g[:, 32 + k:33 + k], rstd[:, :n], ALU.mult, ALU.mult, [(xt, k), g, rstd], [(yf, k)])
        for s in range(n // 128):
            ot = otm[s % 2]
            for hf in range(2):
                ps = self.nps()
                for kk in range(4):
                    k = hf * 4 + kk
                    P.tr(ps[:, kk * 128:(kk + 1) * 128], yf[:, k, s * 128:(s + 1) * 128], cs[:, C_ID:C_ID + 128], [(yf, k), cs], [ps])
                P.copy("act" if hf == 0 else "dve", ot[:, hf * 512:(hf + 1) * 512], ps[:, :], [ps], [ot])
            tt0 = t0 - 256 + s * 128
            d = P.dma(self.out[b, tt0:tt0 + 128, :], ot[:], reads=[ot], writes=[self.out])
            self.out_deps.append(d)

    def c2_dense(self, l):
        P = self.P
        last = (l == NL - 1)
        P.phase_begin()
        wg = P.psb("wg", [128, 8, 5632], BF16)
        wd = P.psb("wd", [128, 22, D], BF16)
        P.dma(wg[:, 0:4, :], self.ffn_gu_b.ap()[0:512, :].rearrange("(k p) n -> p k n", p=128), reads=[self.ffn_gu_b], writes=[wg])
        P.dma(wg[:, 4:8, :], self.ffn_gu_b.ap()[512:1024, :].rearrange("(k p) n -> p k n", p=128), reads=[self.ffn_gu_b], writes=[wg])
        P.dma(wd[:], self.ffn_down_b.ap().rearrange("(m p) n -> p m n", p=128), reads=[self.ffn_down_b], writes=[wd])
        NT = 256
        xt2 = [P.psb("xt", [128, 8, NT], F32) for _ in range(2)]
        hb2 = [P.psb("hb", [128, 8, NT], BF16) for _ in range(2)]
        a = P.psb("a", [128, 22, NT], BF16)
        sg = [P.psb("sg", [128, NT], F32) for _ in range(2)]
        bgf = [P.psb("bgf", [128, 1024], F32) for _ in range(2)]
        bgb = [P.psb("bgb", [128, 1024], BF16) for _ in range(2)]
        it = 0
        for b in range(NB):
            for t0 in range(0, TOK, NT):
                if t0 == 0 and last:
                    continue
                n = NT
                if self.nl > 1:
                    self.bg_step(7, bgf, bgb)
                j = 4 if t0 == 0 else b
                xt, hb = xt2[it % 2], hb2[it % 2]
                it += 1
                P.dma(xt[:], fm_view(self.xs[b], 0, 8, t0, n), reads=[self.xs[b]], writes=[xt])
                P.dma(hb[:], fm_view(self.h2[b], 0, 8, t0, n), reads=[self.h2[b]], writes=[hb])
                for m in range(22):
                    pg, pu = self.nps(), self.nps()
                    for k in range(8):
                        P.mm(pg[:, :n], wg[:, k, m * 128:(m + 1) * 128], hb[:, k, :], k == 0, k == 7, [wg, hb], [pg])
                    for k in range(8):
                        P.mm(pu[:, :n], wg[:, k, 2816 + m * 128:2816 + (m + 1) * 128], hb[:, k, :], k == 0, k == 7, [wg, hb], [pu])
                    s_ = sg[m % 2]
                    P.act(s_[:], pg[:, :n], AF.Silu, [pg], [s_])
                    P.tt("dve", a[:, m, :], pu[:, :n], s_[:], ALU.mult, [pu, s_], [(a, m)])
                for f in range(8):
                    ps = self.nps()
                    for m in range(22):
                        P.mm(ps[:, :n], wd[:, m, f * 128:(f + 1) * 128], a[:, m, :], m == 0, m == 21, [wd, a], [ps])
                    P.stt("dve", xt[:, f, :], ps[:, :n], self.mod[:, 40 + f, j:j + 1], xt[:, f, :], ALU.mult, ALU.add,
                          [ps, self.mod, (xt, f)], [(xt, f)])
                P.dma(fm_view(self.xs[b], 0, 8, t0, n), xt[:], reads=[xt], writes=[self.xs[b]])
        if self.nl > 1:
            self.bg_step(10 ** 6, bgf, bgb)
        P.phase_end()

    def c2_moe(self, l):
        P = self.P
        cs = self.cst_s
        last = (l == NL - 1)
        P.phase_begin()
        wgu = [P.psb("wgu", [128, 8, 2, 768], BF16) for _ in range(2)]
        wdn = [P.psb("wdn", [128, 6, D], BF16) for _ in range(2)]
        hb2 = [P.psb("hb", [128, 8, 512], BF16) for _ in range(2)]
        xt = P.psb("xt", [128, 8, 512], F32)
        acc = P.psb("acc", [128, 8, 512], F32)
        gbc = P.psb("gbc", [128, 8, 512], F32)
        gts = P.psb("gts", [8, 512], F32)
        a2 = [P.psb("a", [128, 6, 512], BF16) for _ in range(2)]
        sg = [P.psb("sg", [128, 512], F32) for _ in range(2)]
        sg2 = [P.psb("sg2", [128, 512], F32) for _ in range(2)]
        if last:
            sq = P.psb("sq", [128, 8, 512], BF16)
            rstd = P.psb("rstd", [128, 512], F32)
            otm = [P.psb("otm", [128, D], F32) for _ in range(2)]
        tiles = [(b, t0) for b in range(NB) for t0 in range(256, TOK, 512)]
        units = [(ti, u) for ti in range(len(tiles)) for u in range(16)]

        def load_unit(ui):
            ti, u = units[ui]
            e, half = u // 2, u % 2
            nm = 6 if half == 0 else 5
            wgt, wdt = wgu[ui % 2], wdn[ui % 2]
            for gu in range(2):
                c0 = gu * 1408 + half * 768
                P.dma(wgt[:, :, gu, 0:nm * 128], self.moe_gu_b[e].ap()[:, c0:c0 + nm * 128].rearrange("(k p) n -> p k n", p=128),
                      reads=[self.moe_gu_b[e]], writes=[wgt])
            P.dma(wdt[:, 0:nm, :], self.moe_down_b[e].ap()[half * 768:half * 768 + nm * 128, :].rearrange("(m p) n -> p m n", p=128),
                  reads=[self.moe_down_b[e]], writes=[wdt])

        def load_tile(ti):
            b, t0 = tiles[ti]
            P.dma(hb2[ti % 2][:], fm_view(self.h2[b], 0, 8, t0, 512), reads=[self.h2[b]], writes=[hb2[ti % 2]])

        load_unit(0)
        load_tile(0)
        ui = 0
        for ti, (b, t0) in enumerate(tiles):
            n = 512
            hb = hb2[ti % 2]
            if ti + 1 < len(tiles):
                load_tile(ti + 1)
            P.dma(xt[:], fm_view(self.xs[b], 0, 8, t0, n), reads=[self.xs[b]], writes=[xt])
            P.dma(gts[:], self.gT[b].ap()[:, t0 - 256:t0 - 256 + n], reads=[self.gT[b]], writes=[gts])
            for e in range(8):
                ps = self.nps()
                P.mm(ps[:, :n], cs[0:8, C_SEL + e * 128:C_SEL + (e + 1) * 128], gts[:, :n], True, True, [cs, gts], [ps])
                P.copy("act", gbc[:, e, :], ps[:, :n], [ps], [(gbc, e)])
            for u in range(16):
                if ui + 1 < len(units):
                    load_unit(ui + 1)
                e, half = u // 2, u % 2
                nm = 6 if half == 0 else 5
                wgt, wdt = wgu[ui % 2], wdn[ui % 2]
                a = a2[ui % 2]
                ui += 1
                for mi in range(nm):
                    pg, pu = self.nps(), self.nps()
                    for k in range(8):
                        P.mm(pg[:, :n], wgt[:, k, 0, mi * 128:(mi + 1) * 128], hb[:, k, :], k == 0, k == 7, [wgt, hb], [pg])
                    for k in range(8):
                        P.mm(pu[:, :n], wgt[:, k, 1, mi * 128:(mi + 1) * 128], hb[:, k, :], k == 0, k == 7, [wgt, hb], [pu])
                    s_, s2_ = sg[mi % 2], sg2[mi % 2]
                    P.act(s_[:], pg[:, :n], AF.Silu, [pg], [s_])
                    P.tt("pool", s2_[:], s_[:], gbc[:, e, :], ALU.mult, [s_, (gbc, e)], [s2_])
                    P.tt("dve", a[:, mi, :], pu[:, :n], s2_[:], ALU.mult, [pu, s2_], [(a, mi)])
                for f in range(8):
                    ps = self.nps()
                    for mi in range(nm):
                        P.mm(ps[:, :n], wdt[:, mi, f * 128:(f + 1) * 128], a[:, mi, :], mi == 0, mi == nm - 1, [wdt, a], [ps])
                    if u == 0:
                        P.copy("act", acc[:, f, :], ps[:, :n], [ps], [(acc, f)])
                    else:
                        P.tt("dve", acc[:, f, :], acc[:, f, :], ps[:, :n], ALU.add, [ps, (acc, f)], [(acc, f)])
            for f in range(8):
                P.stt("dve", xt[:, f, :], acc[:, f, :], self.mod[:, 40 + f, b:b + 1], xt[:, f, :], ALU.mult, ALU.add,
                      [(acc, f), self.mod, (xt, f)], [(xt, f)])
            if last:
                self.final_out(b, t0, n, xt, acc, sq, rstd, otm)
            else:
                P.dma(fm_view(self.xs[b], 0, 8, t0, n), xt[:], reads=[xt], writes=[self.xs[b]])
        P.phase_end()

    def build(self):
        P = self.P
        self.out_deps = []
        self.setup()
        self.bg_jobs_init()
        self.convert([0])
        for l in range(self.nl):
            ctx_out = l < NL - 1
            self.adaln(l)
            if self.stop_after == "adaln":
                break
            self.phaseA(l)
            if self.stop_after == "A":
                break
            for b in range(NB):
                self.m1(l, b)
                self.m23(l, b, ctx_out)
            if self.stop_after == "M":
                break
            self.c1(l)
            if self.stop_after == "C1":
                break
            if l % 2 == 0:
                self.c2_dense(l)
            else:
                self.c2_moe(l)
        P.barrier()
        return P.nc


def _consts():
    c = np.zeros((128, NCST), np.float32)
    i = np.arange(128)
    c[:, C_ID:C_ID + 128] = np.eye(128)
    c[:, C_TRI:C_TRI + 128] = (i[:, None] <= i[None, :])
    c[:, C_TRU:C_TRU + 128] = (i[:, None] >= i[None, :])
    c[:, C_SG:C_SG + 128] = (i[:, None] > i[None, :])
    c[:, C_SL:C_SL + 128] = (i[:, None] < i[None, :])
    c[:, C_ONE:C_ONE + 128] = 1.0
    for e in range(8):
        c[e, C_SEL + e * 128:C_SEL + (e + 1) * 128] = 1.0
    invc = np.ones((128, 2, WP), np.float32)
    wins = (2, 4, 8, 16)
    for g, win in enumerate(wins):
        for (L, off) in ((CTXL, 16), (SEQ, 304)):
            t = np.arange(L)
            lo = np.clip(t - win // 2, 0, L)
            hi = np.clip(t - win // 2 + win, 0, L)
            p0 = (g % 2) * 64
            invc[p0:p0 + 64, g // 2, off:off + L] = (1.0 / (hi - lo).astype(np.float32))[None, :]
    return c, invc.reshape(128, 2 * WP)


def _prep_shared(inp):
    f = lambda a: np.ascontiguousarray(np.asarray(a, dtype=np.float32))
    small = np.zeros((NL, 128, NSM), np.float32)
    ttab = np.zeros((NL, 128, 4, 15, 64), np.float32)
    cq = np.arange(64)
    sc = np.clip(cq - 8, 0, 48)
    kc = np.arange(64)
    valid = (kc[None, :] >= sc[:, None]) & (kc[None, :] < sc[:, None] + 16)
    idx = np.clip(kc[None, :] - cq[:, None] + 15, 0, 30)
    for l in range(NL):
        small[l, :, O_BADA:O_BADA + 48] = f(inp["b_ada"])[l].reshape(48, 128).T
        small[l, :, O_PSC:O_PSC + 2] = f(inp["pool_scale"])[l].reshape(2, 128).T
        cw = f(inp["conv_w"])[l]
        small[l, :, O_CW:O_CW + 6] = cw.reshape(3, 2, 128).transpose(2, 1, 0).reshape(128, 6)
        scw = f(inp["ssd_conv_w"])[l]
        small[l, :, O_SCW:O_SCW + 18] = scw.reshape(3, 6, 128).transpose(2, 1, 0).reshape(128, 18)
        small[l, :, O_SCB:O_SCB + 6] = f(inp["ssd_conv_b"])[l].reshape(6, 128).T
        small[l, :, O_DTB:O_DTB + 8] = f(inp["ssd_dt_bias"])[l].reshape(1, 8)
        small[l, :, O_ALOG:O_ALOG + 8] = f(inp["ssd_a_log"])[l].reshape(1, 8)
        small[l, :, O_DSK:O_DSK + 256] = np.repeat(f(inp["ssd_d"])[l], 64)[None, :]
        small[l, :, O_SNG:O_SNG + 256] = f(inp["ssd_norm_g"])[l][None, :]
        pw = f(inp["pool_w"])[l]
        blk = np.zeros((128, 2, 128), np.float32)
        for g in range(4):
            p0 = (g % 2) * 64
            blk[p0:p0 + 64, g // 2, p0:p0 + 64] = pw[g]
        small[l, :, O_PW:O_PW + 256] = blk.reshape(128, 256)
        rpb = f(inp["na_rpb"])[l]
        gath = rpb[:, :, idx]
        tab = np.where(valid[None, None], gath, np.float32(-30000.0))
        tab = tab.transpose(2, 0, 1, 3)
        ttab[l, 0:64] = tab
        ttab[l, 64:128] = tab
    gsm = np.zeros((128, 104), np.float32)
    gsm[:, 0:8] = f(inp["g_mix"])[0].reshape(8, 128).T
    gsm[:, 8:16] = f(inp["g_mix"])[1].reshape(8, 128).T
    gsm[:, 16:24] = f(inp["g_ffn"])[0].reshape(8, 128).T
    gsm[:, 24:32] = f(inp["g_ffn"])[1].reshape(8, 128).T
    gsm[:, 32:40] = f(inp["g_final"]).reshape(8, 128).T
    gsm[:, 40:104] = f(inp["moe_router"])[0].reshape(8, 128, 8).transpose(1, 0, 2).reshape(128, 64)
    cst, invc = _consts()
    return {
        "w_ada": f(inp["w_ada"]), "w_in": f(inp["w_in"]), "w_out": f(inp["w_out"]), "small": small,
        "ttab": ttab.reshape(NL, 128, 3840), "gsm": gsm, "cst": cst, "invc": invc,
        "ffn_gu": f(inp["ffn_w_gu"])[0], "ffn_down": f(inp["ffn_w_down"])[0],
        "moe_gu": f(inp["moe_w_gu"])[0], "moe_down": f(inp["moe_w_down"])[0],
    }


def _core_inputs(inp, shared, core):
    b0 = core * NB
    x = np.asarray(inp["x"], dtype=np.float32)
    c = np.asarray(inp["c"], dtype=np.float32)
    ctx = np.asarray(inp["ctx"], dtype=np.float32)
    cc = np.concatenate([c[b0:b0 + NB], np.asarray(inp["c_ctx"], dtype=np.float32)[None, :]], axis=0).T
    m = dict(shared)
    m["x"] = np.ascontiguousarray(x[b0:b0 + NB])
    m["ctx"] = np.ascontiguousarray(ctx[b0:b0 + NB])
    m["cc"] = np.ascontiguousarray(cc)
    return m


_NC_CACHE = {}


def kernel(**inputs):
    if "nc" not in _NC_CACHE:
        mdl = Model()
        _NC_CACHE["nc"] = mdl.build()
    nc = _NC_CACHE["nc"]
    shared = _prep_shared(inputs)
    in_maps = [_core_inputs(inputs, shared, core) for core in range(8)]
    res = run_bass_kernel_spmd(nc, in_maps, core_ids=list(range(8)))
    out = np.concatenate([np.asarray(r["out"], dtype=np.float32) for r in res.results], axis=0)
    return out
```
